# Optimizing a Trainium2 kernel written in Bass

```python
import math
import jax
import jax.numpy as jnp
from jax import lax
import numpy as np

D_MODEL = 1024
BATCH = 4
SEQ = 8192
DEPTH = 1

GRID_W = 64
CTX_LEN = 256
QBLOCK = 128
ROPE_THETA = 10000.0
EPS = 1e-6

MLA_HEADS = 8
MLA_Q_RANK = 256
MLA_KV_RANK = 128
MLA_NOPE = 64
MLA_ROPE = 32
MLA_V = 64
DIFF_HEADS = 4
DIFF_HD = 64
N_EXPERTS = 16
EXPERT_FF = 1024
EC_CAPACITY = 2

MLA_Q_OFF = 0
MLA_KV_OFF = MLA_Q_OFF + MLA_Q_RANK
MLA_KR_OFF = MLA_KV_OFF + MLA_KV_RANK
DIFF_Q_OFF = MLA_KR_OFF + MLA_ROPE
DIFF_K_OFF = DIFF_Q_OFF + DIFF_HEADS * 2 * DIFF_HD
DIFF_V_OFF = DIFF_K_OFF + DIFF_HEADS * 2 * DIFF_HD
GATE_OFF = DIFF_V_OFF + DIFF_HEADS * 2 * DIFF_HD
N_IN = GATE_OFF + 2 * D_MODEL
MLA_WIDTH = MLA_HEADS * MLA_V
DIFF_WIDTH = DIFF_HEADS * 2 * DIFF_HD
MLA_SCALE = (MLA_NOPE + MLA_ROPE) ** -0.5
DIFF_SCALE = DIFF_HD ** -0.5
DEEPNORM_ALPHA = (2 * DEPTH) ** 0.25
DEEPNORM_BETA = (8 * DEPTH) ** -0.25

kernel_name = 'hybrid_mla_diffattn_ecmoe_dit_block'


def layer_norm(x, g, b):
    xf = x.astype(jnp.float32)
    mu = jnp.mean(xf, axis=-1, keepdims=True)
    var = jnp.mean(jnp.square(xf - mu), axis=-1, keepdims=True)
    y = (xf - mu) * lax.rsqrt(var + EPS) * g.astype(jnp.float32) + b.astype(jnp.float32)
    return y.astype(x.dtype)


def rms_norm(x, g):
    xf = x.astype(jnp.float32)
    y = xf * lax.rsqrt(jnp.mean(xf * xf, axis=-1, keepdims=True) + EPS) * g.astype(jnp.float32)
    return y.astype(x.dtype)


def rope_1d(x, pos):
    d = x.shape[-1]
    half = d // 2
    inv = ROPE_THETA ** (-(jnp.arange(half, dtype=jnp.float32) * 2.0 / d))
    ang = pos.astype(jnp.float32)[:, None] * inv
    cos, sin = jnp.cos(ang), jnp.sin(ang)
    xf = x.astype(jnp.float32)
    x1, x2 = xf[..., :half], xf[..., half:]
    return jnp.concatenate([x1 * cos - x2 * sin, x2 * cos + x1 * sin], axis=-1).astype(x.dtype)


def axial_rope(x, row, col):
    h = x.shape[-1] // 2
    return jnp.concatenate([rope_1d(x[..., :h], row), rope_1d(x[..., h:], col)], axis=-1)


def split_heads(t, n_heads):
    b, s, _ = t.shape
    return t.reshape(b, s, n_heads, -1).transpose(0, 2, 1, 3)


def merge_heads(t):
    b, h, s, d = t.shape
    return t.transpose(0, 2, 1, 3).reshape(b, s, h * d)


def over_query_blocks(fn, q):
    b, h, sq = q.shape[:3]
    rest = q.shape[3:]
    nb = sq // QBLOCK
    qb = jnp.moveaxis(q.reshape(b, h, nb, QBLOCK, *rest), 2, 0)
    out = lax.map(fn, qb)
    out = jnp.moveaxis(out, 0, 2)
    return out.reshape(b, h, sq, out.shape[-1])


def softmax_attention(q, k, v, scale):
    def one(qb):
        s = jnp.einsum('bhqd,bhkd->bhqk', qb, k).astype(jnp.float32) * scale
        p = jax.nn.softmax(s, axis=-1)
        return jnp.einsum('bhqk,bhkd->bhqd', p.astype(v.dtype), v)
    return over_query_blocks(one, q)


def diff_attention(q, k, v, lam, scale):
    def one(qb):
        s = jnp.einsum('bhqmd,bhkmd->bhmqk', qb, k).astype(jnp.float32) * scale
        p = jax.nn.softmax(s, axis=-1)
        w = p[:, :, 0] - lam * p[:, :, 1]
        return jnp.einsum('bhqk,bhkd->bhqd', w.astype(v.dtype), v)
    return over_query_blocks(one, q)


def mixer_projections(h, w_in, g_q, w_uq, g_kv, w_ukv, pos):
    b, s, _ = h.shape
    p = h @ w_in
    c_q = rms_norm(p[..., MLA_Q_OFF:MLA_KV_OFF], g_q)
    q = split_heads(c_q @ w_uq, MLA_HEADS)
    c_kv = rms_norm(p[..., MLA_KV_OFF:MLA_KR_OFF], g_kv)
    kv = split_heads(c_kv @ w_ukv, MLA_HEADS)
    q_nope, q_rope = q[..., :MLA_NOPE], q[..., MLA_NOPE:]
    k_nope, v_mla = kv[..., :MLA_NOPE], kv[..., MLA_NOPE:]
    k_rope = p[..., MLA_KR_OFF:DIFF_Q_OFF][:, None]
    dq = p[..., DIFF_Q_OFF:DIFF_K_OFF].reshape(b, s, DIFF_HEADS, 2, DIFF_HD).transpose(0, 2, 3, 1, 4)
    dk = p[..., DIFF_K_OFF:DIFF_V_OFF].reshape(b, s, DIFF_HEADS, 2, DIFF_HD).transpose(0, 2, 3, 1, 4)
    dv = split_heads(p[..., DIFF_V_OFF:GATE_OFF], DIFF_HEADS)
    if pos is not None:
        row, col = pos
        q_rope = axial_rope(q_rope, row, col)
        k_rope = axial_rope(k_rope, row, col)
        dq = axial_rope(dq, row, col)
        dk = axial_rope(dk, row, col)
    q_mla = jnp.concatenate([q_nope, q_rope], axis=-1)
    k_mla = jnp.concatenate([k_nope, jnp.broadcast_to(k_rope, (b, MLA_HEADS, s, MLA_ROPE))], axis=-1)
    dq = dq.transpose(0, 1, 3, 2, 4)
    dk = dk.transpose(0, 1, 3, 2, 4)
    gates = jax.nn.sigmoid(p[..., GATE_OFF:].astype(jnp.float32)).astype(h.dtype)
    return (q_mla, k_mla, v_mla, dq, dk, dv, gates)


def mixer_output(q_mla, dq, gates, k_mla, v_mla, dk, dv, lam, lambda_init, g_subln, w_o_mla, w_o_diff, w_out):
    o_mla = merge_heads(softmax_attention(q_mla, k_mla, v_mla, MLA_SCALE))
    o_diff = rms_norm(diff_attention(dq, dk, dv, lam, DIFF_SCALE), g_subln) * (1.0 - lambda_init)
    o_diff = merge_heads(o_diff)
    y = gates[..., :D_MODEL] * (o_mla @ w_o_mla) + gates[..., D_MODEL:] * (o_diff @ w_o_diff)
    return y @ w_out


def ec_moe(h, w_router, b_router, w1, w3, w2):
    b, n, _ = h.shape
    cap = EC_CAPACITY * n // N_EXPERTS
    aff = jax.nn.softmax((h @ w_router + b_router).astype(jnp.float32), axis=-1)
    g, idx = lax.top_k(jnp.swapaxes(aff, 1, 2), cap)
    bidx = jnp.arange(b)[:, None, None]
    xe = h[bidx, idx]
    hid = jax.nn.silu(jnp.einsum('becd,edf->becf', xe, w1)) * jnp.einsum('becd,edf->becf', xe, w3)
    ye = jnp.einsum('becf,efd->becd', hid, w2) * g[..., None].astype(h.dtype)
    return jnp.zeros_like(h).at[bidx, idx].add(ye)


def setup_inputs(seed: int = 0) -> dict:
    key = jax.random.key(seed)
    ks = jax.random.split(key, 32)
    L, D = DEPTH, D_MODEL

    def nrm(k, shape, scale):
        return jax.random.normal(k, shape, jnp.float32) * scale

    return {
        'x': nrm(ks[0], (BATCH, SEQ, D), 1.0),
        'c': nrm(ks[1], (BATCH, D), 1.0),
        'ctx': nrm(ks[2], (BATCH, CTX_LEN, D), 1.0),
        'c_ctx': nrm(ks[3], (D,), 1.0),
        'w_ada': nrm(ks[4], (L, D, 6 * D), 0.5 * D ** -0.5),
        'b_ada': nrm(ks[5], (L, 6 * D), 0.02),
        'w_in': nrm(ks[6], (L, D, N_IN), D ** -0.5),
        'mla_g_q': 1.0 + nrm(ks[7], (L, MLA_Q_RANK), 0.02),
        'mla_w_uq': nrm(ks[8], (L, MLA_Q_RANK, MLA_HEADS * (MLA_NOPE + MLA_ROPE)), MLA_Q_RANK ** -0.5),
        'mla_g_kv': 1.0 + nrm(ks[9], (L, MLA_KV_RANK), 0.02),
        'mla_w_ukv': nrm(ks[10], (L, MLA_KV_RANK, MLA_HEADS * (MLA_NOPE + MLA_V)), MLA_KV_RANK ** -0.5),
        'mla_w_o': nrm(ks[11], (L, MLA_WIDTH, D), MLA_WIDTH ** -0.5),
        'diff_lambda': nrm(ks[12], (L, 4, DIFF_HD), 0.1),
        'diff_g_subln': 1.0 + nrm(ks[13], (L, 2 * DIFF_HD), 0.02),
        'diff_w_o': nrm(ks[14], (L, DIFF_WIDTH, D), DIFF_WIDTH ** -0.5),
        'w_out': nrm(ks[15], (L, D, D), DEEPNORM_BETA * D ** -0.5),
        'ln1_g': 1.0 + nrm(ks[16], (L, D), 0.02),
        'ln1_b': nrm(ks[17], (L, D), 0.02),
        'moe_w_router': nrm(ks[18], (L, D, N_EXPERTS), D ** -0.5),
        'moe_b_router': nrm(ks[19], (L, N_EXPERTS), 0.01),
        'moe_w1': nrm(ks[20], (L, N_EXPERTS, D, EXPERT_FF), D ** -0.5),
        'moe_w3': nrm(ks[21], (L, N_EXPERTS, D, EXPERT_FF), D ** -0.5),
        'moe_w2': nrm(ks[22], (L, N_EXPERTS, EXPERT_FF, D), DEEPNORM_BETA * EXPERT_FF ** -0.5),
        'ln2_g': 1.0 + nrm(ks[23], (L, D), 0.02),
        'ln2_b': nrm(ks[24], (L, D), 0.02),
    }


def reference(x, c, ctx, c_ctx, w_ada, b_ada, w_in, mla_g_q, mla_w_uq, mla_g_kv, mla_w_ukv, mla_w_o,
              diff_lambda, diff_g_subln, diff_w_o, w_out, ln1_g, ln1_b, moe_w_router, moe_b_router,
              moe_w1, moe_w3, moe_w2, ln2_g, ln2_b):
    rows = x.shape[1] // GRID_W
    row = jnp.repeat(jnp.arange(rows, dtype=jnp.int32), GRID_W)
    col = jnp.tile(jnp.arange(GRID_W, dtype=jnp.int32), rows)
    for l in range(DEPTH):
        lambda_init = 0.8 - 0.6 * math.exp(-0.3 * l)
        lq1, lk1, lq2, lk2 = diff_lambda[l].astype(jnp.float32)
        lam = jnp.exp(jnp.sum(lq1 * lk1)) - jnp.exp(jnp.sum(lq2 * lk2)) + lambda_init
        mod = jax.nn.silu(c) @ w_ada[l] + b_ada[l]
        sh1, sc1, g1, sh2, sc2, g2 = jnp.split(mod[:, None, :], 6, axis=-1)
        mod_c = jax.nn.silu(c_ctx) @ w_ada[l] + b_ada[l]
        sh1c, sc1c, g1c, sh2c, sc2c, g2c = jnp.split(mod_c, 6)
        proj = (w_in[l], mla_g_q[l], mla_w_uq[l], mla_g_kv[l], mla_w_ukv[l])
        outp = (lam, lambda_init, diff_g_subln[l], mla_w_o[l], diff_w_o[l], w_out[l])
        moe = (moe_w_router[l], moe_b_router[l], moe_w1[l], moe_w3[l], moe_w2[l])
        qm_c, km_c, vm_c, dq_c, dk_c, dv_c, gt_c = mixer_projections(ctx * (1.0 + sc1c) + sh1c, *proj, None)
        qm, km, vm, dq, dk, dv, gt = mixer_projections(x * (1.0 + sc1) + sh1, *proj, (row, col))
        o_lat = mixer_output(qm, dq, gt,
                             jnp.concatenate([km_c, km], axis=2), jnp.concatenate([vm_c, vm], axis=2),
                             jnp.concatenate([dk_c, dk], axis=2), jnp.concatenate([dv_c, dv], axis=2),
                             *outp)
        x = layer_norm(DEEPNORM_ALPHA * x + g1 * o_lat, ln1_g[l], ln1_b[l])
        x = layer_norm(DEEPNORM_ALPHA * x + g2 * ec_moe(x * (1.0 + sc2) + sh2, *moe), ln2_g[l], ln2_b[l])
        if l + 1 < DEPTH:
            o_ctx = mixer_output(qm_c, dq_c, gt_c, km_c, vm_c, dk_c, dv_c, *outp)
            ctx = layer_norm(DEEPNORM_ALPHA * ctx + g1c * o_ctx, ln1_g[l], ln1_b[l])
            ctx = layer_norm(DEEPNORM_ALPHA * ctx + g2c * ec_moe(ctx * (1.0 + sc2c) + sh2c, *moe),
                             ln2_g[l], ln2_b[l])
    return x
```

```python
import contextlib
import numpy as np
import concourse.bass as bass
import concourse.mybir as mybir
from concourse.bass_utils import run_bass_kernel_spmd

F32 = mybir.dt.float32
BF16 = mybir.dt.bfloat16
U32 = mybir.dt.uint32
I32 = mybir.dt.int32
AF = mybir.ActivationFunctionType
ALU = mybir.AluOpType
AX = mybir.AxisListType

D = 1024
SEQ = 8192
NQ = 4096
CTX = 256
NK = CTX + SEQ
NKT = NK // 128
EPS = 1e-6
MLA_SCALE = 96.0 ** -0.5
DIFF_SCALE = 64.0 ** -0.5
ALPHA = 2.0 ** 0.25
NE = 16
CPAD = 1024
NITER = 28

O_CQ, O_CKV, O_KR, O_KRP, O_DQ, O_DQP, O_DK, O_DKP, O_DV, O_G = 0, 256, 384, 416, 448, 960, 1472, 1984, 2496, 3008
NW = 5056
ENGS = ["sync", "scalar", "vector", "gpsimd", "tensor"]


class Sem:
    def __init__(self, h):
        self.h = h
        self.n = 0


class Prog:
    def __init__(self, nc):
        self.nc = nc
        self.q = {e: [] for e in ENGS}
        self.es = None

    def begin(self):
        self.es = contextlib.ExitStack()
        self.q = {e: [] for e in ENGS}
        self.fs = {}
        self.nphase = getattr(self, "nphase", 0) + 1

    def sem(self, name):
        return Sem(self.es.enter_context(self.nc.semaphore(name)))

    def sb(self, name, shape, dt):
        return self.es.enter_context(self.nc.sbuf_tensor(name, shape, dt))

    def op(self, eng, fn, sem=None, inc=1):
        if sem is None:
            self.q[eng].append(fn)
            return None
        h = sem.h
        self.q[eng].append(lambda e, fn=fn, h=h, inc=inc: fn(e).then_inc(h, inc))
        sem.n += inc
        return sem.n

    def opf(self, eng, fn):
        if self.fs.get(eng) is None:
            self.fs[eng] = self.sem("fence" + eng[:3] + str(self.nphase))
        v = self.op(eng, fn, self.fs[eng], 1)
        self.wait(eng, self.fs[eng], v)
        return v

    def dma(self, eng, out, in_, sem):
        return self.op(eng, lambda e, out=out, in_=in_: e.dma_start(out=out, in_=in_), sem, 16)

    def wait(self, eng, sem, val):
        if val is None or val <= 0:
            return
        h = sem.h
        self.q[eng].append(lambda e, h=h, val=val: e.wait_ge(h, val))

    def end(self):
        with self.nc.Block() as blk:
            for en in ENGS:
                fns = self.q[en]
                if not fns:
                    continue

                def body(e, fns=fns):
                    for f in fns:
                        f(e)

                getattr(blk, en)(body)
        self.es.close()
        self.es = None


class PsRing:
    def __init__(self, P, banks, tag):
        self.P = P
        self.banks = banks
        self.full = P.sem("full" + tag)
        self.free = {"scalar": P.sem("fra" + tag), "vector": P.sem("frv" + tag)}
        self.hist = []
        self.it = 0

    def acquire(self):
        it = self.it
        self.it += 1
        n = len(self.banks)
        if it >= n:
            eng, val = self.hist[it - n]
            self.P.wait("tensor", self.free[eng], val)
        self.hist.append(None)
        return it, self.banks[it % n]

    def produced(self, fn):
        return self.P.op("tensor", fn, self.full, 1)

    def consume_wait(self, eng, it):
        self.P.wait(eng, self.full, it + 1)

    def release(self, eng, its, fn):
        v = self.P.op(eng, fn, self.free[eng], 1)
        for it in its:
            self.hist[it] = (eng, v)
        return v


def build(debug=None, stage=99):
    nc = bass.Bass("TRN2", target_bir_lowering=False)
    dbg = set(debug or [])

    def din(name, shape, dt=F32):
        return nc.dram_tensor(name, list(shape), dt, kind="ExternalInput").ap()

    def scratch(name, shape, dt):
        if name in dbg:
            return nc.dram_tensor(name, list(shape), dt, kind="ExternalOutput").ap()
        return nc.dram_tensor(name, list(shape), dt).ap()

    xT = din("xT", [D, SEQ])
    xown = din("xown", [NQ, D])
    ctxT = din("ctxT", [D, CTX])
    cvec = din("cvec", [128, 8, 2])
    w_ada = din("w_ada", [D, 6 * D])
    b_ada_fm = din("b_ada_fm", [128, 16])
    b_ada_row = din("b_ada_row", [1, 6 * D])
    w_all = din("w_all", [D, NW])
    w_uq_all = din("w_uq_all", [256, 1024])
    w_ukv_all = din("w_ukv_all", [128, 1024])
    g_q_fm = din("g_q_fm", [128, 2])
    g_kv_fm = din("g_kv_fm", [128, 1])
    w_o_mla = din("w_o_mla", [512, D])
    w_o_diff = din("w_o_diff", [512, D])
    w_out = din("w_out", [D, D])
    dlam = din("dlam", [128, 256])
    g_sub_fm = din("g_sub_fm", [128, 1])
    lnrows = din("lnrows", [128, 4, D])
    w_router = din("w_router", [D, NE])
    b_router = din("b_router", [1, NE])
    moe_w1 = din("moe_w1", [NE, D, D])
    moe_w3 = din("moe_w3", [NE, D, D])
    moe_w2 = din("moe_w2", [NE, D, D])
    rope64_c = din("rope64_c", [128, SEQ])
    rope64_s = din("rope64_s", [128, SEQ])
    rope32_c = din("rope32_c", [128, SEQ])
    rope32_s = din("rope32_s", [128, SEQ])
    out = nc.dram_tensor("out", [NQ, D], F32, kind="ExternalOutput").ap()

    KTm = scratch("KTm", [8, 64, NK], BF16)
    KR = scratch("KR", [32, NK], BF16)
    Vm = scratch("Vm", [NK, 8, 65], BF16)
    QTm = scratch("QTm", [8, 96, NQ], BF16)
    KTd = scratch("KTd", [4, 128, NK], BF16)
    Vd = scratch("Vd", [NK, 512], BF16)
    QTd = scratch("QTd", [4, 128, NQ], BF16)
    GT = scratch("GT", [16, 128, NQ], BF16)
    OTm = scratch("OTm", [8, 64, NQ], BF16)
    OTd = scratch("OTd", [4, 128, NQ], F32)
    X1 = scratch("X1", [NQ, D], F32)
    AFF_IN = nc.dram_tensor("AFF_IN", [128, 32 * NE], F32)
    AFF_OUT = nc.dram_tensor("AFF_OUT", [256, 32 * NE], F32)
    XE = [scratch("XE%d" % i, [CPAD, D], BF16) for i in range(NE)]
    YE = [scratch("YE%d" % i, [CPAD, D], F32) for i in range(NE)]
    DBGT = scratch("DBGT", [128, 4096], F32)

    P = Prog(nc)
    top = contextlib.ExitStack()

    def gsb(name, shape, dt):
        return top.enter_context(nc.sbuf_tensor(name, shape, dt))

    modrow = gsb("modrow", [128, 4096], F32)
    modfm = gsb("modfm", [128, 16, 2], F32)
    ones_f = gsb("ones_f", [128, 128], F32)
    ones_b = gsb("ones_b", [128, 128], BF16)
    ident_f = gsb("ident_f", [128, 128], F32)
    ident_b = gsb("ident_b", [128, 128], BF16)
    neglam = gsb("neglam", [128, 1], F32)
    gsub = gsb("gsub", [128, 1], F32)
    eps_t = gsb("eps_t", [128, 1], F32)
    psall = top.enter_context(nc.psum_tensor("psall", [128, 8, 512], F32))
    psum = [psall[:, i, :] for i in range(8)]

    P.begin()
    s_ld = P.sem("p0ld")
    s_a = P.sem("p0a")
    s_v = P.sem("p0v")
    s_g = P.sem("p0g")
    s_pe = P.sem("p0pe")
    s_wfb = [P.sem("p0wf0"), P.sem("p0wf1")]
    cv = P.sb("cv", [128, 8, 2], F32)
    sv = P.sb("sv", [128, 8, 2], F32)
    srep = P.sb("srep", [128, 8, 128], F32)
    zer = P.sb("zer", [128, 128], F32)
    wch = [P.sb("wch%d" % i, [128, 8, 512], F32) for i in range(2)]
    brow = P.sb("brow", [1, 6 * D], F32)
    bfm = P.sb("bfm", [128, 16], F32)
    dl = P.sb("dl", [128, 256], F32)
    dlp = P.sb("dlp", [128, 128], F32)
    lsum = P.sb("lsum", [128, 2], F32)
    iot = P.sb("iot", [128, 128], F32)
    iop = P.sb("iop", [128, 1], F32)

    P.dma("sync", cv[:], cvec, s_ld)
    P.dma("sync", brow[:], b_ada_row, s_ld)
    P.dma("sync", bfm[:], b_ada_fm, s_ld)
    P.dma("sync", dl[:], dlam, s_ld)
    v_ld0 = P.dma("sync", gsub[:], g_sub_fm, s_ld)
    P.op("gpsimd", lambda e: e.memset(ones_f[:], 1.0))
    P.op("gpsimd", lambda e: e.memset(ones_b[:], 1.0))
    P.op("gpsimd", lambda e: e.memset(zer[:], 0.0))
    P.op("gpsimd", lambda e: e.memset(eps_t[:], EPS))
    P.op("gpsimd", lambda e: e.iota(iot[:], [[1, 128]], base=0, channel_multiplier=0, allow_small_or_imprecise_dtypes=True))
    P.opf("gpsimd", lambda e: e.iota(iop[:], [[0, 1]], base=0, channel_multiplier=1, allow_small_or_imprecise_dtypes=True))
    P.opf("gpsimd", lambda e: e.tensor_scalar(out=ident_f[:], in0=iot[:], scalar1=iop[:, 0:1], scalar2=None, op0=ALU.is_equal))
    v_g0 = P.op("gpsimd", lambda e: e.tensor_copy(out=ident_b[:], in_=ident_f[:]), s_g, 1)
    P.wait("scalar", s_ld, v_ld0)
    P.wait("scalar", s_g, v_g0)
    P.opf("scalar", lambda e: e.activation(out=sv[:], in_=cv[:], func=AF.Silu))
    for k in range(8):
        va = P.op("scalar", lambda e, k=k: e.activation(out=srep[:, k, :], in_=zer[:], func=AF.Identity, bias=sv[:, k, 0:1], scale=1.0), s_a, 1)
    v_srep = va
    P.wait("vector", s_ld, v_ld0)
    P.op("vector", lambda e: e.tensor_tensor(out=dlp[:, 0:64], in0=dl[:, 0:64], in1=dl[:, 64:128], op=ALU.mult))
    P.opf("vector", lambda e: e.tensor_tensor(out=dlp[:, 64:128], in0=dl[:, 128:192], in1=dl[:, 192:256], op=ALU.mult))
    P.op("vector", lambda e: e.tensor_reduce(out=lsum[:, 0:1], in_=dlp[:, 0:64], axis=AX.X, op=ALU.add))
    v_ls = P.op("vector", lambda e: e.tensor_reduce(out=lsum[:, 1:2], in_=dlp[:, 64:128], axis=AX.X, op=ALU.add), s_v, 1)
    P.wait("scalar", s_v, v_ls)
    v_le = P.op("scalar", lambda e: e.activation(out=lsum[:], in_=lsum[:], func=AF.Exp), s_a, 1)
    P.wait("vector", s_a, v_le)
    P.opf("vector", lambda e: e.tensor_scalar(out=neglam[:], in0=lsum[:, 1:2], scalar1=lsum[:, 0:1], scalar2=-0.2, op0=ALU.subtract, op1=ALU.add))

    wsrc = w_ada.rearrange("(k p) n -> p k n", p=128)
    pe_done = []
    fm_done = []
    row_done = []
    for j in range(12):
        buf = wch[j % 2]
        if j >= 2:
            P.wait("gpsimd", s_v, pe_done[j - 2])
        v_w = P.dma("gpsimd", buf[:], wsrc[:, :, j * 512:(j + 1) * 512], s_wfb[j % 2])
        P.wait("tensor", s_wfb[j % 2], v_w)
        if j == 0:
            P.wait("tensor", s_a, v_srep)
        if j < 4:
            for q in range(4):
                jj = j * 4 + q
                if jj >= 2:
                    P.wait("tensor", s_v, fm_done[jj - 2])
                for k in range(8):
                    fn = lambda e, k=k, q=q, jj=jj, buf=buf: e.matmul(psum[jj % 2][:, 0:2], lhsT=buf[:, k, q * 128:(q + 1) * 128], rhs=sv[:, k, :], start=(k == 0), stop=(k == 7))
                    if k == 7:
                        vp = P.op("tensor", fn, s_pe, 1)
                    else:
                        P.op("tensor", fn)
                P.wait("vector", s_pe, vp)
                if jj == 0:
                    P.wait("vector", s_ld, v_ld0)
                fm_done.append(P.op("vector", lambda e, jj=jj: e.tensor_scalar(out=modfm[:, jj, :], in0=psum[jj % 2][:, 0:2], scalar1=bfm[:, jj:jj + 1], scalar2=(1.0 if jj >= 8 else 0.0), op0=ALU.add, op1=ALU.add), s_v, 1))
            pe_done.append(fm_done[-1])
        else:
            jb = j - 4
            bank = psum[2 + (jb % 2)]
            if jb >= 2:
                P.wait("tensor", s_v, row_done[jb - 2])
            for k in range(8):
                P.op("tensor", lambda e, k=k, buf=buf, bank=bank: e.matmul(bank[:], lhsT=srep[:, k, :], rhs=buf[:, k, :], start=(k == 0), stop=False))
            vp = P.op("tensor", lambda e, j=j, bank=bank: e.matmul(bank[:], lhsT=ones_f[0:1, :], rhs=brow[0:1, j * 512:(j + 1) * 512], start=False, stop=True), s_pe, 1)
            P.wait("vector", s_pe, vp)
            addc = 1.0 if jb in (4, 5) else 0.0
            row_done.append(P.op("vector", lambda e, jb=jb, bank=bank, addc=addc: e.tensor_scalar(out=modrow[:, jb * 512:(jb + 1) * 512], in0=bank[:], scalar1=addc, scalar2=None, op0=ALU.add), s_v, 1))
            pe_done.append(row_done[-1])
    if "DBG0" in dbg:
        dbt = P.sb("dbt", [128, 64], F32)
        P.op("vector", lambda e: e.memset(dbt[:], 0.0))
        P.op("vector", lambda e: e.tensor_copy(out=dbt[:, 0:1], in_=neglam[:]))
        P.op("vector", lambda e: e.tensor_copy(out=dbt[:, 1:3], in_=lsum[:]))
        P.op("vector", lambda e: e.tensor_copy(out=dbt[:, 3:35], in_=modfm[:].rearrange("p a b -> p (a b)")))
        vdd = P.op("vector", lambda e: e.tensor_copy(out=dbt[:, 35:51], in_=dl[:, 0:16]), s_v, 1)
        P.wait("sync", s_v, vdd)
        P.dma("sync", DBGT[:, 64:4096], modrow[:, 64:4096], s_ld)
        P.dma("sync", DBGT[:, 0:64], dbt[:], s_ld)
        P.wait("sync", s_ld, s_ld.n)
    P.end()
    if stage <= 0:
        top.close()
        return nc

    P.begin()
    wall = P.sb("wall", [128, 8, NW], BF16)
    wuq = P.sb("wuq", [128, 2, 1024], BF16)
    wukv = P.sb("wukv", [128, 1024], BF16)
    gq = P.sb("gq", [128, 2], F32)
    gkv = P.sb("gkv", [128, 1], F32)
    xs = [P.sb("xs%d" % i, [128, 8, 512], F32) for i in range(2)]
    hT = [P.sb("hT%d" % i, [128, 8, 512], BF16) for i in range(2)]
    rt = [[P.sb("rt%d_%d" % (i, t), [128, 512], F32) for t in range(4)] for i in range(2)]
    ckv_sb = P.sb("ckv_sb", [128, 512], F32)
    ckv_sq = P.sb("ckv_sq", [128, 512], F32)
    ckvn = P.sb("ckvn", [128, 512], BF16)
    cq_sb = P.sb("cq_sb", [128, 2, 512], F32)
    cq_sq = P.sb("cq_sq", [128, 2, 512], F32)
    cqn = P.sb("cqn", [128, 2, 512], BF16)
    rtmps = [P.sb("rtmp%d" % i, [128, 512], F32) for i in range(4)]
    rt1 = P.sb("rt1", [128, 512], F32)
    rt2 = P.sb("rt2", [128, 512], F32)
    NST = 6
    stg = {"scalar": [P.sb("stga%d" % i, [128, 512], BF16) for i in range(NST)],
           "vector": [P.sb("stgv%d" % i, [128, 512], BF16) for i in range(NST)]}
    vst = [P.sb("vst%d" % i, [128, 8, 65], BF16) for i in range(2)]

    s_w = P.sem("p1w")
    s_xb = [P.sem("p1x0"), P.sem("p1x1")]
    s_h = P.sem("p1h")
    s_hfree = P.sem("p1hf")
    s_xfree = P.sem("p1xf")
    s_rtfree = P.sem("p1rf")
    s_cn = P.sem("p1cn")
    s_cs = P.sem("p1cs")
    s_st = {"scalar": P.sem("p1sta"), "vector": P.sem("p1stv")}
    s_outs = {"scalar": [P.sem("p1oa%d" % i) for i in range(NST)], "vector": [P.sem("p1ov%d" % i) for i in range(NST)]}
    s_vst = P.sem("p1vst")
    s_vouts = [P.sem("p1vo0"), P.sem("p1vo1")]
    ring = PsRing(P, psum, "p1")
    stg_n = {"scalar": 0, "vector": 0}
    stg_hist = {"scalar": [], "vector": []}

    wsrc = w_all.rearrange("(k p) n -> p k n", p=128)
    for c in range(8):
        P.dma("gpsimd", wall[:, :, c * 632:(c + 1) * 632], wsrc[:, :, c * 632:(c + 1) * 632], s_w)
    P.dma("gpsimd", wuq[:], w_uq_all.rearrange("(k p) n -> p k n", p=128), s_w)
    P.dma("gpsimd", wukv[:], w_ukv_all, s_w)
    P.dma("gpsimd", gq[:], g_q_fm, s_w)
    v_w = P.dma("gpsimd", gkv[:], g_kv_fm, s_w)
    P.op("gpsimd", lambda e: e.memset(vst[0][:, :, 64:65], 1.0))
    v_vm = P.op("gpsimd", lambda e: e.memset(vst[1][:, :, 64:65], 1.0), s_cs, 1)
    P.wait("tensor", s_w, v_w)
    P.wait("vector", s_w, v_w)
    P.wait("scalar", s_w, v_w)
    P.wait("scalar", s_cs, v_vm)
    P.wait("vector", s_cs, v_vm)

    def stage_out(eng, its, compute_fn, dst_list):
        i = stg_n[eng]
        stg_n[eng] += 1
        slot = stg[eng][i % NST]
        so = s_outs[eng][i % NST]
        if i >= NST:
            P.wait(eng, so, stg_hist[eng][i - NST])
        ring.release(eng, its, lambda e, slot=slot: compute_fn(e, slot))
        val = ring.free[eng].n
        P.wait("sync", ring.free[eng], val)
        last = None
        for dram_ap, sl in dst_list:
            last = P.dma("sync", dram_ap, sl(slot), so)
        stg_hist[eng].append(last)

    vst_n = [0]
    vst_hist = []
    alt = [0]

    def next_eng():
        alt[0] += 1
        return "scalar" if alt[0] % 2 else "vector"

    hfree_hist = []
    xfree_hist = []
    rtfree_hist = []
    for tt in range(17):
        N = 256 if tt == 0 else 512
        own = 1 <= tt <= 8
        isctx = tt == 0
        kcol = 0 if isctx else 256 + (tt - 1) * 512
        qcol = (tt - 1) * 512
        b = tt % 2
        mi = 1 if isctx else 0
        if tt >= 2:
            P.wait("gpsimd", s_h, xfree_hist[tt - 2])
        src = (ctxT if isctx else xT[:, qcol:qcol + 512]).rearrange("(k p) n -> p k n", p=128)
        s_x = s_xb[b]
        v_x = P.dma("gpsimd", xs[b][:, :, 0:N], src, s_x)
        if not isctx:
            if tt >= 3:
                P.wait("gpsimd", ring.free["vector"], rtfree_hist[tt - 3])
            for ti, tab in enumerate([rope64_c, rope64_s, rope32_c, rope32_s]):
                v_x = P.dma("gpsimd", rt[b][ti][:], tab[:, qcol:qcol + 512], s_x)
        P.wait("vector", s_x, v_x)
        if tt >= 2:
            P.wait("vector", ring.free["scalar"], hfree_hist[tt - 2])
        for k in range(8):
            fn = lambda e, k=k, b=b, N=N, mi=mi: e.tensor_scalar(out=hT[b][:, k, 0:N], in0=xs[b][:, k, 0:N], scalar1=modfm[:, 8 + k, mi:mi + 1], scalar2=modfm[:, k, mi:mi + 1], op0=ALU.mult, op1=ALU.add)
            if k == 7:
                v_h = P.op("vector", fn, s_h, 1)
            else:
                P.op("vector", fn)
        xfree_hist.append(v_h)
        P.wait("tensor", s_h, v_h)
        H = hT[b]

        def mm_full(col0, M, N=N, H=H):
            it, bank = ring.acquire()
            for k in range(8):
                fn = lambda e, k=k, bank=bank: e.matmul(bank[0:M, 0:N], lhsT=wall[:, k, col0:col0 + M], rhs=H[:, k, 0:N], start=(k == 0), stop=(k == 7))
                if k == 7:
                    ring.produced(fn)
                else:
                    P.op("tensor", fn)
            return it, bank

        it_ckv, bk_ckv = mm_full(O_CKV, 128)
        ring.consume_wait("scalar", it_ckv)
        P.op("scalar", lambda e, bk=bk_ckv, N=N: e.activation(out=ckv_sb[:, 0:N], in_=bk[:, 0:N], func=AF.Copy))
        v_sq = ring.release("scalar", [it_ckv], lambda e, bk=bk_ckv, N=N: e.activation(out=ckv_sq[:, 0:N], in_=bk[:, 0:N], func=AF.Square))
        if own:
            its_cq = []
            for c in range(2):
                it, bk = mm_full(O_CQ + c * 128, 128)
                ring.consume_wait("scalar", it)
                P.op("scalar", lambda e, bk=bk, c=c: e.activation(out=cq_sb[:, c, :], in_=bk[:], func=AF.Copy))
                v_sq = ring.release("scalar", [it], lambda e, bk=bk, c=c: e.activation(out=cq_sq[:, c, :], in_=bk[:], func=AF.Square))

        def rope_item(colx, colp, M, tabc, tabs, scale, dsts):
            it1, b1 = mm_full(colx, M)
            if isctx:
                eng = next_eng()
                ring.consume_wait(eng, it1)
                if eng == "scalar":
                    stage_out(eng, [it1], lambda e, slot, b1=b1, N=N: e.activation(out=slot[0:M, 0:N], in_=b1[0:M, 0:N], func=AF.Copy, scale=scale), dsts)
                else:
                    stage_out(eng, [it1], lambda e, slot, b1=b1, N=N: e.tensor_scalar(out=slot[0:M, 0:N], in0=b1[0:M, 0:N], scalar1=scale, scalar2=None, op0=ALU.mult), dsts)
                return
            it2, b2 = mm_full(colp, M)
            ring.consume_wait("vector", it2)
            P.op("vector", lambda e, b1=b1: e.tensor_tensor(out=rt1[0:M, :], in0=b1[0:M, :], in1=tabc[0:M, :], op=ALU.mult))
            P.op("vector", lambda e, b2=b2: e.tensor_tensor(out=rt2[0:M, :], in0=b2[0:M, :], in1=tabs[0:M, :], op=ALU.mult))
            stage_out("vector", [it1, it2], lambda e, slot: e.tensor_tensor(out=slot[0:M, :], in0=rt1[0:M, :], in1=rt2[0:M, :], op=ALU.add), dsts)

        R = rt[b]
        for hd in range(4):
            rope_item(O_DK + hd * 128, O_DKP + hd * 128, 128, R[0], R[1], 1.0,
                      [(KTd[hd, :, kcol:kcol + N], lambda s, N=N: s[:, 0:N])])
        rope_item(O_KR, O_KRP, 32, R[2], R[3], 1.0, [(KR[:, kcol:kcol + N], lambda s, N=N: s[0:32, 0:N])])
        for s4 in range(N // 128):
            it, bank = ring.acquire()
            for k in range(8):
                fn = lambda e, k=k, bank=bank, s4=s4, H=H: e.matmul(bank[:], lhsT=H[:, k, s4 * 128:(s4 + 1) * 128], rhs=wall[:, k, O_DV:O_DV + 512], start=(k == 0), stop=(k == 7))
                if k == 7:
                    ring.produced(fn)
                else:
                    P.op("tensor", fn)
            eng = next_eng()
            ring.consume_wait(eng, it)
            r0 = kcol + s4 * 128
            if eng == "scalar":
                stage_out(eng, [it], lambda e, slot, bank=bank: e.activation(out=slot[:], in_=bank[:], func=AF.Copy), [(Vd[r0:r0 + 128, :], lambda s: s[:])])
            else:
                stage_out(eng, [it], lambda e, slot, bank=bank: e.tensor_copy(out=slot[:], in_=bank[:]), [(Vd[r0:r0 + 128, :], lambda s: s[:])])

        def rms_finish(sq_aps, nin, src_aps, g_ap_fn, dst_aps, rtmp, rtmp2, N=N):
            it, bank = ring.acquire()
            P.wait("tensor", ring.free["scalar"], v_sq)
            for c in range(len(sq_aps)):
                fn = lambda e, c=c, bank=bank: e.matmul(bank[:, 0:N], lhsT=ones_f[:], rhs=sq_aps[c], start=(c == 0), stop=(c == len(sq_aps) - 1))
                if c == len(sq_aps) - 1:
                    ring.produced(fn)
                else:
                    P.op("tensor", fn)
            ring.consume_wait("scalar", it)
            v = ring.release("scalar", [it], lambda e, bank=bank: e.activation(out=rtmp[:, 0:N], in_=bank[:, 0:N], func=AF.Sqrt, bias=eps_t[:, 0:1], scale=1.0 / nin))
            P.wait("vector", ring.free["scalar"], v)
            P.op("vector", lambda e: e.reciprocal(out=rtmp2[:, 0:N], in_=rtmp[:, 0:N]))
            for c in range(len(src_aps)):
                fn = lambda e, c=c: e.scalar_tensor_tensor(out=dst_aps[c], in0=src_aps[c], scalar=g_ap_fn(c), in1=rtmp2[:, 0:N], op0=ALU.mult, op1=ALU.mult)
                if c == len(src_aps) - 1:
                    vv = P.op("vector", fn, s_cn, 1)
                else:
                    P.op("vector", fn)
            return vv

        v_ckvn = rms_finish([ckv_sq[:, 0:N]], 128.0, [ckv_sb[:, 0:N]], lambda c: gkv[:, 0:1], [ckvn[:, 0:N]], rtmps[0], rtmps[1])
        if own:
            v_cqn = rms_finish([cq_sq[:, 0, :], cq_sq[:, 1, :]], 256.0, [cq_sb[:, 0, :], cq_sb[:, 1, :]], lambda c: gq[:, c:c + 1], [cqn[:, 0, :], cqn[:, 1, :]], rtmps[2], rtmps[3])
            for hd in range(4):
                rope_item_q = None
                it1, b1 = mm_full(O_DQ + hd * 128, 128)
                it2, b2 = mm_full(O_DQP + hd * 128, 128)
                ring.consume_wait("vector", it2)
                P.op("vector", lambda e, b1=b1, R=R: e.tensor_tensor(out=rt1[:], in0=b1[:], in1=R[0][:], op=ALU.mult))
                P.op("vector", lambda e, b2=b2, R=R: e.tensor_tensor(out=rt2[:], in0=b2[:], in1=R[1][:], op=ALU.mult))
                P.op("vector", lambda e: e.tensor_tensor(out=rt1[:], in0=rt1[:], in1=rt2[:], op=ALU.add))
                stage_out("vector", [it1, it2], lambda e, slot: e.tensor_scalar(out=slot[:], in0=rt1[:], scalar1=DIFF_SCALE, scalar2=None, op0=ALU.mult),
                          [(QTd[hd, :, qcol:qcol + 512], lambda s: s[:])])
            for gc in range(16):
                it, bank = mm_full(O_G + gc * 128, 128)
                ring.consume_wait("scalar", it)
                stage_out("scalar", [it], lambda e, slot, bank=bank: e.activation(out=slot[:], in_=bank[:], func=AF.Sigmoid),
                          [(GT[gc, :, qcol:qcol + 512], lambda s: s[:])])
        P.wait("tensor", s_cn, v_ckvn)
        for j in range(4):
            it, bank = ring.acquire()
            ring.produced(lambda e, j=j, bank=bank, N=N: e.matmul(bank[:, 0:N], lhsT=wukv[:, j * 128:(j + 1) * 128], rhs=ckvn[:, 0:N], start=True, stop=True))
            eng = next_eng()
            ring.consume_wait(eng, it)
            dsts = [(KTm[2 * j, :, kcol:kcol + N], lambda s, N=N: s[0:64, 0:N]), (KTm[2 * j + 1, :, kcol:kcol + N], lambda s, N=N: s[64:128, 0:N])]
            if eng == "scalar":
                stage_out(eng, [it], lambda e, slot, bank=bank, N=N: e.activation(out=slot[:, 0:N], in_=bank[:, 0:N], func=AF.Copy), dsts)
            else:
                stage_out(eng, [it], lambda e, slot, bank=bank, N=N: e.tensor_copy(out=slot[:, 0:N], in_=bank[:, 0:N]), dsts)
        for s4 in range(N // 128):
            it, bank = ring.acquire()
            ring.produced(lambda e, s4=s4, bank=bank: e.matmul(bank[:], lhsT=ckvn[:, s4 * 128:(s4 + 1) * 128], rhs=wukv[:, 512:1024], start=True, stop=True))
            i = vst_n[0]
            vst_n[0] += 1
            vs = vst[i % 2]
            ring.consume_wait("vector", it)
            s_vout = s_vouts[i % 2]
            if i >= 2:
                P.wait("vector", s_vout, vst_hist[i - 2])
            v = ring.release("vector", [it], lambda e, vs=vs, bank=bank: e.tensor_copy(out=vs[:, :, 0:64], in_=bank[:].rearrange("p (h d) -> p h d", h=8)))
            P.wait("sync", ring.free["vector"], v)
            r0 = kcol + s4 * 128
            vst_hist.append(P.dma("sync", Vm[r0:r0 + 128, :, :], vs[:], s_vout))
        if own:
            P.wait("tensor", s_cn, v_cqn)
            for j in range(4):
                it, bank = ring.acquire()
                P.op("tensor", lambda e, j=j, bank=bank: e.matmul(bank[:], lhsT=wuq[:, 0, j * 128:(j + 1) * 128], rhs=cqn[:, 0, :], start=True, stop=False))
                ring.produced(lambda e, j=j, bank=bank: e.matmul(bank[:], lhsT=wuq[:, 1, j * 128:(j + 1) * 128], rhs=cqn[:, 1, :], start=False, stop=True))
                ring.consume_wait("scalar", it)
                dsts = [(QTm[2 * j, 0:64, qcol:qcol + 512], lambda s: s[0:64, :]), (QTm[2 * j + 1, 0:64, qcol:qcol + 512], lambda s: s[64:128, :])]
                stage_out("scalar", [it], lambda e, slot, bank=bank: e.activation(out=slot[:], in_=bank[:], func=AF.Copy, scale=MLA_SCALE), dsts)
            for j in range(2):
                it1, b1 = ring.acquire()
                P.op("tensor", lambda e, j=j, b1=b1: e.matmul(b1[:], lhsT=wuq[:, 0, 512 + j * 128:512 + (j + 1) * 128], rhs=cqn[:, 0, :], start=True, stop=False))
                ring.produced(lambda e, j=j, b1=b1: e.matmul(b1[:], lhsT=wuq[:, 1, 512 + j * 128:512 + (j + 1) * 128], rhs=cqn[:, 1, :], start=False, stop=True))
                it2, b2 = ring.acquire()
                P.op("tensor", lambda e, j=j, b2=b2: e.matmul(b2[:], lhsT=wuq[:, 0, 768 + j * 128:768 + (j + 1) * 128], rhs=cqn[:, 0, :], start=True, stop=False))
                ring.produced(lambda e, j=j, b2=b2: e.matmul(b2[:], lhsT=wuq[:, 1, 768 + j * 128:768 + (j + 1) * 128], rhs=cqn[:, 1, :], start=False, stop=True))
                ring.consume_wait("vector", it2)
                P.op("vector", lambda e, b1=b1, R=R: e.tensor_tensor(out=rt1[:], in0=b1[:], in1=R[2][:], op=ALU.mult))
                P.op("vector", lambda e, b2=b2, R=R: e.tensor_tensor(out=rt2[:], in0=b2[:], in1=R[3][:], op=ALU.mult))
                P.op("vector", lambda e: e.tensor_tensor(out=rt1[:], in0=rt1[:], in1=rt2[:], op=ALU.add))
                dsts = [(QTm[4 * j + hh, 64:96, qcol:qcol + 512], lambda s, hh=hh: s[hh * 32:(hh + 1) * 32, :]) for hh in range(4)]
                stage_out("vector", [it1, it2], lambda e, slot: e.tensor_scalar(out=slot[:], in0=rt1[:], scalar1=MLA_SCALE, scalar2=None, op0=ALU.mult), dsts)
        hfree_hist.append(ring.free["scalar"].n)
        if not isctx:
            rtfree_hist.append(ring.free["vector"].n)
    for eng in ("scalar", "vector"):
        for so in s_outs[eng]:
            P.wait("sync", so, so.n)
    for so in s_vouts:
        P.wait("sync", so, so.n)
    P.end()
    if stage <= 1:
        top.close()
        return nc


    P.begin()
    KTb = [P.sb("KTb%d" % i, [128, NK], BF16) for i in range(2)]
    Vb = [P.sb("Vb%d" % i, [128, NKT, 128], BF16) for i in range(2)]
    QTb = [P.sb("QTb%d" % i, [128, NQ], BF16) for i in range(2)]
    Pb = P.sb("Pb", [128, 4, 512], BF16)
    osb = [P.sb("osb%d" % i, [128, 512], F32) for i in range(2)]
    rden = [P.sb("rden%d" % i, [128, 512], F32) for i in range(2)]
    ostm = [P.sb("ostm%d" % i, [64, 512], BF16) for i in range(2)]
    ostd = [P.sb("ostd%d" % i, [128, 512], F32) for i in range(2)]
    dr0 = P.sb("dr0", [128, 512], F32)
    dr1 = P.sb("dr1", [128, 512], F32)
    dt1 = P.sb("dt1", [128, 512], F32)
    s_uld = [P.sem("p2ld0"), P.sem("p2ld1")]
    s_pes = P.sem("p2pes")
    s_act = P.sem("p2act")
    s_pv = P.sem("p2pv")
    s_fv = P.sem("p2fv")
    s_bc = P.sem("p2bc")
    s_ods = [P.sem("p2od0"), P.sem("p2od1")]
    od_hist = []
    nstep = [0]
    unit_end_pv = []

    def load_unit(u):
        b = u % 2
        if u >= 2:
            P.wait("sync", s_pv, unit_end_pv[u - 2])
        sem = s_uld[b]
        if u < 8:
            P.dma("sync", KTb[b][0:64, :], KTm[u], sem)
            P.dma("sync", KTb[b][64:96, :], KR, sem)
            P.dma("sync", QTb[b][0:96, :], QTm[u], sem)
            for g in range(6):
                P.dma("sync", Vb[b][:, g * 11:(g + 1) * 11, 0:65], Vm[g * 1408:(g + 1) * 1408, u, :].rearrange("(i p) d -> p i d", p=128), sem)
        else:
            hd = u - 8
            P.dma("sync", KTb[b][:, :], KTd[hd], sem)
            P.dma("sync", QTb[b][:, :], QTd[hd], sem)
            for g in range(6):
                P.dma("sync", Vb[b][:, g * 11:(g + 1) * 11, :], Vd[g * 1408:(g + 1) * 1408, hd * 128:(hd + 1) * 128].rearrange("(i p) d -> p i d", p=128), sem)
        return sem.n

    fin_state = {"n": 0, "bc_free": 0, "o_free": [0, 0], "od": []}
    uld_val = {0: load_unit(0)}
    for u in range(12):
        b = u % 2
        mla = u < 8
        if u + 1 < 12:
            uld_val[u + 1] = load_unit(u + 1)
        P.wait("tensor", s_uld[b], uld_val[u])
        R = 4 if mla else 2
        L = 2 if mla else 1
        steps = [(j, i) for j in range(8) for i in range(NKT)]
        base = nstep[0]
        KT, V, QT = KTb[b], Vb[b], QTb[b]
        pend_fin = []

        def emit_S(s, base=base, mla=mla, R=R, KT=KT, QT=QT):
            j, i = steps[s]
            n = base + s
            if s >= R:
                P.wait("tensor", s_act, n - R + 1)
            elif base > 0:
                P.wait("tensor", s_act, base)
            if mla:
                P.op("tensor", lambda e, s=s, i=i, j=j: e.matmul(psum[s % 4][:, :], lhsT=KT[0:96, i * 128:(i + 1) * 128], rhs=QT[0:96, j * 512:(j + 1) * 512], start=True, stop=True), s_pes, 1)
            else:
                r = s % 2
                P.op("tensor", lambda e, r=r, i=i, j=j: e.matmul(psum[2 * r][:, :], lhsT=KT[0:64, i * 128:(i + 1) * 128], rhs=QT[0:64, j * 512:(j + 1) * 512], start=True, stop=True))
                P.op("tensor", lambda e, r=r, i=i, j=j: e.matmul(psum[2 * r + 1][:, :], lhsT=KT[64:128, i * 128:(i + 1) * 128], rhs=QT[64:128, j * 512:(j + 1) * 512], start=True, stop=True), s_pes, 1)

        def emit_exp(s, base=base, mla=mla, R=R):
            n = base + s
            P.wait("scalar", s_pes, n + 1)
            if s >= R:
                P.wait("scalar", s_pv, n - R + 1)
            elif base > 0:
                P.wait("scalar", s_pv, base)
            if mla:
                P.op("scalar", lambda e, s=s: e.activation(out=Pb[:, s % 4, :], in_=psum[s % 4][:, :], func=AF.Exp), s_act, 1)
            else:
                r = s % 2
                P.op("scalar", lambda e, r=r: e.activation(out=Pb[:, 2 * r:2 * r + 2, :], in_=psall[:, 2 * r:2 * r + 2, :], func=AF.Exp), s_act, 1)

        def emit_PV(s, base=base, mla=mla, V=V, u=u):
            j, i = steps[s]
            n = base + s
            P.wait("tensor", s_act, n + 1)
            if mla:
                ob = psum[4 + j % 2]
                if i == 0 and fin_state["o_free"][j % 2]:
                    P.wait("tensor", s_fv, fin_state["o_free"][j % 2])
                P.op("tensor", lambda e, s=s, i=i, ob=ob: e.matmul(ob[0:65, :], lhsT=V[:, i, 0:65], rhs=Pb[:, s % 4, :], start=(i == 0), stop=(i == NKT - 1)), s_pv, 1)
            else:
                r = s % 2
                if i == 0 and fin_state["o_free"][0]:
                    P.wait("tensor", s_fv, fin_state["o_free"][0])
                st, sp = (i == 0), (i == NKT - 1)
                P.op("tensor", lambda e, r=r, i=i, st=st, sp=sp: e.matmul(psum[4][:, :], lhsT=V[:, i, :], rhs=Pb[:, 2 * r, :], start=st, stop=sp))
                P.op("tensor", lambda e, r=r, i=i, st=st, sp=sp: e.matmul(psum[5][:, :], lhsT=V[:, i, :], rhs=Pb[:, 2 * r + 1, :], start=st, stop=sp))
                P.op("tensor", lambda e, r=r, st=st, sp=sp: e.matmul(psum[6][:, :], lhsT=ones_b[:, :], rhs=Pb[:, 2 * r, :], start=st, stop=sp))
                P.op("tensor", lambda e, r=r, st=st, sp=sp: e.matmul(psum[7][:, :], lhsT=ones_b[:, :], rhs=Pb[:, 2 * r + 1, :], start=st, stop=sp), s_pv, 1)
            if i == NKT - 1:
                fin_dve(u, j, n + 1)

        def fin_dve(u, j, pvval):
            k = fin_state["n"]
            fin_state["n"] += 1
            P.wait("vector", s_pv, pvval)
            if u < 8:
                ob = psum[4 + j % 2]
                o, rd, stg = osb[k % 2], rden[k % 2], ostm[k % 2]
                v = P.op("vector", lambda e, ob=ob, o=o: e.tensor_copy(out=o[0:65, :], in_=ob[0:65, :]), s_fv, 1)
                fin_state["o_free"][j % 2] = v
                v2 = P.op("vector", lambda e, o=o, rd=rd: e.reciprocal(out=rd[64:65, :], in_=o[64:65, :]), s_fv, 1)
                pend_fin.append((u, j, k, v2))
            else:
                hd = u - 8
                stg = ostd[k % 2]
                s_od = s_ods[k % 2]
                if len(fin_state["od"]) >= 2:
                    P.wait("vector", s_od, fin_state["od"][-2])
                P.op("vector", lambda e: e.reciprocal(out=dr0[:], in_=psum[6][:, :]))
                P.op("vector", lambda e: e.reciprocal(out=dr1[:], in_=psum[7][:, :]))
                P.op("vector", lambda e: e.tensor_tensor(out=dr0[:], in0=psum[4][:, :], in1=dr0[:], op=ALU.mult))
                v = P.op("vector", lambda e: e.tensor_tensor(out=dt1[:], in0=psum[5][:, :], in1=dr1[:], op=ALU.mult), s_fv, 1)
                fin_state["o_free"][0] = v
                v3 = P.op("vector", lambda e, stg=stg: e.scalar_tensor_tensor(out=stg[:], in0=dt1[:], scalar=neglam[:, 0:1], in1=dr0[:], op0=ALU.mult, op1=ALU.add), s_fv, 1)
                P.wait("sync", s_fv, v3)
                fin_state["od"].append(P.dma("sync", OTd[hd, :, j * 512:(j + 1) * 512], stg[:], s_od))

        def fin_pe():
            while pend_fin:
                u_, j, k, v2 = pend_fin.pop(0)
                o, rd, stg = osb[k % 2], rden[k % 2], ostm[k % 2]
                P.wait("tensor", s_fv, max(v2, fin_state["bc_free"]))
                vb = P.op("tensor", lambda e, rd=rd: e.matmul(psum[6][0:64, :], lhsT=ones_f[64:65, 0:64], rhs=rd[64:65, :], start=True, stop=True), s_bc, 1)
                P.wait("vector", s_bc, vb)
                s_od = s_ods[k % 2]
                if len(fin_state["od"]) >= 2:
                    P.wait("vector", s_od, fin_state["od"][-2])
                v3 = P.op("vector", lambda e, o=o, stg=stg: e.tensor_tensor(out=stg[:, :], in0=o[0:64, :], in1=psum[6][0:64, :], op=ALU.mult), s_fv, 1)
                fin_state["bc_free"] = v3
                P.wait("sync", s_fv, v3)
                fin_state["od"].append(P.dma("sync", OTm[u_, :, j * 512:(j + 1) * 512], stg[:, :], s_od))

        ns = len(steps)
        for s in range(min(L, ns)):
            emit_S(s)
            emit_exp(s)
        for s in range(ns):
            if s + L < ns:
                emit_S(s + L)
                emit_exp(s + L)
            emit_PV(s)
            if mla and pend_fin and (steps[s][1] == 4):
                fin_pe()
        if mla:
            fin_pe()
        nstep[0] += ns
        unit_end_pv.append(s_pv.n)
    for so in s_ods:
        P.wait("sync", so, so.n)
    P.end()
    if stage <= 2:
        top.close()
        return nc

    H2D = scratch("H2D", [NQ, D], BF16)
    aff = gsb("aff", [128, 32, NE], F32)
    iotg = gsb("iotg", [128, 128], F32)
    iopg = gsb("iopg", [128, 1], F32)

    class Tk:
        def __init__(self, tag):
            self.s = {en: P.sem(tag + en[:3]) for en in ENGS}

        def do(self, eng, fn, deps=(), dma=False):
            for d_ in deps:
                if d_ is not None and d_[0] != eng:
                    P.wait(eng, self.s[d_[0]], d_[1])
            v = P.op(eng, fn, self.s[eng], 1)
            return (eng, v)

    def dmado(eng, out_, in_, sem, deps, tk):
        for d_ in deps:
            if d_ is not None and d_[0] != eng:
                P.wait(eng, tk.s[d_[0]], d_[1])
        return P.dma(eng, out_, in_, sem)

    P.begin()
    tk = Tk("q")
    Wom = P.sb("Wom", [64, 8, D], BF16)
    Wod = P.sb("Wod", [128, 4, D], BF16)
    Wout = P.sb("Wout", [128, 8, D], BF16)
    wr = P.sb("wr", [128, 8, NE], F32)
    brt = P.sb("brt", [1, NE], F32)
    lnr = P.sb("lnr", [128, 4, D], F32)
    gsub08 = P.sb("gsub08", [128, 1], F32)
    om = P.sb("om", [64, 8, 512], BF16)
    od32 = P.sb("od32", [128, 4, 512], F32)
    odb = P.sb("odb", [128, 4, 512], BF16)
    gtb = P.sb("gtb", [128, 16, 512], BF16)
    yT = P.sb("yT", [128, 8, 512], BF16)
    xt = [P.sb("xt%d" % i, [128, D], F32) for i in range(2)]
    tA = P.sb("tA", [128, 512], F32)
    tB = P.sb("tB", [128, 512], F32)
    tS = P.sb("tS", [128, 512], F32)
    vvs = [P.sb("vv%d" % i, [128, D], F32) for i in range(2)]
    x1t = [P.sb("x1t%d" % i, [128, D], F32) for i in range(2)]
    h2fs = [P.sb("h2f%d" % i, [128, D], F32) for i in range(2)]
    h2b = [P.sb("h2b%d" % i, [128, D], BF16) for i in range(2)]
    h2Ts = [P.sb("h2T%d" % i, [128, 8, 128], F32) for i in range(2)]
    bsts = [P.sb("bst%d" % i, [128, 2, 6], F32) for i in range(2)]
    mvs = [P.sb("mv%d" % i, [128, 2], F32) for i in range(2)]
    sd1s = [P.sb("sd1%d" % i, [128, 1], F32) for i in range(2)]
    rs1s = [P.sb("rs1%d" % i, [128, 1], F32) for i in range(2)]
    lgs = [P.sb("lg%d" % i, [128, NE], F32) for i in range(2)]
    exs = [P.sb("ex%d" % i, [128, NE], F32) for i in range(2)]
    mxs = [P.sb("mx%d" % i, [128, 1], F32) for i in range(2)]
    ssums = [P.sb("ssum%d" % i, [128, 1], F32) for i in range(2)]
    s_w = P.sem("qw")
    s_lt = P.sem("qlt")
    s_lx = [P.sem("qlx0"), P.sem("qlx1")]
    s_o1 = [P.sem("qo10"), P.sem("qo11")]
    s_o2 = [P.sem("qo20"), P.sem("qo21")]
    P.dma("gpsimd", Wom[:], w_o_mla.rearrange("(h d) n -> d h n", d=64), s_w)
    P.dma("gpsimd", Wod[:], w_o_diff.rearrange("(h d) n -> d h n", d=128), s_w)
    P.dma("gpsimd", Wout[:], w_out.rearrange("(k p) n -> p k n", p=128), s_w)
    P.dma("gpsimd", wr[:], w_router.rearrange("(k p) n -> p k n", p=128), s_w)
    P.dma("gpsimd", brt[:], b_router, s_w)
    v_w = P.dma("gpsimd", lnr[:], lnrows, s_w)
    P.op("gpsimd", lambda e: e.iota(iotg[:], [[1, 128]], base=0, channel_multiplier=0, allow_small_or_imprecise_dtypes=True))
    P.op("gpsimd", lambda e: e.iota(iopg[:], [[0, 1]], base=0, channel_multiplier=1, allow_small_or_imprecise_dtypes=True))
    for en in ("tensor", "vector", "scalar"):
        P.wait(en, s_w, v_w)
    P.opf("vector", lambda e: e.tensor_scalar(out=gsub08[:], in0=gsub[:], scalar1=0.8, scalar2=None, op0=ALU.mult))
    ring = PsRing(P, psum, "q")
    last_tile_done = None
    xfree = {}
    x1_hist = {}
    h2_hist = {}
    h2f_free = {}
    lasts = {}
    for j in range(8):
        c0 = j * 512
        deps = [last_tile_done]
        for d_ in deps:
            if d_ is not None:
                P.wait("sync", tk.s[d_[0]], d_[1])
        P.dma("sync", om[:], OTm[:, :, c0:c0 + 512].rearrange("h d t -> d h t"), s_lt)
        P.dma("sync", od32[:], OTd[:, :, c0:c0 + 512].rearrange("h p t -> p h t"), s_lt)
        v_lt = P.dma("sync", gtb[:], GT[:, :, c0:c0 + 512].rearrange("c p t -> p c t"), s_lt)
        for en in ("tensor", "vector", "scalar"):
            P.wait(en, s_lt, v_lt)
        for hd in range(4):
            t1 = tk.do("scalar", lambda e, hd=hd: e.activation(out=tS[:], in_=od32[:, hd, :], func=AF.Square))
            it, bank = ring.acquire()
            P.wait("tensor", tk.s["scalar"], t1[1])
            ring.produced(lambda e, bank=bank: e.matmul(bank[:, :], lhsT=ones_f[:], rhs=tS[:], start=True, stop=True))
            ring.consume_wait("scalar", it)
            v = ring.release("scalar", [it], lambda e, bank=bank: e.activation(out=tA[:], in_=bank[:, :], func=AF.Sqrt, bias=eps_t[:, 0:1], scale=1.0 / 128.0))
            P.wait("vector", ring.free["scalar"], v)
            P.op("vector", lambda e: e.reciprocal(out=tB[:], in_=tA[:]))
            t2 = tk.do("vector", lambda e, hd=hd: e.scalar_tensor_tensor(out=odb[:, hd, :], in0=od32[:, hd, :], scalar=gsub08[:, 0:1], in1=tB[:], op0=ALU.mult, op1=ALU.mult))
            P.wait("scalar", tk.s["vector"], t2[1])
        P.wait("tensor", tk.s["vector"], t2[1])
        for c in range(8):
            itA, bA = ring.acquire()
            for h in range(8):
                fn = lambda e, h=h, c=c, bA=bA: e.matmul(bA[:, :], lhsT=Wom[0:64, h, c * 128:(c + 1) * 128], rhs=om[0:64, h, :], start=(h == 0), stop=(h == 7))
                if h == 7:
                    ring.produced(fn)
                else:
                    P.op("tensor", fn)
            itB, bB = ring.acquire()
            for hd in range(4):
                fn = lambda e, hd=hd, c=c, bB=bB: e.matmul(bB[:, :], lhsT=Wod[:, hd, c * 128:(c + 1) * 128], rhs=odb[:, hd, :], start=(hd == 0), stop=(hd == 3))
                if hd == 3:
                    ring.produced(fn)
                else:
                    P.op("tensor", fn)
            ring.consume_wait("vector", itB)
            P.op("vector", lambda e, c=c, bA=bA: e.tensor_tensor(out=tA[:], in0=bA[:, :], in1=gtb[:, c, :], op=ALU.mult))
            P.op("vector", lambda e, c=c, bB=bB: e.tensor_tensor(out=tB[:], in0=bB[:, :], in1=gtb[:, 8 + c, :], op=ALU.mult))
            vy = ring.release("vector", [itA, itB], lambda e, c=c: e.tensor_tensor(out=yT[:, c, :], in0=tA[:], in1=tB[:], op=ALU.add))
        P.wait("tensor", ring.free["vector"], vy)
        def subtile(t, s4, ltd):
            p_ = t % 2
            vv, h2f, h2T, bst, mv, sd1, rs1 = vvs[p_], h2fs[p_], h2Ts[p_], bsts[p_], mvs[p_], sd1s[p_], rs1s[p_]
            lg, ex, mx, ssum = lgs[p_], exs[p_], mxs[p_], ssums[p_]
            xb = xt[t % 2]
            r0 = t * 128
            if t >= 2:
                P.wait("gpsimd", tk.s["vector"], xfree[t - 2])
            v_x = P.dma("gpsimd", xb[:], xown[r0:r0 + 128, :], s_lx[t % 2])
            its = []
            bks = []
            for nh in range(2):
                it, bank = ring.acquire()
                for c in range(8):
                    fn = lambda e, c=c, nh=nh, s4=s4, bank=bank: e.matmul(bank[:, :], lhsT=yT[:, c, s4 * 128:(s4 + 1) * 128], rhs=Wout[:, c, nh * 512:(nh + 1) * 512], start=(c == 0), stop=(c == 7))
                    if c == 7:
                        ring.produced(fn)
                    else:
                        P.op("tensor", fn)
                its.append(it)
                bks.append(bank)
            yield
            ring.consume_wait("vector", its[1])
            P.op("vector", lambda e, b0=bks[0]: e.tensor_tensor(out=vv[:, 0:512], in0=b0[:, :], in1=modrow[:, 0:512], op=ALU.mult))
            ring.release("vector", its, lambda e, b1=bks[1]: e.tensor_tensor(out=vv[:, 512:1024], in0=b1[:, :], in1=modrow[:, 512:1024], op=ALU.mult))
            P.wait("vector", s_lx[t % 2], v_x)
            xfree[t] = tk.do("vector", lambda e, xb=xb: e.scalar_tensor_tensor(out=vv[:], in0=xb[:], scalar=ALPHA, in1=vv[:], op0=ALU.mult, op1=ALU.add))[1]
            P.op("vector", lambda e: e.bn_stats(out=bst[:, 0, :], in_=vv[:, 0:512]))
            P.opf("vector", lambda e: e.bn_stats(out=bst[:, 1, :], in_=vv[:, 512:1024]))
            P.opf("vector", lambda e: e.tensor_copy(out=tA[:], in_=vv[:, 0:512]))
            t3 = tk.do("vector", lambda e: e.bn_aggr(out=mv[:], in_=bst[:].rearrange("p a b -> p (a b)")))
            t4 = tk.do("scalar", lambda e: e.activation(out=sd1[:], in_=mv[:, 1:2], func=AF.Sqrt, bias=eps_t[:, 0:1], scale=1.0), [t3])
            yield
            P.wait("vector", tk.s["scalar"], t4[1])
            P.opf("vector", lambda e: e.reciprocal(out=rs1[:], in_=sd1[:]))
            x1b = x1t[t % 2]
            if t >= 2:
                P.wait("vector", s_o1[t % 2], x1_hist[t - 2])
            P.op("vector", lambda e, x1b=x1b: e.tensor_scalar(out=x1b[:], in0=vv[:], scalar1=mv[:, 0:1], scalar2=rs1[:, 0:1], op0=ALU.subtract, op1=ALU.mult))
            t5 = tk.do("vector", lambda e, x1b=x1b: e.tensor_tensor(out=x1b[:], in0=x1b[:], in1=lnr[:, 0, :], op=ALU.mult))
            t6 = tk.do("gpsimd", lambda e, x1b=x1b: e.tensor_tensor(out=x1b[:], in0=x1b[:], in1=lnr[:, 1, :], op=ALU.add), [t5, ltd])
            P.wait("sync", tk.s["gpsimd"], t6[1])
            x1_hist[t] = P.dma("sync", X1[r0:r0 + 128, :], x1b[:], s_o1[t % 2])
            if t >= 2:
                P.wait("gpsimd", tk.s["scalar"], h2f_free[t - 2])
            P.op("gpsimd", lambda e, x1b=x1b: e.tensor_tensor(out=h2f[:], in0=x1b[:], in1=modrow[:, 2048:3072], op=ALU.mult))
            t7 = tk.do("gpsimd", lambda e: e.tensor_tensor(out=h2f[:], in0=h2f[:], in1=modrow[:, 1024:2048], op=ALU.add))
            hb = h2b[t % 2]
            if t >= 2:
                P.wait("scalar", s_o2[t % 2], h2_hist[t - 2])
            t8 = tk.do("scalar", lambda e, hb=hb: e.activation(out=hb[:], in_=h2f[:], func=AF.Copy), [t7])
            P.wait("sync", tk.s["scalar"], t8[1])
            h2_hist[t] = P.dma("sync", H2D[r0:r0 + 128, :], hb[:], s_o2[t % 2])
            yield
            P.wait("tensor", tk.s["gpsimd"], t7[1])
            tits = []
            tbk = []
            for hh in range(2):
                it, bank = ring.acquire()
                for q in range(4):
                    c = hh * 4 + q
                    fn = lambda e, c=c, q=q, bank=bank: e.transpose(out=bank[:, q * 128:(q + 1) * 128], in_=h2f[:, c * 128:(c + 1) * 128], identity=ident_f[:])
                    if q == 3:
                        ring.produced(fn)
                    else:
                        P.op("tensor", fn)
                tits.append(it)
                tbk.append(bank)
            for hh in range(2):
                ring.consume_wait("scalar", tits[hh])
                vh = ring.release("scalar", [tits[hh]], lambda e, hh=hh, bank=tbk[hh]: e.activation(out=h2T[:, hh * 4:(hh + 1) * 4, :], in_=bank[:, :].rearrange("p (q n) -> p q n", q=4), func=AF.Copy))
            tk.do("scalar", lambda e: e.nop())
            h2f_free[t] = tk.s["scalar"].n
            yield
            P.wait("tensor", ring.free["scalar"], vh)
            it, bank = ring.acquire()
            for c in range(8):
                P.op("tensor", lambda e, c=c, bank=bank: e.matmul(bank[:, 0:NE], lhsT=h2T[:, c, :], rhs=wr[:, c, :], start=(c == 0), stop=False))
            ring.produced(lambda e, bank=bank: e.matmul(bank[:, 0:NE], lhsT=ones_f[0:1, :], rhs=brt[0:1, :], start=False, stop=True))
            ring.consume_wait("vector", it)
            P.opf("vector", lambda e, bank=bank: e.tensor_reduce(out=mx[:], in_=bank[:, 0:NE], axis=AX.X, op=ALU.max, negate=True))
            vl = ring.release("vector", [it], lambda e, bank=bank: e.tensor_copy(out=lg[:], in_=bank[:, 0:NE]))
            yield
            P.wait("scalar", ring.free["vector"], vl)
            t9 = tk.do("scalar", lambda e: e.activation(out=ex[:], in_=lg[:], func=AF.Exp, bias=mx[:, 0:1], scale=1.0, accum_out=ssum[:, 0:1]))
            P.wait("vector", tk.s["scalar"], t9[1])
            P.opf("vector", lambda e: e.reciprocal(out=ssum[:], in_=ssum[:]))
            last_ = tk.do("vector", lambda e, t=t: e.tensor_scalar(out=aff[:, t, :], in0=ex[:], scalar1=ssum[:, 0:1], scalar2=None, op0=ALU.mult))
            P.wait("scalar", tk.s["vector"], last_[1])
            lasts[t] = last_

        for pair in ((0, 1), (2, 3)):
            gens = [subtile(j * 4 + a_, a_, last_tile_done) for a_ in pair]
            alive = True
            while alive:
                alive = False
                for g_ in gens:
                    try:
                        next(g_)
                        alive = True
                    except StopIteration:
                        pass
        last = lasts[j * 4 + 3]
        last_tile_done = last
    for so in s_o1 + s_o2:
        P.wait("sync", so, so.n)
    P.end()
    if stage <= 3:
        top.close()
        return nc

    slotidx = gsb("slotidx", [128, NE, 32], I32)
    gmask = gsb("gmask", [128, NE, 32], F32)
    P.begin()
    tk = Tk("r")
    affall = P.sb("affall", [128, 64, NE], F32)
    affT = P.sb("affT", [128, NE, 64], F32)
    affTo = P.sb("affTo", [128, NE, 32], F32)
    junk = P.sb("junk", [128, 64], F32)
    cnt = P.sb("cnt", [128, NE], F32)
    lo = P.sb("lo", [128, NE], F32)
    mid = P.sb("mid", [128, NE], F32)
    ge = P.sb("ge", [128, NE], F32)
    maskT = P.sb("maskT", [128, NE, 32], F32)
    maskb = P.sb("maskb", [128, NE, 32], BF16)
    Sx = P.sb("Sx", [128, NE, 32], F32)
    Sb = P.sb("Sb", [128, NE, 32], BF16)
    Ub = P.sb("Ub", [128, 128], BF16)
    posf = P.sb("posf", [128, NE, 32], F32)
    s_d = P.sem("rd")
    s_cc = P.sem("rcc")
    v = P.dma("gpsimd", AFF_IN.ap(), aff[:].rearrange("p t e -> p (t e)"), s_d)
    P.wait("gpsimd", s_d, v)
    P.op("gpsimd", lambda e: e.collective_compute("AllGather", ALU.bypass, replica_groups=[[0, 1], [2, 3], [4, 5], [6, 7]], ins=[AFF_IN.ap().opt()], outs=[AFF_OUT.ap().opt()]), s_cc, 1)
    P.wait("gpsimd", s_cc, 1)
    P.dma("gpsimd", affall[:, 0:32, :], AFF_OUT.ap()[0:128, :].rearrange("p (t e) -> p t e", e=NE), s_d)
    v = P.dma("gpsimd", affall[:, 32:64, :], AFF_OUT.ap()[128:256, :].rearrange("p (t e) -> p t e", e=NE), s_d)
    P.wait("vector", s_d, v)
    P.op("vector", lambda e: e.tensor_copy(out=affT[:], in_=affall[:].rearrange("p t e -> p e t")))
    P.op("vector", lambda e: e.tensor_copy(out=affTo[:], in_=aff[:].rearrange("p t e -> p e t")))
    P.opf("vector", lambda e: e.memset(lo[:], 0.0))
    P.op("vector", lambda e: e.tensor_scalar(out=Ub[:], in0=iotg[:], scalar1=iopg[:, 0:1], scalar2=None, op0=ALU.is_gt))
    for itn in range(NITER):
        step = 2.0 ** -(itn + 1)
        P.opf("vector", lambda e, step=step: e.tensor_scalar(out=mid[:], in0=lo[:], scalar1=step, scalar2=None, op0=ALU.add))
        for ex_ in range(NE):
            fn = lambda e, ex_=ex_: e.tensor_scalar(out=junk[:], in0=affT[:, ex_, :], scalar1=mid[:, ex_:ex_ + 1], scalar2=0.0, op0=ALU.is_gt, op1=ALU.add, accum_out=cnt[:, ex_:ex_ + 1])
            if ex_ == NE - 1:
                tc_ = tk.do("vector", fn)
            else:
                P.op("vector", fn)
        bank = psum[itn % 2]
        tp = tk.do("tensor", lambda e, bank=bank: e.matmul(bank[:, 0:NE], lhsT=ones_f[:], rhs=cnt[:], start=True, stop=True), [tc_])
        P.wait("vector", tk.s["tensor"], tp[1])
        P.opf("vector", lambda e, bank=bank: e.tensor_scalar(out=ge[:], in0=bank[:, 0:NE], scalar1=float(CPAD) - 0.5, scalar2=None, op0=ALU.is_ge))
        P.opf("vector", lambda e, step=step: e.scalar_tensor_tensor(out=lo[:], in0=ge[:], scalar=step, in1=lo[:], op0=ALU.mult, op1=ALU.add))
    for ex_ in range(NE):
        P.op("vector", lambda e, ex_=ex_: e.tensor_scalar(out=maskT[:, ex_, :], in0=affTo[:, ex_, :], scalar1=lo[:, ex_:ex_ + 1], scalar2=None, op0=ALU.is_gt))
    P.op("vector", lambda e: e.tensor_tensor(out=gmask[:], in0=maskT[:], in1=affTo[:], op=ALU.mult))
    P.op("vector", lambda e: e.tensor_copy(out=maskb[:], in_=maskT[:]))
    P.opf("vector", lambda e: e.memset(Sx[:], 0.0))
    for t in range(1, 32):
        P.opf("vector", lambda e, t=t: e.tensor_tensor(out=Sx[:, :, t], in0=Sx[:, :, t - 1], in1=maskT[:, :, t - 1], op=ALU.add))
    tsb = tk.do("vector", lambda e: e.tensor_copy(out=Sb[:], in_=Sx[:]))
    P.wait("tensor", tk.s["vector"], tsb[1])
    P.op("tensor", lambda e: e.matmul(psum[2][:, :], lhsT=ones_b[:], rhs=Sb[:].rearrange("p e t -> p (e t)"), start=True, stop=False))
    tpp = tk.do("tensor", lambda e: e.matmul(psum[2][:, :], lhsT=Ub[:], rhs=maskb[:].rearrange("p e t -> p (e t)"), start=False, stop=True))
    P.wait("vector", tk.s["tensor"], tpp[1])
    P.op("vector", lambda e: e.scalar_tensor_tensor(out=posf[:].rearrange("p e t -> p (e t)"), in0=maskT[:].rearrange("p e t -> p (e t)"), scalar=-1048576.0, in1=psum[2][:, :], op0=ALU.mult, op1=ALU.add))
    P.op("vector", lambda e: e.tensor_scalar(out=posf[:], in0=posf[:], scalar1=1048576.0, scalar2=None, op0=ALU.add))
    tdb = tk.do("vector", lambda e: e.tensor_copy(out=slotidx[:], in_=posf[:]))
    if "DBGT" in dbg:
        P.wait("sync", tk.s["vector"], tdb[1])
        P.dma("sync", DBGT[:, 0:512], aff[:].rearrange("p t e -> p (t e)"), s_d)
        P.dma("sync", DBGT[:, 512:1024], posf[:].rearrange("p e t -> p (e t)"), s_d)
        P.dma("sync", DBGT[:, 1024:1536], gmask[:].rearrange("p e t -> p (e t)"), s_d)
        vdb = P.dma("sync", DBGT[:, 1536:1552], lo[:], s_d)
        P.wait("sync", s_d, vdb)
    P.end()
    if stage <= 4:
        top.close()
        return nc

    P.begin()
    tk = Tk("m")
    w1b = [P.sb("w1b%d" % i, [128, 8, D], BF16) for i in range(2)]
    w3b = [P.sb("w3b%d" % i, [128, 8, D], BF16) for i in range(2)]
    w2b = [P.sb("w2b%d" % i, [128, 8, D], BF16) for i in range(2)]
    h2t = [P.sb("h2t%d" % i, [128, D], BF16) for i in range(4)]
    xtok = [P.sb("xtok%d" % i, [128, 4, D], BF16) for i in range(2)]
    xeT = [P.sb("xeT%d" % i, [128, 8, 512], BF16) for i in range(2)]
    hid = P.sb("hid", [128, 8, 512], BF16)
    sgt = [P.sb("sgt%d" % i, [128, 512], F32) for i in range(2)]
    yst = [P.sb("yst%d" % i, [128, D], F32) for i in range(2)]
    s_wl = [P.sem("mw0"), P.sem("mw1")]
    s_hl = [P.sem("mh%d" % i) for i in range(4)]
    s_sc = [P.sem("msc%d" % i) for i in range(4)]
    s_scall = P.sem("mscall")
    s_xl = [P.sem("mx0"), P.sem("mx1")]
    s_yo = [P.sem("my0"), P.sem("my1")]
    ring = PsRing(P, psum, "m")
    psb = [psall[:, i, :].bitcast(BF16) for i in range(8)]
    wl_val = {}
    sc_done = {}
    exp_done = {}
    hcount = [0]
    h_hist = []
    sc_hist = []

    def load_w(e_):
        b = e_ % 2
        if e_ >= 2:
            a_, v_ = exp_done[e_ - 2]
            P.wait("gpsimd", ring.free["scalar"], a_)
            P.wait("gpsimd", ring.free["vector"], v_)
        P.dma("gpsimd", w1b[b][:], moe_w1[e_].rearrange("(k p) n -> p k n", p=128), s_wl[b])
        P.dma("gpsimd", w3b[b][:], moe_w3[e_].rearrange("(k p) n -> p k n", p=128), s_wl[b])
        wl_val[e_] = P.dma("gpsimd", w2b[b][:], moe_w2[e_].rearrange("(k p) n -> p k n", p=128), s_wl[b])

    def dispatch(e_):
        base = hcount[0]

        def load(t):
            i = base + t
            if i >= 4:
                P.wait("gpsimd", s_sc[i % 4], sc_hist[i - 4])
            return P.dma("gpsimd", h2t[i % 4][:], H2D[t * 128:(t + 1) * 128, :], s_hl[i % 4])

        lv = {}
        lv[0] = load(0)
        lv[1] = load(1)
        for t in range(32):
            i = base + t
            hb = h2t[i % 4]
            P.wait("gpsimd", s_hl[i % 4], lv[t])
            P.op("gpsimd", lambda e, e_=e_, t=t, hb=hb: e.indirect_dma_start(out=XE[e_], out_offset=bass.IndirectOffsetOnAxis(ap=slotidx[:, e_, t:t + 1], axis=0), in_=hb[:, :], in_offset=None, bounds_check=breg["r"], oob_is_err=False), s_sc[i % 4], 16)
            sc_hist.append(s_sc[i % 4].n)
            if t + 2 < 32:
                lv[t + 2] = load(t + 2)
        hcount[0] += 32
        sc_done[e_] = [s_sc[k].n for k in range(4)]

    alt2 = [0]

    def nxt():
        alt2[0] += 1
        return "scalar" if alt2[0] % 2 else "vector"

    xcount = [0]
    x_hist = []
    ycount = [0]
    y_hist = []
    breg = {}

    def _mk_reg(e):
        breg["r"] = e.alloc_register("bchk")
        e.reg_mov(breg["r"], CPAD - 1)

    P.op("gpsimd", _mk_reg)
    NST_ = CPAD // 512
    vxs = {}

    def issue_xload(T):
        ee, st_ = T // NST_, T % NST_
        xb_ = xtok[T % 2]
        if st_ == 0:
            for k in range(4):
                P.wait("sync", s_sc[k], sc_done[ee][k])
        if T >= 2:
            a_, v_ = x_hist[T - 2]
            P.wait("sync", ring.free["scalar"], a_)
            P.wait("sync", ring.free["vector"], v_)
        vxs[T] = P.dma("sync", xb_[:], XE[ee][st_ * 512:(st_ + 1) * 512, :].rearrange("(s p) d -> p s d", p=128), s_xl[T % 2])

    load_w(0)
    dispatch(0)
    dispatch(1)
    issue_xload(0)
    for e_ in range(NE):
        b = e_ % 2
        if e_ + 1 < NE:
            load_w(e_ + 1)
        if e_ + 2 < NE:
            dispatch(e_ + 2)
        P.wait("tensor", s_wl[b], wl_val[e_])
        for st in range(NST_):
            xi = xcount[0]
            xcount[0] += 1
            xb = xtok[xi % 2]
            P.wait("tensor", s_xl[xi % 2], vxs[xi])
            xT_ = xeT[xi % 2]
            for c in range(8):
                it, bank = ring.acquire()
                pb = psb[(it) % 8]
                for s4 in range(4):
                    fn = lambda e, c=c, s4=s4, pb=pb, xb=xb: e.transpose(out=pb[:, s4 * 128:(s4 + 1) * 128], in_=xb[:, s4, c * 128:(c + 1) * 128], identity=ident_b[:])
                    if s4 == 3:
                        ring.produced(fn)
                    else:
                        P.op("tensor", fn)
                eng = nxt()
                ring.consume_wait(eng, it)
                if eng == "scalar":
                    vt = ring.release(eng, [it], lambda e, c=c, pb=pb, xT_=xT_: e.activation(out=xT_[:, c, :], in_=pb[:, 0:512], func=AF.Copy))
                else:
                    vt = ring.release(eng, [it], lambda e, c=c, pb=pb, xT_=xT_: e.tensor_copy(out=xT_[:, c, :], in_=pb[:, 0:512]))
            x_hist.append((ring.free["scalar"].n, ring.free["vector"].n))
            if xi + 1 < NE * NST_:
                issue_xload(xi + 1)
            P.wait("tensor", ring.free["scalar"], ring.free["scalar"].n)
            P.wait("tensor", ring.free["vector"], ring.free["vector"].n)
            for f in range(8):
                itA, bA = ring.acquire()
                for c in range(8):
                    fn = lambda e, c=c, f=f, bA=bA, b=b, xT_=xT_: e.matmul(bA[:, :], lhsT=w1b[b][:, c, f * 128:(f + 1) * 128], rhs=xT_[:, c, :], start=(c == 0), stop=(c == 7))
                    if c == 7:
                        ring.produced(fn)
                    else:
                        P.op("tensor", fn)
                itB, bB = ring.acquire()
                for c in range(8):
                    fn = lambda e, c=c, f=f, bB=bB, b=b, xT_=xT_: e.matmul(bB[:, :], lhsT=w3b[b][:, c, f * 128:(f + 1) * 128], rhs=xT_[:, c, :], start=(c == 0), stop=(c == 7))
                    if c == 7:
                        ring.produced(fn)
                    else:
                        P.op("tensor", fn)
                sg = sgt[f % 2]
                ring.consume_wait("scalar", itA)
                if f >= 2:
                    P.wait("scalar", ring.free["vector"], sg_free[f % 2])
                else:
                    if f == 0:
                        sg_free = {}
                    if (e_, st) != (0, 0):
                        P.wait("scalar", ring.free["vector"], prev_sg_free[f % 2])
                va = ring.release("scalar", [itA], lambda e, bA=bA, sg=sg: e.activation(out=sg[:], in_=bA[:, :], func=AF.Silu))
                ring.consume_wait("vector", itB)
                P.wait("vector", ring.free["scalar"], va)
                sg_free[f % 2] = ring.release("vector", [itB], lambda e, bB=bB, sg=sg, f=f: e.tensor_tensor(out=hid[:, f, :], in0=sg[:], in1=bB[:, :], op=ALU.mult))
            prev_sg_free = dict(sg_free)
            P.wait("tensor", ring.free["vector"], ring.free["vector"].n)
            for s4 in range(4):
                yi = ycount[0]
                ycount[0] += 1
                yb = yst[yi % 2]
                for nh in range(2):
                    it, bank = ring.acquire()
                    for f in range(8):
                        fn = lambda e, f=f, s4=s4, nh=nh, bank=bank, b=b: e.matmul(bank[:, :], lhsT=hid[:, f, s4 * 128:(s4 + 1) * 128], rhs=w2b[b][:, f, nh * 512:(nh + 1) * 512], start=(f == 0), stop=(f == 7))
                        if f == 7:
                            ring.produced(fn)
                        else:
                            P.op("tensor", fn)
                    eng = "scalar" if nh == 0 else "vector"
                    ring.consume_wait(eng, it)
                    if nh == 0 and yi >= 2:
                        P.wait("scalar", s_yo[yi % 2], y_hist[yi - 2])
                    if nh == 1 and yi >= 2:
                        P.wait("vector", s_yo[yi % 2], y_hist[yi - 2])
                    if eng == "scalar":
                        vy0 = ring.release(eng, [it], lambda e, bank=bank, yb=yb: e.activation(out=yb[:, 0:512], in_=bank[:, :], func=AF.Copy))
                    else:
                        vy1 = ring.release(eng, [it], lambda e, bank=bank, yb=yb: e.tensor_copy(out=yb[:, 512:1024], in_=bank[:, :]))
                P.wait("sync", ring.free["scalar"], vy0)
                P.wait("sync", ring.free["vector"], vy1)
                r0 = st * 512 + s4 * 128
                y_hist.append(P.dma("sync", YE[e_][r0:r0 + 128, :], yb[:], s_yo[yi % 2]))
        exp_done[e_] = (ring.free["scalar"].n, ring.free["vector"].n)
    for so in s_yo:
        P.wait("sync", so, so.n)
    P.end()
    if stage <= 5:
        top.close()
        return nc

    P.begin()
    tk = Tk("z")
    NG = 6
    gb = [P.sb("gb%d" % i, [128, D], F32) for i in range(NG)]
    acc = [P.sb("acc%d" % i, [128, D], F32) for i in range(2)]
    x1l = [P.sb("x1l%d" % i, [128, D], F32) for i in range(2)]
    lnr2 = P.sb("lnr2", [128, 2, D], F32)
    bst2 = P.sb("bst2", [128, 2, 6], F32)
    spc = P.sb("spc", [128, 512], F32)
    junk2 = P.sb("junk2", [128, D], F32)
    s12 = P.sb("s12", [128, 2], F32)
    msq = P.sb("msq", [128, 1], F32)
    mv2 = P.sb("mv2", [128, 2], F32)
    sd2 = P.sb("sd2", [128, 1], F32)
    rs2 = P.sb("rs2", [128, 1], F32)
    s_g = [P.sem("zg%d" % i) for i in range(NG)]
    s_x1 = [P.sem("zx0"), P.sem("zx1")]
    s_oo = [P.sem("zo0"), P.sem("zo1")]
    s_l = P.sem("zl")
    v_l = P.dma("sync", lnr2[:], lnrows[:, 2:4, :], s_l)
    breg2 = {}

    def _mk_reg2(e):
        breg2["r"] = e.alloc_register("bchk2")
        e.reg_mov(breg2["r"], CPAD - 1)

    P.op("gpsimd", _mk_reg2)
    for i in range(NG):
        P.op("gpsimd", lambda e, i=i: e.memset(gb[i][:], 0.0))
    P.wait("vector", s_l, v_l)
    gcount = 0
    g_hist = []
    gfree = []
    o_hist = []
    accfree = {}
    NGA = 32 * NE
    vgs = {}
    vx1s = {}

    def issue(i):
        t, e_ = (i // NE, i % NE) if i < NGA else (0, 0)
        g = gb[i % NG]
        if i >= NG:
            P.wait("gpsimd", tk.s["vector"], gfree[i - NG])
        vgs[i] = P.op("gpsimd", lambda e, e_=e_, t=t, g=g: e.indirect_dma_start(out=g[:, :], out_offset=None, in_=YE[e_], in_offset=bass.IndirectOffsetOnAxis(ap=slotidx[:, e_, t:t + 1], axis=0), bounds_check=breg2["r"], oob_is_err=False), s_g[i % NG], 16)

    def consume(i):
        t, e_ = i // NE, i % NE
        a = acc[t % 2]
        g = gb[i % NG]
        if e_ == 0:
            xl = x1l[t % 2]
            if t >= 2:
                P.wait("sync", tk.s["vector"], accfree[t - 2])
            vx1s[t] = P.dma("sync", xl[:], X1[t * 128:(t + 1) * 128, :], s_x1[t % 2])
        for k in (i, i + 1, i + 2):
            P.wait("vector", s_g[k % NG], vgs[k])
        if e_ == 0:
            if t >= 2:
                P.wait("vector", s_oo[t % 2], o_hist[t - 2])
            tg = tk.do("vector", lambda e, g=g, a=a, e_=e_, t=t: e.tensor_scalar(out=a[:], in0=g[:], scalar1=gmask[:, e_, t:t + 1], scalar2=None, op0=ALU.mult))
        else:
            tg = tk.do("vector", lambda e, g=g, a=a, e_=e_, t=t: e.scalar_tensor_tensor(out=a[:], in0=g[:], scalar=gmask[:, e_, t:t + 1], in1=a[:], op0=ALU.mult, op1=ALU.add))
        gfree.append(tg[1])

    def finish_tile(t):
        a = acc[t % 2]
        xl = x1l[t % 2]
        vx1 = vx1s[t]
        P.wait("vector", s_x1[t % 2], vx1)
        P.op("vector", lambda e, a=a: e.tensor_tensor(out=a[:], in0=a[:], in1=modrow[:, 3072:4096], op=ALU.mult))
        P.op("vector", lambda e, a=a, xl=xl: e.scalar_tensor_tensor(out=a[:], in0=xl[:], scalar=ALPHA, in1=a[:], op0=ALU.mult, op1=ALU.add))
        tv = tk.do("vector", lambda e, a=a: e.tensor_copy(out=spc[:], in_=a[:, 0:512]))
        P.wait("scalar", tk.s["vector"], tv[1])
        P.op("scalar", lambda e, a=a: e.activation(out=junk2[:], in_=a[:], func=AF.Copy, accum_out=s12[:, 0:1]))
        ts = tk.do("scalar", lambda e, a=a: e.activation(out=junk2[:], in_=a[:], func=AF.Square, accum_out=s12[:, 1:2]))
        P.wait("vector", tk.s["scalar"], ts[1])
        P.opf("vector", lambda e: e.tensor_scalar(out=mv2[:, 0:1], in0=s12[:, 0:1], scalar1=1.0 / D, scalar2=None, op0=ALU.mult))
        P.opf("vector", lambda e: e.tensor_tensor(out=msq[:], in0=mv2[:, 0:1], in1=mv2[:, 0:1], op=ALU.mult))
        t3 = tk.do("vector", lambda e: e.scalar_tensor_tensor(out=mv2[:, 1:2], in0=s12[:, 1:2], scalar=1.0 / D, in1=msq[:], op0=ALU.mult, op1=ALU.subtract))
        t4 = tk.do("scalar", lambda e: e.activation(out=sd2[:], in_=mv2[:, 1:2], func=AF.Sqrt, bias=eps_t[:, 0:1], scale=1.0), [t3])
        P.wait("vector", tk.s["scalar"], t4[1])
        P.opf("vector", lambda e: e.reciprocal(out=rs2[:], in_=sd2[:]))
        P.op("vector", lambda e, a=a: e.tensor_scalar(out=a[:], in0=a[:], scalar1=mv2[:, 0:1], scalar2=rs2[:, 0:1], op0=ALU.subtract, op1=ALU.mult))
        P.op("vector", lambda e, a=a: e.tensor_tensor(out=a[:], in0=a[:], in1=lnr2[:, 0, :], op=ALU.mult))
        t5 = tk.do("vector", lambda e, a=a: e.tensor_tensor(out=a[:], in0=a[:], in1=lnr2[:, 1, :], op=ALU.add))
        accfree[t] = t5[1]
        P.wait("sync", tk.s["vector"], t5[1])
        o_hist.append(P.dma("sync", out[t * 128:(t + 1) * 128, :], a[:], s_oo[t % 2]))
    for i in range(NGA + 2):
        issue(i)
        if i >= 2:
            consume(i - 2)
            if (i - 2) % NE == NE - 1:
                finish_tile((i - 2) // NE)
    for so in s_oo:
        P.wait("sync", so, so.n)
    P.end()
    top.close()
    return nc


def _rope_tables(tok_idx):
    row = (tok_idx // 64).astype(np.float32)
    col = (tok_idx % 64).astype(np.float32)

    def tab(h):
        half = h // 2
        inv = (10000.0 ** (-(np.arange(half, dtype=np.float32) * 2.0 / h))).astype(np.float32)
        ar = (row[None, :] * inv[:, None]).astype(np.float32)
        ac = (col[None, :] * inv[:, None]).astype(np.float32)
        c = np.concatenate([np.cos(ar), np.cos(ar), np.cos(ac), np.cos(ac)], 0)
        s = np.concatenate([-np.sin(ar), np.sin(ar), -np.sin(ac), np.sin(ac)], 0)
        return c.astype(np.float32), s.astype(np.float32)

    c64, s64 = tab(32)
    c32, s32 = tab(16)
    return (np.tile(c64, (2, 1)), np.tile(s64, (2, 1)), np.tile(c32, (4, 1)), np.tile(s32, (4, 1)))


def _perm(d):
    q = d // 4
    return np.concatenate([np.arange(q, 2 * q), np.arange(0, q), np.arange(3 * q, 4 * q), np.arange(2 * q, 3 * q)])


def _prep(inp):
    f = lambda a: np.ascontiguousarray(np.asarray(a, dtype=np.float32))
    x, c, ctx, c_ctx = f(inp["x"]), f(inp["c"]), f(inp["ctx"]), f(inp["c_ctx"])
    w_in = f(inp["w_in"])[0]
    p64 = _perm(64)
    p32 = _perm(32)
    w_all = np.zeros((D, NW), np.float32)
    w_all[:, O_CQ:O_CQ + 256] = w_in[:, 0:256]
    w_all[:, O_CKV:O_CKV + 128] = w_in[:, 256:384]
    w_all[:, O_KR:O_KR + 32] = w_in[:, 384:416]
    w_all[:, O_KRP:O_KRP + 32] = w_in[:, 384:416][:, p32]
    dq = w_in[:, 416:928]
    dk = w_in[:, 928:1440]
    pall = np.concatenate([g * 64 + p64 for g in range(8)])
    w_all[:, O_DQ:O_DQ + 512] = dq
    w_all[:, O_DQP:O_DQP + 512] = dq[:, pall]
    w_all[:, O_DK:O_DK + 512] = dk
    w_all[:, O_DKP:O_DKP + 512] = dk[:, pall]
    w_all[:, O_DV:O_DV + 512] = w_in[:, 1440:1952]
    w_all[:, O_G:O_G + 2048] = w_in[:, 1952:4000]
    wuq = f(inp["mla_w_uq"])[0].reshape(256, 8, 96)
    w_uq_all = np.zeros((256, 1024), np.float32)
    w_uq_all[:, 0:512] = wuq[:, :, 0:64].reshape(256, 512)
    w_uq_all[:, 512:768] = wuq[:, :, 64:96].reshape(256, 256)
    w_uq_all[:, 768:1024] = wuq[:, :, 64:96][:, :, p32].reshape(256, 256)
    wukv = f(inp["mla_w_ukv"])[0].reshape(128, 8, 128)
    w_ukv_all = np.concatenate([wukv[:, :, 0:64].reshape(128, 512), wukv[:, :, 64:128].reshape(128, 512)], 1)
    b_ada = f(inp["b_ada"])[0]
    common = {
        "w_ada": f(inp["w_ada"])[0],
        "b_ada_fm": np.ascontiguousarray(b_ada[0:2048].reshape(16, 128).T),
        "b_ada_row": b_ada.reshape(1, 6 * D),
        "w_all": w_all,
        "w_uq_all": w_uq_all,
        "w_ukv_all": np.ascontiguousarray(w_ukv_all),
        "g_q_fm": np.ascontiguousarray(f(inp["mla_g_q"])[0].reshape(2, 128).T),
        "g_kv_fm": f(inp["mla_g_kv"])[0].reshape(128, 1),
        "w_o_mla": f(inp["mla_w_o"])[0],
        "w_o_diff": f(inp["diff_w_o"])[0],
        "w_out": f(inp["w_out"])[0],
        "dlam": np.ascontiguousarray(np.broadcast_to(f(inp["diff_lambda"])[0].reshape(1, 256), (128, 256))),
        "g_sub_fm": f(inp["diff_g_subln"])[0].reshape(128, 1),
        "lnrows": np.ascontiguousarray(np.broadcast_to(np.stack([f(inp["ln1_g"])[0], f(inp["ln1_b"])[0], f(inp["ln2_g"])[0], f(inp["ln2_b"])[0]], 0)[None], (128, 4, D))),
        "w_router": f(inp["moe_w_router"])[0],
        "b_router": f(inp["moe_b_router"])[0].reshape(1, NE),
        "moe_w1": f(inp["moe_w1"])[0],
        "moe_w3": f(inp["moe_w3"])[0],
        "moe_w2": f(inp["moe_w2"])[0],
    }
    maps = []
    for core in range(8):
        b, hf = core // 2, core % 2
        own = np.arange(hf * NQ, (hf + 1) * NQ)
        oth = np.arange((1 - hf) * NQ, (2 - hf) * NQ)
        order = np.concatenate([own, oth])
        c64, s64, c32, s32 = _rope_tables(order)
        m = dict(common)
        m["xT"] = np.ascontiguousarray(x[b][order].T)
        m["xown"] = np.ascontiguousarray(x[b][own])
        m["ctxT"] = np.ascontiguousarray(ctx[b].T)
        cv = np.stack([c[b].reshape(8, 128).T, c_ctx.reshape(8, 128).T], -1)
        m["cvec"] = np.ascontiguousarray(cv)
        m["rope64_c"], m["rope64_s"], m["rope32_c"], m["rope32_s"] = c64, s64, c32, s32
        maps.append(m)
    return maps


_NC_CACHE = {}


def kernel(**inputs):
    maps = _prep(inputs)
    if "nc" not in _NC_CACHE:
        _NC_CACHE["nc"] = build()
    res = run_bass_kernel_spmd(_NC_CACHE["nc"], maps, core_ids=list(range(8)))
    outp = np.zeros((4, SEQ, D), np.float32)
    for core in range(8):
        b, hf = core // 2, core % 2
        outp[b, hf * NQ:(hf + 1) * NQ] = res.results[core]["out"]
    return outp
```

```python
import contextlib
import numpy as np
import concourse.bass as bass
import concourse.mybir as mybir
from concourse.bass_utils import run_bass_kernel_spmd

F32 = mybir.dt.float32
BF16 = mybir.dt.bfloat16
U32 = mybir.dt.uint32
I32 = mybir.dt.int32
AF = mybir.ActivationFunctionType
ALU = mybir.AluOpType
AX = mybir.AxisListType

D = 1024
SEQ = 8192
NQ = 4096
CTX = 256
NK = CTX + SEQ
NKT = NK // 128
EPS = 1e-6
MLA_SCALE = 96.0 ** -0.5
DIFF_SCALE = 64.0 ** -0.5
ALPHA = 2.0 ** 0.25
NE = 16
CPAD = 1024
NITER = 28

O_CQ, O_CKV, O_KR, O_KRP, O_DQ, O_DQP, O_DK, O_DKP, O_DV, O_G = 0, 256, 384, 416, 448, 960, 1472, 1984, 2496, 3008
NW = 5056
ENGS = ["sync", "scalar", "vector", "gpsimd", "tensor"]


class Sem:
    def __init__(self, h):
        self.h = h
        self.n = 0


class Prog:
    def __init__(self, nc):
        self.nc = nc
        self.q = {e: [] for e in ENGS}
        self.es = None

    def begin(self):
        self.es = contextlib.ExitStack()
        self.q = {e: [] for e in ENGS}
        self.fs = {}
        self.nphase = getattr(self, "nphase", 0) + 1

    def sem(self, name):
        return Sem(self.es.enter_context(self.nc.semaphore(name)))

    def sb(self, name, shape, dt):
        return self.es.enter_context(self.nc.sbuf_tensor(name, shape, dt))

    def op(self, eng, fn, sem=None, inc=1):
        if sem is None:
            self.q[eng].append(fn)
            return None
        h = sem.h
        self.q[eng].append(lambda e, fn=fn, h=h, inc=inc: fn(e).then_inc(h, inc))
        sem.n += inc
        return sem.n

    def opf(self, eng, fn):
        if self.fs.get(eng) is None:
            self.fs[eng] = self.sem("fence" + eng[:3] + str(self.nphase))
        v = self.op(eng, fn, self.fs[eng], 1)
        self.wait(eng, self.fs[eng], v)
        return v

    def dma(self, eng, out, in_, sem):
        return self.op(eng, lambda e, out=out, in_=in_: e.dma_start(out=out, in_=in_), sem, 16)

    def wait(self, eng, sem, val):
        if val is None or val <= 0:
            return
        h = sem.h
        self.q[eng].append(lambda e, h=h, val=val: e.wait_ge(h, val))

    def end(self):
        with self.nc.Block() as blk:
            for en in ENGS:
                fns = self.q[en]
                if not fns:
                    continue

                def body(e, fns=fns):
                    for f in fns:
                        f(e)

                getattr(blk, en)(body)
        self.es.close()
        self.es = None


class PsRing:
    def __init__(self, P, banks, tag):
        self.P = P
        self.banks = banks
        self.full = P.sem("full" + tag)
        self.free = {"scalar": P.sem("fra" + tag), "vector": P.sem("frv" + tag)}
        self.hist = []
        self.it = 0

    def acquire(self):
        it = self.it
        self.it += 1
        n = len(self.banks)
        if it >= n:
            eng, val = self.hist[it - n]
            self.P.wait("tensor", self.free[eng], val)
        self.hist.append(None)
        return it, self.banks[it % n]

    def produced(self, fn):
        return self.P.op("tensor", fn, self.full, 1)

    def consume_wait(self, eng, it):
        self.P.wait(eng, self.full, it + 1)

    def release(self, eng, its, fn):
        v = self.P.op(eng, fn, self.free[eng], 1)
        for it in its:
            self.hist[it] = (eng, v)
        return v


def build(debug=None, stage=99):
    nc = bass.Bass("TRN2", target_bir_lowering=False)
    dbg = set(debug or [])

    def din(name, shape, dt=F32):
        return nc.dram_tensor(name, list(shape), dt, kind="ExternalInput").ap()

    def scratch(name, shape, dt):
        if name in dbg:
            return nc.dram_tensor(name, list(shape), dt, kind="ExternalOutput").ap()
        return nc.dram_tensor(name, list(shape), dt).ap()

    xT = din("xT", [D, SEQ])
    xown = din("xown", [NQ, D])
    ctxT = din("ctxT", [D, CTX])
    cvec = din("cvec", [128, 8, 2])
    w_ada = din("w_ada", [D, 6 * D])
    b_ada_fm = din("b_ada_fm", [128, 16])
    b_ada_row = din("b_ada_row", [1, 6 * D])
    w_all = din("w_all", [D, NW])
    w_uq_all = din("w_uq_all", [256, 1024])
    w_ukv_all = din("w_ukv_all", [128, 1024])
    g_q_fm = din("g_q_fm", [128, 2])
    g_kv_fm = din("g_kv_fm", [128, 1])
    w_o_mla = din("w_o_mla", [512, D])
    w_o_diff = din("w_o_diff", [512, D])
    w_out = din("w_out", [D, D])
    dlam = din("dlam", [128, 256])
    g_sub_fm = din("g_sub_fm", [128, 1])
    lnrows = din("lnrows", [128, 4, D])
    w_router = din("w_router", [D, NE])
    b_router = din("b_router", [1, NE])
    moe_w1 = din("moe_w1", [NE, D, D])
    moe_w3 = din("moe_w3", [NE, D, D])
    moe_w2 = din("moe_w2", [NE, D, D])
    rope64_c = din("rope64_c", [128, SEQ])
    rope64_s = din("rope64_s", [128, SEQ])
    rope32_c = din("rope32_c", [128, SEQ])
    rope32_s = din("rope32_s", [128, SEQ])
    out = nc.dram_tensor("out", [NQ, D], F32, kind="ExternalOutput").ap()

    KTm = scratch("KTm", [8, 64, NK], BF16)
    KR = scratch("KR", [32, NK], BF16)
    Vm = scratch("Vm", [NK, 8, 65], BF16)
    QTm = scratch("QTm", [8, 96, NQ], BF16)
    KTd = scratch("KTd", [4, 128, NK], BF16)
    Vd = scratch("Vd", [NK, 512], BF16)
    QTd = scratch("QTd", [4, 128, NQ], BF16)
    GT = scratch("GT", [16, 128, NQ], BF16)
    OTm = scratch("OTm", [8, 64, NQ], BF16)
    OTd = scratch("OTd", [4, 128, NQ], F32)
    X1 = scratch("X1", [NQ, D], F32)
    AFF_IN = nc.dram_tensor("AFF_IN", [128, 32 * NE], F32)
    AFF_OUT = nc.dram_tensor("AFF_OUT", [256, 32 * NE], F32)
    XE = [scratch("XE%d" % i, [CPAD, D], BF16) for i in range(NE)]
    YE = [scratch("YE%d" % i, [CPAD, D], F32) for i in range(NE)]
    DBGT = scratch("DBGT", [128, 4096], F32)

    P = Prog(nc)
    top = contextlib.ExitStack()

    def gsb(name, shape, dt):
        return top.enter_context(nc.sbuf_tensor(name, shape, dt))

    modrow = gsb("modrow", [128, 4096], F32)
    modfm = gsb("modfm", [128, 16, 2], F32)
    ones_f = gsb("ones_f", [128, 128], F32)
    ones_b = gsb("ones_b", [128, 128], BF16)
    ident_f = gsb("ident_f", [128, 128], F32)
    ident_b = gsb("ident_b", [128, 128], BF16)
    neglam = gsb("neglam", [128, 1], F32)
    gsub = gsb("gsub", [128, 1], F32)
    eps_t = gsb("eps_t", [128, 1], F32)
    psall = top.enter_context(nc.psum_tensor("psall", [128, 8, 512], F32))
    psum = [psall[:, i, :] for i in range(8)]

    P.begin()
    s_ld = P.sem("p0ld")
    s_a = P.sem("p0a")
    s_v = P.sem("p0v")
    s_g = P.sem("p0g")
    s_pe = P.sem("p0pe")
    s_wfb = [P.sem("p0wf0"), P.sem("p0wf1")]
    cv = P.sb("cv", [128, 8, 2], F32)
    sv = P.sb("sv", [128, 8, 2], F32)
    srep = P.sb("srep", [128, 8, 128], F32)
    zer = P.sb("zer", [128, 128], F32)
    wch = [P.sb("wch%d" % i, [128, 8, 512], F32) for i in range(2)]
    brow = P.sb("brow", [1, 6 * D], F32)
    bfm = P.sb("bfm", [128, 16], F32)
    dl = P.sb("dl", [128, 256], F32)
    dlp = P.sb("dlp", [128, 128], F32)
    lsum = P.sb("lsum", [128, 2], F32)
    iot = P.sb("iot", [128, 128], F32)
    iop = P.sb("iop", [128, 1], F32)

    P.dma("sync", cv[:], cvec, s_ld)
    P.dma("sync", brow[:], b_ada_row, s_ld)
    P.dma("sync", bfm[:], b_ada_fm, s_ld)
    P.dma("sync", dl[:], dlam, s_ld)
    v_ld0 = P.dma("sync", gsub[:], g_sub_fm, s_ld)
    P.op("gpsimd", lambda e: e.memset(ones_f[:], 1.0))
    P.op("gpsimd", lambda e: e.memset(ones_b[:], 1.0))
    P.op("gpsimd", lambda e: e.memset(zer[:], 0.0))
    P.op("gpsimd", lambda e: e.memset(eps_t[:], EPS))
    P.op("gpsimd", lambda e: e.iota(iot[:], [[1, 128]], base=0, channel_multiplier=0, allow_small_or_imprecise_dtypes=True))
    P.opf("gpsimd", lambda e: e.iota(iop[:], [[0, 1]], base=0, channel_multiplier=1, allow_small_or_imprecise_dtypes=True))
    P.opf("gpsimd", lambda e: e.tensor_scalar(out=ident_f[:], in0=iot[:], scalar1=iop[:, 0:1], scalar2=None, op0=ALU.is_equal))
    v_g0 = P.op("gpsimd", lambda e: e.tensor_copy(out=ident_b[:], in_=ident_f[:]), s_g, 1)
    P.wait("scalar", s_ld, v_ld0)
    P.wait("scalar", s_g, v_g0)
    P.opf("scalar", lambda e: e.activation(out=sv[:], in_=cv[:], func=AF.Silu))
    for k in range(8):
        va = P.op("scalar", lambda e, k=k: e.activation(out=srep[:, k, :], in_=zer[:], func=AF.Identity, bias=sv[:, k, 0:1], scale=1.0), s_a, 1)
    v_srep = va
    P.wait("vector", s_ld, v_ld0)
    P.op("vector", lambda e: e.tensor_tensor(out=dlp[:, 0:64], in0=dl[:, 0:64], in1=dl[:, 64:128], op=ALU.mult))
    P.opf("vector", lambda e: e.tensor_tensor(out=dlp[:, 64:128], in0=dl[:, 128:192], in1=dl[:, 192:256], op=ALU.mult))
    P.op("vector", lambda e: e.tensor_reduce(out=lsum[:, 0:1], in_=dlp[:, 0:64], axis=AX.X, op=ALU.add))
    v_ls = P.op("vector", lambda e: e.tensor_reduce(out=lsum[:, 1:2], in_=dlp[:, 64:128], axis=AX.X, op=ALU.add), s_v, 1)
    P.wait("scalar", s_v, v_ls)
    v_le = P.op("scalar", lambda e: e.activation(out=lsum[:], in_=lsum[:], func=AF.Exp), s_a, 1)
    P.wait("vector", s_a, v_le)
    P.opf("vector", lambda e: e.tensor_scalar(out=neglam[:], in0=lsum[:, 1:2], scalar1=lsum[:, 0:1], scalar2=-0.2, op0=ALU.subtract, op1=ALU.add))

    wsrc = w_ada.rearrange("(k p) n -> p k n", p=128)
    pe_done = []
    fm_done = []
    row_done = []
    for j in range(12):
        buf = wch[j % 2]
        if j >= 2:
            P.wait("gpsimd", s_v, pe_done[j - 2])
        v_w = P.dma("gpsimd", buf[:], wsrc[:, :, j * 512:(j + 1) * 512], s_wfb[j % 2])
        P.wait("tensor", s_wfb[j % 2], v_w)
        if j == 0:
            P.wait("tensor", s_a, v_srep)
        if j < 4:
            for q in range(4):
                jj = j * 4 + q
                if jj >= 2:
                    P.wait("tensor", s_v, fm_done[jj - 2])
                for k in range(8):
                    fn = lambda e, k=k, q=q, jj=jj, buf=buf: e.matmul(psum[jj % 2][:, 0:2], lhsT=buf[:, k, q * 128:(q + 1) * 128], rhs=sv[:, k, :], start=(k == 0), stop=(k == 7))
                    if k == 7:
                        vp = P.op("tensor", fn, s_pe, 1)
                    else:
                        P.op("tensor", fn)
                P.wait("vector", s_pe, vp)
                if jj == 0:
                    P.wait("vector", s_ld, v_ld0)
                fm_done.append(P.op("vector", lambda e, jj=jj: e.tensor_scalar(out=modfm[:, jj, :], in0=psum[jj % 2][:, 0:2], scalar1=bfm[:, jj:jj + 1], scalar2=(1.0 if jj >= 8 else 0.0), op0=ALU.add, op1=ALU.add), s_v, 1))
            pe_done.append(fm_done[-1])
        else:
            jb = j - 4
            bank = psum[2 + (jb % 2)]
            if jb >= 2:
                P.wait("tensor", s_v, row_done[jb - 2])
            for k in range(8):
                P.op("tensor", lambda e, k=k, buf=buf, bank=bank: e.matmul(bank[:], lhsT=srep[:, k, :], rhs=buf[:, k, :], start=(k == 0), stop=False))
            vp = P.op("tensor", lambda e, j=j, bank=bank: e.matmul(bank[:], lhsT=ones_f[0:1, :], rhs=brow[0:1, j * 512:(j + 1) * 512], start=False, stop=True), s_pe, 1)
            P.wait("vector", s_pe, vp)
            addc = 1.0 if jb in (4, 5) else 0.0
            row_done.append(P.op("vector", lambda e, jb=jb, bank=bank, addc=addc: e.tensor_scalar(out=modrow[:, jb * 512:(jb + 1) * 512], in0=bank[:], scalar1=addc, scalar2=None, op0=ALU.add), s_v, 1))
            pe_done.append(row_done[-1])
    if "DBG0" in dbg:
        dbt = P.sb("dbt", [128, 64], F32)
        P.op("vector", lambda e: e.memset(dbt[:], 0.0))
        P.op("vector", lambda e: e.tensor_copy(out=dbt[:, 0:1], in_=neglam[:]))
        P.op("vector", lambda e: e.tensor_copy(out=dbt[:, 1:3], in_=lsum[:]))
        P.op("vector", lambda e: e.tensor_copy(out=dbt[:, 3:35], in_=modfm[:].rearrange("p a b -> p (a b)")))
        vdd = P.op("vector", lambda e: e.tensor_copy(out=dbt[:, 35:51], in_=dl[:, 0:16]), s_v, 1)
        P.wait("sync", s_v, vdd)
        P.dma("sync", DBGT[:, 64:4096], modrow[:, 64:4096], s_ld)
        P.dma("sync", DBGT[:, 0:64], dbt[:], s_ld)
        P.wait("sync", s_ld, s_ld.n)
    P.end()
    if stage <= 0:
        top.close()
        return nc

    P.begin()
    wall = P.sb("wall", [128, 8, NW], BF16)
    wuq = P.sb("wuq", [128, 2, 1024], BF16)
    wukv = P.sb("wukv", [128, 1024], BF16)
    gq = P.sb("gq", [128, 2], F32)
    gkv = P.sb("gkv", [128, 1], F32)
    xs = [P.sb("xs%d" % i, [128, 8, 512], F32) for i in range(2)]
    hT = [P.sb("hT%d" % i, [128, 8, 512], BF16) for i in range(2)]
    rt = [[P.sb("rt%d_%d" % (i, t), [128, 512], F32) for t in range(4)] for i in range(2)]
    ckv_sb = P.sb("ckv_sb", [128, 512], F32)
    ckv_sq = P.sb("ckv_sq", [128, 512], F32)
    ckvn = P.sb("ckvn", [128, 512], BF16)
    cq_sb = P.sb("cq_sb", [128, 2, 512], F32)
    cq_sq = P.sb("cq_sq", [128, 2, 512], F32)
    cqn = P.sb("cqn", [128, 2, 512], BF16)
    rtmps = [P.sb("rtmp%d" % i, [128, 512], F32) for i in range(4)]
    rt1 = P.sb("rt1", [128, 512], F32)
    rt2 = P.sb("rt2", [128, 512], F32)
    NST = 6
    stg = {"scalar": [P.sb("stga%d" % i, [128, 512], BF16) for i in range(NST)],
           "vector": [P.sb("stgv%d" % i, [128, 512], BF16) for i in range(NST)]}
    vst = [P.sb("vst%d" % i, [128, 8, 65], BF16) for i in range(2)]

    s_w = P.sem("p1w")
    s_xb = [P.sem("p1x0"), P.sem("p1x1")]
    s_h = P.sem("p1h")
    s_hfree = P.sem("p1hf")
    s_xfree = P.sem("p1xf")
    s_rtfree = P.sem("p1rf")
    s_cn = P.sem("p1cn")
    s_cs = P.sem("p1cs")
    s_st = {"scalar": P.sem("p1sta"), "vector": P.sem("p1stv")}
    s_outs = {"scalar": [P.sem("p1oa%d" % i) for i in range(NST)], "vector": [P.sem("p1ov%d" % i) for i in range(NST)]}
    s_vst = P.sem("p1vst")
    s_vouts = [P.sem("p1vo0"), P.sem("p1vo1")]
    ring = PsRing(P, psum, "p1")
    stg_n = {"scalar": 0, "vector": 0}
    stg_hist = {"scalar": [], "vector": []}

    wsrc = w_all.rearrange("(k p) n -> p k n", p=128)
    for c in range(8):
        P.dma("gpsimd", wall[:, :, c * 632:(c + 1) * 632], wsrc[:, :, c * 632:(c + 1) * 632], s_w)
    P.dma("gpsimd", wuq[:], w_uq_all.rearrange("(k p) n -> p k n", p=128), s_w)
    P.dma("gpsimd", wukv[:], w_ukv_all, s_w)
    P.dma("gpsimd", gq[:], g_q_fm, s_w)
    v_w = P.dma("gpsimd", gkv[:], g_kv_fm, s_w)
    P.op("gpsimd", lambda e: e.memset(vst[0][:, :, 64:65], 1.0))
    v_vm = P.op("gpsimd", lambda e: e.memset(vst[1][:, :, 64:65], 1.0), s_cs, 1)
    P.wait("tensor", s_w, v_w)
    P.wait("vector", s_w, v_w)
    P.wait("scalar", s_w, v_w)
    P.wait("scalar", s_cs, v_vm)
    P.wait("vector", s_cs, v_vm)

    def stage_out(eng, its, compute_fn, dst_list):
        i = stg_n[eng]
        stg_n[eng] += 1
        slot = stg[eng][i % NST]
        so = s_outs[eng][i % NST]
        if i >= NST:
            P.wait(eng, so, stg_hist[eng][i - NST])
        ring.release(eng, its, lambda e, slot=slot: compute_fn(e, slot))
        val = ring.free[eng].n
        P.wait("sync", ring.free[eng], val)
        last = None
        for dram_ap, sl in dst_list:
            last = P.dma("sync", dram_ap, sl(slot), so)
        stg_hist[eng].append(last)

    vst_n = [0]
    vst_hist = []
    alt = [0]

    def next_eng():
        alt[0] += 1
        return "scalar" if alt[0] % 2 else "vector"

    hfree_hist = []
    xfree_hist = []
    rtfree_hist = []
    for tt in range(17):
        N = 256 if tt == 0 else 512
        own = 1 <= tt <= 8
        isctx = tt == 0
        kcol = 0 if isctx else 256 + (tt - 1) * 512
        qcol = (tt - 1) * 512
        b = tt % 2
        mi = 1 if isctx else 0
        if tt >= 2:
            P.wait("gpsimd", s_h, xfree_hist[tt - 2])
        src = (ctxT if isctx else xT[:, qcol:qcol + 512]).rearrange("(k p) n -> p k n", p=128)
        s_x = s_xb[b]
        v_x = P.dma("gpsimd", xs[b][:, :, 0:N], src, s_x)
        if not isctx:
            if tt >= 3:
                P.wait("gpsimd", ring.free["vector"], rtfree_hist[tt - 3])
            for ti, tab in enumerate([rope64_c, rope64_s, rope32_c, rope32_s]):
                v_x = P.dma("gpsimd", rt[b][ti][:], tab[:, qcol:qcol + 512], s_x)
        P.wait("vector", s_x, v_x)
        if tt >= 2:
            P.wait("vector", ring.free["scalar"], hfree_hist[tt - 2])
        for k in range(8):
            fn = lambda e, k=k, b=b, N=N, mi=mi: e.tensor_scalar(out=hT[b][:, k, 0:N], in0=xs[b][:, k, 0:N], scalar1=modfm[:, 8 + k, mi:mi + 1], scalar2=modfm[:, k, mi:mi + 1], op0=ALU.mult, op1=ALU.add)
            if k == 7:
                v_h = P.op("vector", fn, s_h, 1)
            else:
                P.op("vector", fn)
        xfree_hist.append(v_h)
        P.wait("tensor", s_h, v_h)
        H = hT[b]

        def mm_full(col0, M, N=N, H=H):
            it, bank = ring.acquire()
            for k in range(8):
                fn = lambda e, k=k, bank=bank: e.matmul(bank[0:M, 0:N], lhsT=wall[:, k, col0:col0 + M], rhs=H[:, k, 0:N], start=(k == 0), stop=(k == 7))
                if k == 7:
                    ring.produced(fn)
                else:
                    P.op("tensor", fn)
            return it, bank

        it_ckv, bk_ckv = mm_full(O_CKV, 128)
        ring.consume_wait("scalar", it_ckv)
        P.op("scalar", lambda e, bk=bk_ckv, N=N: e.activation(out=ckv_sb[:, 0:N], in_=bk[:, 0:N], func=AF.Copy))
        v_sq = ring.release("scalar", [it_ckv], lambda e, bk=bk_ckv, N=N: e.activation(out=ckv_sq[:, 0:N], in_=bk[:, 0:N], func=AF.Square))
        if own:
            its_cq = []
            for c in range(2):
                it, bk = mm_full(O_CQ + c * 128, 128)
                ring.consume_wait("scalar", it)
                P.op("scalar", lambda e, bk=bk, c=c: e.activation(out=cq_sb[:, c, :], in_=bk[:], func=AF.Copy))
                v_sq = ring.release("scalar", [it], lambda e, bk=bk, c=c: e.activation(out=cq_sq[:, c, :], in_=bk[:], func=AF.Square))

        def rope_item(colx, colp, M, tabc, tabs, scale, dsts):
            it1, b1 = mm_full(colx, M)
            if isctx:
                eng = next_eng()
                ring.consume_wait(eng, it1)
                if eng == "scalar":
                    stage_out(eng, [it1], lambda e, slot, b1=b1, N=N: e.activation(out=slot[0:M, 0:N], in_=b1[0:M, 0:N], func=AF.Copy, scale=scale), dsts)
                else:
                    stage_out(eng, [it1], lambda e, slot, b1=b1, N=N: e.tensor_scalar(out=slot[0:M, 0:N], in0=b1[0:M, 0:N], scalar1=scale, scalar2=None, op0=ALU.mult), dsts)
                return
            it2, b2 = mm_full(colp, M)
            ring.consume_wait("vector", it2)
            P.op("vector", lambda e, b1=b1: e.tensor_tensor(out=rt1[0:M, :], in0=b1[0:M, :], in1=tabc[0:M, :], op=ALU.mult))
            P.op("vector", lambda e, b2=b2: e.tensor_tensor(out=rt2[0:M, :], in0=b2[0:M, :], in1=tabs[0:M, :], op=ALU.mult))
            stage_out("vector", [it1, it2], lambda e, slot: e.tensor_tensor(out=slot[0:M, :], in0=rt1[0:M, :], in1=rt2[0:M, :], op=ALU.add), dsts)

        R = rt[b]
        for hd in range(4):
            rope_item(O_DK + hd * 128, O_DKP + hd * 128, 128, R[0], R[1], 1.0,
                      [(KTd[hd, :, kcol:kcol + N], lambda s, N=N: s[:, 0:N])])
        rope_item(O_KR, O_KRP, 32, R[2], R[3], 1.0, [(KR[:, kcol:kcol + N], lambda s, N=N: s[0:32, 0:N])])
        for s4 in range(N // 128):
            it, bank = ring.acquire()
            for k in range(8):
                fn = lambda e, k=k, bank=bank, s4=s4, H=H: e.matmul(bank[:], lhsT=H[:, k, s4 * 128:(s4 + 1) * 128], rhs=wall[:, k, O_DV:O_DV + 512], start=(k == 0), stop=(k == 7))
                if k == 7:
                    ring.produced(fn)
                else:
                    P.op("tensor", fn)
            eng = next_eng()
            ring.consume_wait(eng, it)
            r0 = kcol + s4 * 128
            if eng == "scalar":
                stage_out(eng, [it], lambda e, slot, bank=bank: e.activation(out=slot[:], in_=bank[:], func=AF.Copy), [(Vd[r0:r0 + 128, :], lambda s: s[:])])
            else:
                stage_out(eng, [it], lambda e, slot, bank=bank: e.tensor_copy(out=slot[:], in_=bank[:]), [(Vd[r0:r0 + 128, :], lambda s: s[:])])

        def rms_finish(sq_aps, nin, src_aps, g_ap_fn, dst_aps, rtmp, rtmp2, N=N):
            it, bank = ring.acquire()
            P.wait("tensor", ring.free["scalar"], v_sq)
            for c in range(len(sq_aps)):
                fn = lambda e, c=c, bank=bank: e.matmul(bank[:, 0:N], lhsT=ones_f[:], rhs=sq_aps[c], start=(c == 0), stop=(c == len(sq_aps) - 1))
                if c == len(sq_aps) - 1:
                    ring.produced(fn)
                else:
                    P.op("tensor", fn)
            ring.consume_wait("scalar", it)
            v = ring.release("scalar", [it], lambda e, bank=bank: e.activation(out=rtmp[:, 0:N], in_=bank[:, 0:N], func=AF.Sqrt, bias=eps_t[:, 0:1], scale=1.0 / nin))
            P.wait("vector", ring.free["scalar"], v)
            P.op("vector", lambda e: e.reciprocal(out=rtmp2[:, 0:N], in_=rtmp[:, 0:N]))
            for c in range(len(src_aps)):
                fn = lambda e, c=c: e.scalar_tensor_tensor(out=dst_aps[c], in0=src_aps[c], scalar=g_ap_fn(c), in1=rtmp2[:, 0:N], op0=ALU.mult, op1=ALU.mult)
                if c == len(src_aps) - 1:
                    vv = P.op("vector", fn, s_cn, 1)
                else:
                    P.op("vector", fn)
            return vv

        v_ckvn = rms_finish([ckv_sq[:, 0:N]], 128.0, [ckv_sb[:, 0:N]], lambda c: gkv[:, 0:1], [ckvn[:, 0:N]], rtmps[0], rtmps[1])
        if own:
            v_cqn = rms_finish([cq_sq[:, 0, :], cq_sq[:, 1, :]], 256.0, [cq_sb[:, 0, :], cq_sb[:, 1, :]], lambda c: gq[:, c:c + 1], [cqn[:, 0, :], cqn[:, 1, :]], rtmps[2], rtmps[3])
            for hd in range(4):
                rope_item_q = None
                it1, b1 = mm_full(O_DQ + hd * 128, 128)
                it2, b2 = mm_full(O_DQP + hd * 128, 128)
                ring.consume_wait("vector", it2)
                P.op("vector", lambda e, b1=b1, R=R: e.tensor_tensor(out=rt1[:], in0=b1[:], in1=R[0][:], op=ALU.mult))
                P.op("vector", lambda e, b2=b2, R=R: e.tensor_tensor(out=rt2[:], in0=b2[:], in1=R[1][:], op=ALU.mult))
                P.op("vector", lambda e: e.tensor_tensor(out=rt1[:], in0=rt1[:], in1=rt2[:], op=ALU.add))
                stage_out("vector", [it1, it2], lambda e, slot: e.tensor_scalar(out=slot[:], in0=rt1[:], scalar1=DIFF_SCALE, scalar2=None, op0=ALU.mult),
                          [(QTd[hd, :, qcol:qcol + 512], lambda s: s[:])])
            for gc in range(16):
                it, bank = mm_full(O_G + gc * 128, 128)
                ring.consume_wait("scalar", it)
                stage_out("scalar", [it], lambda e, slot, bank=bank: e.activation(out=slot[:], in_=bank[:], func=AF.Sigmoid),
                          [(GT[gc, :, qcol:qcol + 512], lambda s: s[:])])
        P.wait("tensor", s_cn, v_ckvn)
        for j in range(4):
            it, bank = ring.acquire()
            ring.produced(lambda e, j=j, bank=bank, N=N: e.matmul(bank[:, 0:N], lhsT=wukv[:, j * 128:(j + 1) * 128], rhs=ckvn[:, 0:N], start=True, stop=True))
            eng = next_eng()
            ring.consume_wait(eng, it)
            dsts = [(KTm[2 * j, :, kcol:kcol + N], lambda s, N=N: s[0:64, 0:N]), (KTm[2 * j + 1, :, kcol:kcol + N], lambda s, N=N: s[64:128, 0:N])]
            if eng == "scalar":
                stage_out(eng, [it], lambda e, slot, bank=bank, N=N: e.activation(out=slot[:, 0:N], in_=bank[:, 0:N], func=AF.Copy), dsts)
            else:
                stage_out(eng, [it], lambda e, slot, bank=bank, N=N: e.tensor_copy(out=slot[:, 0:N], in_=bank[:, 0:N]), dsts)
        for s4 in range(N // 128):
            it, bank = ring.acquire()
            ring.produced(lambda e, s4=s4, bank=bank: e.matmul(bank[:], lhsT=ckvn[:, s4 * 128:(s4 + 1) * 128], rhs=wukv[:, 512:1024], start=True, stop=True))
            i = vst_n[0]
            vst_n[0] += 1
            vs = vst[i % 2]
            ring.consume_wait("vector", it)
            s_vout = s_vouts[i % 2]
            if i >= 2:
                P.wait("vector", s_vout, vst_hist[i - 2])
            v = ring.release("vector", [it], lambda e, vs=vs, bank=bank: e.tensor_copy(out=vs[:, :, 0:64], in_=bank[:].rearrange("p (h d) -> p h d", h=8)))
            P.wait("sync", ring.free["vector"], v)
            r0 = kcol + s4 * 128
            vst_hist.append(P.dma("sync", Vm[r0:r0 + 128, :, :], vs[:], s_vout))
        if own:
            P.wait("tensor", s_cn, v_cqn)
            for j in range(4):
                it, bank = ring.acquire()
                P.op("tensor", lambda e, j=j, bank=bank: e.matmul(bank[:], lhsT=wuq[:, 0, j * 128:(j + 1) * 128], rhs=cqn[:, 0, :], start=True, stop=False))
                ring.produced(lambda e, j=j, bank=bank: e.matmul(bank[:], lhsT=wuq[:, 1, j * 128:(j + 1) * 128], rhs=cqn[:, 1, :], start=False, stop=True))
                ring.consume_wait("scalar", it)
                dsts = [(QTm[2 * j, 0:64, qcol:qcol + 512], lambda s: s[0:64, :]), (QTm[2 * j + 1, 0:64, qcol:qcol + 512], lambda s: s[64:128, :])]
                stage_out("scalar", [it], lambda e, slot, bank=bank: e.activation(out=slot[:], in_=bank[:], func=AF.Copy, scale=MLA_SCALE), dsts)
            for j in range(2):
                it1, b1 = ring.acquire()
                P.op("tensor", lambda e, j=j, b1=b1: e.matmul(b1[:], lhsT=wuq[:, 0, 512 + j * 128:512 + (j + 1) * 128], rhs=cqn[:, 0, :], start=True, stop=False))
                ring.produced(lambda e, j=j, b1=b1: e.matmul(b1[:], lhsT=wuq[:, 1, 512 + j * 128:512 + (j + 1) * 128], rhs=cqn[:, 1, :], start=False, stop=True))
                it2, b2 = ring.acquire()
                P.op("tensor", lambda e, j=j, b2=b2: e.matmul(b2[:], lhsT=wuq[:, 0, 768 + j * 128:768 + (j + 1) * 128], rhs=cqn[:, 0, :], start=True, stop=False))
                ring.produced(lambda e, j=j, b2=b2: e.matmul(b2[:], lhsT=wuq[:, 1, 768 + j * 128:768 + (j + 1) * 128], rhs=cqn[:, 1, :], start=False, stop=True))
                ring.consume_wait("vector", it2)
                P.op("vector", lambda e, b1=b1, R=R: e.tensor_tensor(out=rt1[:], in0=b1[:], in1=R[2][:], op=ALU.mult))
                P.op("vector", lambda e, b2=b2, R=R: e.tensor_tensor(out=rt2[:], in0=b2[:], in1=R[3][:], op=ALU.mult))
                P.op("vector", lambda e: e.tensor_tensor(out=rt1[:], in0=rt1[:], in1=rt2[:], op=ALU.add))
                dsts = [(QTm[4 * j + hh, 64:96, qcol:qcol + 512], lambda s, hh=hh: s[hh * 32:(hh + 1) * 32, :]) for hh in range(4)]
                stage_out("vector", [it1, it2], lambda e, slot: e.tensor_scalar(out=slot[:], in0=rt1[:], scalar1=MLA_SCALE, scalar2=None, op0=ALU.mult), dsts)
        hfree_hist.append(ring.free["scalar"].n)
        if not isctx:
            rtfree_hist.append(ring.free["vector"].n)
    for eng in ("scalar", "vector"):
        for so in s_outs[eng]:
            P.wait("sync", so, so.n)
    for so in s_vouts:
        P.wait("sync", so, so.n)
    P.end()
    if stage <= 1:
        top.close()
        return nc


    P.begin()
    KTb = [P.sb("KTb%d" % i, [128, NK], BF16) for i in range(2)]
    Vb = [P.sb("Vb%d" % i, [128, NKT, 128], BF16) for i in range(2)]
    QTb = [P.sb("QTb%d" % i, [128, NQ], BF16) for i in range(2)]
    Pb = P.sb("Pb", [128, 4, 512], BF16)
    osb = [P.sb("osb%d" % i, [128, 512], F32) for i in range(2)]
    rden = [P.sb("rden%d" % i, [128, 512], F32) for i in range(2)]
    ostm = [P.sb("ostm%d" % i, [64, 512], BF16) for i in range(2)]
    ostd = [P.sb("ostd%d" % i, [128, 512], F32) for i in range(2)]
    dr0 = P.sb("dr0", [128, 512], F32)
    dr1 = P.sb("dr1", [128, 512], F32)
    dt1 = P.sb("dt1", [128, 512], F32)
    s_uld = [P.sem("p2ld0"), P.sem("p2ld1")]
    s_pes = P.sem("p2pes")
    s_act = P.sem("p2act")
    s_pv = P.sem("p2pv")
    s_fv = P.sem("p2fv")
    s_bc = P.sem("p2bc")
    s_dacc = P.sem("p2dacc")
    s_ones = P.sem("p2ones")
    accD = [P.sb("accD%d" % i, [128, 2, 512], F32) for i in range(2)]
    dstate = {"m": 0, "q": 0, "ones": []}
    s_ods = [P.sem("p2od0"), P.sem("p2od1")]
    od_hist = []
    nstep = [0]
    unit_end_pv = []

    def load_unit(u):
        b = u % 2
        if u >= 2:
            P.wait("sync", s_pv, unit_end_pv[u - 2])
        sem = s_uld[b]
        if u < 8:
            P.dma("sync", KTb[b][0:64, :], KTm[u], sem)
            P.dma("sync", KTb[b][64:96, :], KR, sem)
            P.dma("sync", QTb[b][0:96, :], QTm[u], sem)
            for g in range(6):
                P.dma("sync", Vb[b][:, g * 11:(g + 1) * 11, 0:65], Vm[g * 1408:(g + 1) * 1408, u, :].rearrange("(i p) d -> p i d", p=128), sem)
        else:
            hd = u - 8
            P.dma("sync", KTb[b][:, :], KTd[hd], sem)
            P.dma("sync", QTb[b][:, :], QTd[hd], sem)
            for g in range(6):
                P.dma("sync", Vb[b][:, g * 11:(g + 1) * 11, :], Vd[g * 1408:(g + 1) * 1408, hd * 128:(hd + 1) * 128].rearrange("(i p) d -> p i d", p=128), sem)
        return sem.n

    fin_state = {"n": 0, "bc_free": 0, "o_free": [0, 0], "od": []}
    uld_val = {0: load_unit(0)}
    for u in range(12):
        b = u % 2
        mla = u < 8
        if u + 1 < 12:
            uld_val[u + 1] = load_unit(u + 1)
        P.wait("tensor", s_uld[b], uld_val[u])
        R = 4 if mla else 2
        L = 2 if mla else 1
        dbase = dstate["m"]
        steps = [(j, i) for j in range(8) for i in range(NKT)]
        base = nstep[0]
        KT, V, QT = KTb[b], Vb[b], QTb[b]
        pend_fin = []

        def emit_S(s, base=base, mla=mla, R=R, KT=KT, QT=QT):
            j, i = steps[s]
            n = base + s
            if s >= R:
                P.wait("tensor", s_act, n - R + 1)
            elif base > 0:
                P.wait("tensor", s_act, base)
            if mla:
                P.op("tensor", lambda e, s=s, i=i, j=j: e.matmul(psum[s % 4][:, :], lhsT=KT[0:96, i * 128:(i + 1) * 128], rhs=QT[0:96, j * 512:(j + 1) * 512], start=True, stop=True), s_pes, 1)
            else:
                r = s % 2
                P.op("tensor", lambda e, r=r, i=i, j=j: e.matmul(psum[2 * r][:, :], lhsT=KT[0:64, i * 128:(i + 1) * 128], rhs=QT[0:64, j * 512:(j + 1) * 512], start=True, stop=True))
                P.op("tensor", lambda e, r=r, i=i, j=j: e.matmul(psum[2 * r + 1][:, :], lhsT=KT[64:128, i * 128:(i + 1) * 128], rhs=QT[64:128, j * 512:(j + 1) * 512], start=True, stop=True), s_pes, 1)

        def emit_exp(s, base=base, mla=mla, R=R):
            n = base + s
            P.wait("scalar", s_pes, n + 1)
            if s >= R:
                P.wait("scalar", s_pv, n - R + 1)
            elif base > 0:
                P.wait("scalar", s_pv, base)
            if mla:
                P.op("scalar", lambda e, s=s: e.activation(out=Pb[:, s % 4, :], in_=psum[s % 4][:, :], func=AF.Exp), s_act, 1)
            else:
                r = s % 2
                j, i = steps[s]
                need = (dbase + s - 1) if s >= 2 else dbase
                P.wait("scalar", s_dacc, need)
                P.op("scalar", lambda e, r=r: e.activation(out=Pb[:, 2 * r:2 * r + 2, :], in_=psall[:, 2 * r:2 * r + 2, :], func=AF.Exp), s_act, 1)
                qd = dstate["q"] + j
                a_ = accD[qd % 2]
                P.wait("vector", s_act, n + 1)
                if i == 0:
                    if qd >= 2:
                        P.wait("vector", s_ones, dstate["ones"][qd - 2])
                    P.op("vector", lambda e, a_=a_, r=r: e.tensor_copy(out=a_[:], in_=Pb[:, 2 * r:2 * r + 2, :]), s_dacc, 1)
                else:
                    P.op("vector", lambda e, a_=a_, r=r: e.tensor_tensor(out=a_[:], in0=a_[:], in1=Pb[:, 2 * r:2 * r + 2, :], op=ALU.add), s_dacc, 1)

        def emit_PV(s, base=base, mla=mla, V=V, u=u):
            j, i = steps[s]
            n = base + s
            P.wait("tensor", s_act, n + 1)
            if mla:
                ob = psum[4 + j % 2]
                if i == 0 and fin_state["o_free"][j % 2]:
                    P.wait("tensor", s_fv, fin_state["o_free"][j % 2])
                P.op("tensor", lambda e, s=s, i=i, ob=ob: e.matmul(ob[0:65, :], lhsT=V[:, i, 0:65], rhs=Pb[:, s % 4, :], start=(i == 0), stop=(i == NKT - 1)), s_pv, 1)
            else:
                r = s % 2
                if i == 0:
                    P.wait("tensor", s_fv, max(fin_state["o_free"][0], fin_state["o_free"][1], fin_state["bc_free"]))
                st, sp = (i == 0), (i == NKT - 1)
                P.op("tensor", lambda e, r=r, i=i, st=st, sp=sp: e.matmul(psum[4][:, :], lhsT=V[:, i, :], rhs=Pb[:, 2 * r, :], start=st, stop=sp))
                P.op("tensor", lambda e, r=r, i=i, st=st, sp=sp: e.matmul(psum[5][:, :], lhsT=V[:, i, :], rhs=Pb[:, 2 * r + 1, :], start=st, stop=sp), s_pv, 1)
            if i == NKT - 1:
                fin_dve(u, j, n + 1, dbase + s + 1)

        def fin_dve(u, j, pvval, accval=0):
            k = fin_state["n"]
            fin_state["n"] += 1
            P.wait("vector", s_pv, pvval)
            if u < 8:
                ob = psum[4 + j % 2]
                o, rd, stg = osb[k % 2], rden[k % 2], ostm[k % 2]
                v = P.op("vector", lambda e, ob=ob, o=o: e.tensor_copy(out=o[0:65, :], in_=ob[0:65, :]), s_fv, 1)
                fin_state["o_free"][j % 2] = v
                v2 = P.op("vector", lambda e, o=o, rd=rd: e.reciprocal(out=rd[64:65, :], in_=o[64:65, :]), s_fv, 1)
                pend_fin.append((u, j, k, v2))
            else:
                hd = u - 8
                stg = ostd[k % 2]
                qd = dstate["q"] + j
                P.wait("tensor", s_dacc, accval)
                P.wait("tensor", s_fv, max(fin_state["o_free"][0], fin_state["o_free"][1], fin_state["bc_free"]))
                P.op("tensor", lambda e, qd=qd: e.matmul(psum[6][:, :], lhsT=ones_f[:, :], rhs=accD[qd % 2][:, 0, :], start=True, stop=True))
                vo = P.op("tensor", lambda e, qd=qd: e.matmul(psum[7][:, :], lhsT=ones_f[:, :], rhs=accD[qd % 2][:, 1, :], start=True, stop=True), s_ones, 1)
                dstate["ones"].append(vo)
                P.wait("vector", s_ones, vo)
                s_od = s_ods[k % 2]
                if len(fin_state["od"]) >= 2:
                    P.wait("vector", s_od, fin_state["od"][-2])
                P.op("vector", lambda e: e.reciprocal(out=dr0[:], in_=psum[6][:, :]))
                P.op("vector", lambda e: e.reciprocal(out=dr1[:], in_=psum[7][:, :]))
                P.op("vector", lambda e: e.tensor_tensor(out=dr0[:], in0=psum[4][:, :], in1=dr0[:], op=ALU.mult))
                v = P.op("vector", lambda e: e.tensor_tensor(out=dt1[:], in0=psum[5][:, :], in1=dr1[:], op=ALU.mult), s_fv, 1)
                fin_state["o_free"][0] = v
                v3 = P.op("vector", lambda e, stg=stg: e.scalar_tensor_tensor(out=stg[:], in0=dt1[:], scalar=neglam[:, 0:1], in1=dr0[:], op0=ALU.mult, op1=ALU.add), s_fv, 1)
                P.wait("sync", s_fv, v3)
                fin_state["od"].append(P.dma("sync", OTd[hd, :, j * 512:(j + 1) * 512], stg[:], s_od))

        def fin_pe():
            while pend_fin:
                u_, j, k, v2 = pend_fin.pop(0)
                o, rd, stg = osb[k % 2], rden[k % 2], ostm[k % 2]
                P.wait("tensor", s_fv, max(v2, fin_state["bc_free"]))
                vb = P.op("tensor", lambda e, rd=rd: e.matmul(psum[6][0:64, :], lhsT=ones_f[64:65, 0:64], rhs=rd[64:65, :], start=True, stop=True), s_bc, 1)
                P.wait("vector", s_bc, vb)
                s_od = s_ods[k % 2]
                if len(fin_state["od"]) >= 2:
                    P.wait("vector", s_od, fin_state["od"][-2])
                v3 = P.op("vector", lambda e, o=o, stg=stg: e.tensor_tensor(out=stg[:, :], in0=o[0:64, :], in1=psum[6][0:64, :], op=ALU.mult), s_fv, 1)
                fin_state["bc_free"] = v3
                P.wait("sync", s_fv, v3)
                fin_state["od"].append(P.dma("sync", OTm[u_, :, j * 512:(j + 1) * 512], stg[:, :], s_od))

        ns = len(steps)
        for s in range(min(L, ns)):
            emit_S(s)
            emit_exp(s)
        for s in range(ns):
            if s + L < ns:
                emit_S(s + L)
                emit_exp(s + L)
            emit_PV(s)
            if mla and pend_fin and (steps[s][1] == 4):
                fin_pe()
        if mla:
            fin_pe()
        nstep[0] += ns
        if not mla:
            dstate["m"] += ns
            dstate["q"] += 8
        unit_end_pv.append(s_pv.n)
    for so in s_ods:
        P.wait("sync", so, so.n)
    P.end()
    if stage <= 2:
        top.close()
        return nc

    H2D = scratch("H2D", [NQ, D], BF16)
    aff = gsb("aff", [128, 32, NE], F32)
    iotg = gsb("iotg", [128, 128], F32)
    iopg = gsb("iopg", [128, 1], F32)

    class Tk:
        def __init__(self, tag):
            self.s = {en: P.sem(tag + en[:3]) for en in ENGS}

        def do(self, eng, fn, deps=(), dma=False):
            for d_ in deps:
                if d_ is not None and d_[0] != eng:
                    P.wait(eng, self.s[d_[0]], d_[1])
            v = P.op(eng, fn, self.s[eng], 1)
            return (eng, v)

    def dmado(eng, out_, in_, sem, deps, tk):
        for d_ in deps:
            if d_ is not None and d_[0] != eng:
                P.wait(eng, tk.s[d_[0]], d_[1])
        return P.dma(eng, out_, in_, sem)

    P.begin()
    tk = Tk("q")
    Wom = P.sb("Wom", [64, 8, D], BF16)
    Wod = P.sb("Wod", [128, 4, D], BF16)
    Wout = P.sb("Wout", [128, 8, D], BF16)
    wr = P.sb("wr", [128, 8, NE], F32)
    brt = P.sb("brt", [1, NE], F32)
    lnr = P.sb("lnr", [128, 4, D], F32)
    gsub08 = P.sb("gsub08", [128, 1], F32)
    om = P.sb("om", [64, 8, 512], BF16)
    od32 = P.sb("od32", [128, 4, 512], F32)
    odb = P.sb("odb", [128, 4, 512], BF16)
    gtb = P.sb("gtb", [128, 16, 512], BF16)
    yT = P.sb("yT", [128, 8, 512], BF16)
    xt = [P.sb("xt%d" % i, [128, D], F32) for i in range(2)]
    tA = P.sb("tA", [128, 512], F32)
    tB = P.sb("tB", [128, 512], F32)
    tS = P.sb("tS", [128, 512], F32)
    vvs = [P.sb("vv%d" % i, [128, D], F32) for i in range(2)]
    x1t = [P.sb("x1t%d" % i, [128, D], F32) for i in range(2)]
    h2fs = [P.sb("h2f%d" % i, [128, D], F32) for i in range(2)]
    h2b = [P.sb("h2b%d" % i, [128, D], BF16) for i in range(2)]
    h2Ts = [P.sb("h2T%d" % i, [128, 8, 128], F32) for i in range(2)]
    bsts = [P.sb("bst%d" % i, [128, 2, 6], F32) for i in range(2)]
    mvs = [P.sb("mv%d" % i, [128, 2], F32) for i in range(2)]
    sd1s = [P.sb("sd1%d" % i, [128, 1], F32) for i in range(2)]
    rs1s = [P.sb("rs1%d" % i, [128, 1], F32) for i in range(2)]
    lgs = [P.sb("lg%d" % i, [128, NE], F32) for i in range(2)]
    exs = [P.sb("ex%d" % i, [128, NE], F32) for i in range(2)]
    mxs = [P.sb("mx%d" % i, [128, 1], F32) for i in range(2)]
    ssums = [P.sb("ssum%d" % i, [128, 1], F32) for i in range(2)]
    s_w = P.sem("qw")
    s_lt = P.sem("qlt")
    s_lx = [P.sem("qlx0"), P.sem("qlx1")]
    s_o1 = [P.sem("qo10"), P.sem("qo11")]
    s_o2 = [P.sem("qo20"), P.sem("qo21")]
    P.dma("gpsimd", Wom[:], w_o_mla.rearrange("(h d) n -> d h n", d=64), s_w)
    P.dma("gpsimd", Wod[:], w_o_diff.rearrange("(h d) n -> d h n", d=128), s_w)
    P.dma("gpsimd", Wout[:], w_out.rearrange("(k p) n -> p k n", p=128), s_w)
    P.dma("gpsimd", wr[:], w_router.rearrange("(k p) n -> p k n", p=128), s_w)
    P.dma("gpsimd", brt[:], b_router, s_w)
    v_w = P.dma("gpsimd", lnr[:], lnrows, s_w)
    P.op("gpsimd", lambda e: e.iota(iotg[:], [[1, 128]], base=0, channel_multiplier=0, allow_small_or_imprecise_dtypes=True))
    P.op("gpsimd", lambda e: e.iota(iopg[:], [[0, 1]], base=0, channel_multiplier=1, allow_small_or_imprecise_dtypes=True))
    for en in ("tensor", "vector", "scalar"):
        P.wait(en, s_w, v_w)
    P.opf("vector", lambda e: e.tensor_scalar(out=gsub08[:], in0=gsub[:], scalar1=0.8, scalar2=None, op0=ALU.mult))
    ring = PsRing(P, psum, "q")
    last_tile_done = None
    xfree = {}
    x1_hist = {}
    h2_hist = {}
    h2f_free = {}
    lasts = {}
    for j in range(8):
        c0 = j * 512
        deps = [last_tile_done]
        for d_ in deps:
            if d_ is not None:
                P.wait("sync", tk.s[d_[0]], d_[1])
        P.dma("sync", om[:], OTm[:, :, c0:c0 + 512].rearrange("h d t -> d h t"), s_lt)
        P.dma("sync", od32[:], OTd[:, :, c0:c0 + 512].rearrange("h p t -> p h t"), s_lt)
        v_lt = P.dma("sync", gtb[:], GT[:, :, c0:c0 + 512].rearrange("c p t -> p c t"), s_lt)
        for en in ("tensor", "vector", "scalar"):
            P.wait(en, s_lt, v_lt)
        for hd in range(4):
            t1 = tk.do("scalar", lambda e, hd=hd: e.activation(out=tS[:], in_=od32[:, hd, :], func=AF.Square))
            it, bank = ring.acquire()
            P.wait("tensor", tk.s["scalar"], t1[1])
            ring.produced(lambda e, bank=bank: e.matmul(bank[:, :], lhsT=ones_f[:], rhs=tS[:], start=True, stop=True))
            ring.consume_wait("scalar", it)
            v = ring.release("scalar", [it], lambda e, bank=bank: e.activation(out=tA[:], in_=bank[:, :], func=AF.Sqrt, bias=eps_t[:, 0:1], scale=1.0 / 128.0))
            P.wait("vector", ring.free["scalar"], v)
            P.op("vector", lambda e: e.reciprocal(out=tB[:], in_=tA[:]))
            t2 = tk.do("vector", lambda e, hd=hd: e.scalar_tensor_tensor(out=odb[:, hd, :], in0=od32[:, hd, :], scalar=gsub08[:, 0:1], in1=tB[:], op0=ALU.mult, op1=ALU.mult))
            P.wait("scalar", tk.s["vector"], t2[1])
        P.wait("tensor", tk.s["vector"], t2[1])
        for c in range(8):
            itA, bA = ring.acquire()
            for h in range(8):
                fn = lambda e, h=h, c=c, bA=bA: e.matmul(bA[:, :], lhsT=Wom[0:64, h, c * 128:(c + 1) * 128], rhs=om[0:64, h, :], start=(h == 0), stop=(h == 7))
                if h == 7:
                    ring.produced(fn)
                else:
                    P.op("tensor", fn)
            itB, bB = ring.acquire()
            for hd in range(4):
                fn = lambda e, hd=hd, c=c, bB=bB: e.matmul(bB[:, :], lhsT=Wod[:, hd, c * 128:(c + 1) * 128], rhs=odb[:, hd, :], start=(hd == 0), stop=(hd == 3))
                if hd == 3:
                    ring.produced(fn)
                else:
                    P.op("tensor", fn)
            ring.consume_wait("vector", itB)
            P.op("vector", lambda e, c=c, bA=bA: e.tensor_tensor(out=tA[:], in0=bA[:, :], in1=gtb[:, c, :], op=ALU.mult))
            P.op("vector", lambda e, c=c, bB=bB: e.tensor_tensor(out=tB[:], in0=bB[:, :], in1=gtb[:, 8 + c, :], op=ALU.mult))
            vy = ring.release("vector", [itA, itB], lambda e, c=c: e.tensor_tensor(out=yT[:, c, :], in0=tA[:], in1=tB[:], op=ALU.add))
        P.wait("tensor", ring.free["vector"], vy)
        def subtile(t, s4, ltd):
            p_ = t % 2
            vv, h2f, h2T, bst, mv, sd1, rs1 = vvs[p_], h2fs[p_], h2Ts[p_], bsts[p_], mvs[p_], sd1s[p_], rs1s[p_]
            lg, ex, mx, ssum = lgs[p_], exs[p_], mxs[p_], ssums[p_]
            xb = xt[t % 2]
            r0 = t * 128
            if t >= 2:
                P.wait("gpsimd", tk.s["vector"], xfree[t - 2])
            v_x = P.dma("gpsimd", xb[:], xown[r0:r0 + 128, :], s_lx[t % 2])
            its = []
            bks = []
            for nh in range(2):
                it, bank = ring.acquire()
                for c in range(8):
                    fn = lambda e, c=c, nh=nh, s4=s4, bank=bank: e.matmul(bank[:, :], lhsT=yT[:, c, s4 * 128:(s4 + 1) * 128], rhs=Wout[:, c, nh * 512:(nh + 1) * 512], start=(c == 0), stop=(c == 7))
                    if c == 7:
                        ring.produced(fn)
                    else:
                        P.op("tensor", fn)
                its.append(it)
                bks.append(bank)
            yield
            ring.consume_wait("vector", its[1])
            P.op("vector", lambda e, b0=bks[0]: e.tensor_tensor(out=vv[:, 0:512], in0=b0[:, :], in1=modrow[:, 0:512], op=ALU.mult))
            ring.release("vector", its, lambda e, b1=bks[1]: e.tensor_tensor(out=vv[:, 512:1024], in0=b1[:, :], in1=modrow[:, 512:1024], op=ALU.mult))
            P.wait("vector", s_lx[t % 2], v_x)
            xfree[t] = tk.do("vector", lambda e, xb=xb: e.scalar_tensor_tensor(out=vv[:], in0=xb[:], scalar=ALPHA, in1=vv[:], op0=ALU.mult, op1=ALU.add))[1]
            P.op("vector", lambda e: e.bn_stats(out=bst[:, 0, :], in_=vv[:, 0:512]))
            P.opf("vector", lambda e: e.bn_stats(out=bst[:, 1, :], in_=vv[:, 512:1024]))
            P.opf("vector", lambda e: e.tensor_copy(out=tA[:], in_=vv[:, 0:512]))
            t3 = tk.do("vector", lambda e: e.bn_aggr(out=mv[:], in_=bst[:].rearrange("p a b -> p (a b)")))
            t4 = tk.do("scalar", lambda e: e.activation(out=sd1[:], in_=mv[:, 1:2], func=AF.Sqrt, bias=eps_t[:, 0:1], scale=1.0), [t3])
            yield
            P.wait("vector", tk.s["scalar"], t4[1])
            P.opf("vector", lambda e: e.reciprocal(out=rs1[:], in_=sd1[:]))
            x1b = x1t[t % 2]
            if t >= 2:
                P.wait("vector", s_o1[t % 2], x1_hist[t - 2])
            P.op("vector", lambda e, x1b=x1b: e.tensor_scalar(out=x1b[:], in0=vv[:], scalar1=mv[:, 0:1], scalar2=rs1[:, 0:1], op0=ALU.subtract, op1=ALU.mult))
            t5 = tk.do("vector", lambda e, x1b=x1b: e.tensor_tensor(out=x1b[:], in0=x1b[:], in1=lnr[:, 0, :], op=ALU.mult))
            t6 = tk.do("gpsimd", lambda e, x1b=x1b: e.tensor_tensor(out=x1b[:], in0=x1b[:], in1=lnr[:, 1, :], op=ALU.add), [t5, ltd])
            P.wait("sync", tk.s["gpsimd"], t6[1])
            x1_hist[t] = P.dma("sync", X1[r0:r0 + 128, :], x1b[:], s_o1[t % 2])
            if t >= 2:
                P.wait("gpsimd", tk.s["scalar"], h2f_free[t - 2])
            P.op("gpsimd", lambda e, x1b=x1b: e.tensor_tensor(out=h2f[:], in0=x1b[:], in1=modrow[:, 2048:3072], op=ALU.mult))
            t7 = tk.do("gpsimd", lambda e: e.tensor_tensor(out=h2f[:], in0=h2f[:], in1=modrow[:, 1024:2048], op=ALU.add))
            hb = h2b[t % 2]
            if t >= 2:
                P.wait("scalar", s_o2[t % 2], h2_hist[t - 2])
            t8 = tk.do("scalar", lambda e, hb=hb: e.activation(out=hb[:], in_=h2f[:], func=AF.Copy), [t7])
            P.wait("sync", tk.s["scalar"], t8[1])
            h2_hist[t] = P.dma("sync", H2D[r0:r0 + 128, :], hb[:], s_o2[t % 2])
            yield
            P.wait("tensor", tk.s["gpsimd"], t7[1])
            tits = []
            tbk = []
            for hh in range(2):
                it, bank = ring.acquire()
                for q in range(4):
                    c = hh * 4 + q
                    fn = lambda e, c=c, q=q, bank=bank: e.transpose(out=bank[:, q * 128:(q + 1) * 128], in_=h2f[:, c * 128:(c + 1) * 128], identity=ident_f[:])
                    if q == 3:
                        ring.produced(fn)
                    else:
                        P.op("tensor", fn)
                tits.append(it)
                tbk.append(bank)
            for hh in range(2):
                ring.consume_wait("scalar", tits[hh])
                vh = ring.release("scalar", [tits[hh]], lambda e, hh=hh, bank=tbk[hh]: e.activation(out=h2T[:, hh * 4:(hh + 1) * 4, :], in_=bank[:, :].rearrange("p (q n) -> p q n", q=4), func=AF.Copy))
            tk.do("scalar", lambda e: e.nop())
            h2f_free[t] = tk.s["scalar"].n
            yield
            P.wait("tensor", ring.free["scalar"], vh)
            it, bank = ring.acquire()
            for c in range(8):
                P.op("tensor", lambda e, c=c, bank=bank: e.matmul(bank[:, 0:NE], lhsT=h2T[:, c, :], rhs=wr[:, c, :], start=(c == 0), stop=False))
            ring.produced(lambda e, bank=bank: e.matmul(bank[:, 0:NE], lhsT=ones_f[0:1, :], rhs=brt[0:1, :], start=False, stop=True))
            ring.consume_wait("vector", it)
            P.opf("vector", lambda e, bank=bank: e.tensor_reduce(out=mx[:], in_=bank[:, 0:NE], axis=AX.X, op=ALU.max, negate=True))
            vl = ring.release("vector", [it], lambda e, bank=bank: e.tensor_copy(out=lg[:], in_=bank[:, 0:NE]))
            yield
            P.wait("scalar", ring.free["vector"], vl)
            t9 = tk.do("scalar", lambda e: e.activation(out=ex[:], in_=lg[:], func=AF.Exp, bias=mx[:, 0:1], scale=1.0, accum_out=ssum[:, 0:1]))
            P.wait("vector", tk.s["scalar"], t9[1])
            P.opf("vector", lambda e: e.reciprocal(out=ssum[:], in_=ssum[:]))
            last_ = tk.do("vector", lambda e, t=t: e.tensor_scalar(out=aff[:, t, :], in0=ex[:], scalar1=ssum[:, 0:1], scalar2=None, op0=ALU.mult))
            P.wait("scalar", tk.s["vector"], last_[1])
            lasts[t] = last_

        for pair in ((0, 1), (2, 3)):
            gens = [subtile(j * 4 + a_, a_, last_tile_done) for a_ in pair]
            alive = True
            while alive:
                alive = False
                for g_ in gens:
                    try:
                        next(g_)
                        alive = True
                    except StopIteration:
                        pass
        last = lasts[j * 4 + 3]
        last_tile_done = last
    for so in s_o1 + s_o2:
        P.wait("sync", so, so.n)
    P.end()
    if stage <= 3:
        top.close()
        return nc

    slotidx = gsb("slotidx", [128, NE, 32], I32)
    gmask = gsb("gmask", [128, NE, 32], F32)
    P.begin()
    tk = Tk("r")
    affall = P.sb("affall", [128, 64, NE], F32)
    affT = P.sb("affT", [128, NE, 64], F32)
    affTo = P.sb("affTo", [128, NE, 32], F32)
    junk = P.sb("junk", [128, 64], F32)
    cnt = P.sb("cnt", [128, NE], F32)
    lo = P.sb("lo", [128, NE], F32)
    mid = P.sb("mid", [128, NE], F32)
    ge = P.sb("ge", [128, NE], F32)
    maskT = P.sb("maskT", [128, NE, 32], F32)
    maskb = P.sb("maskb", [128, NE, 32], BF16)
    Sx = P.sb("Sx", [128, NE, 32], F32)
    Sb = P.sb("Sb", [128, NE, 32], BF16)
    Ub = P.sb("Ub", [128, 128], BF16)
    posf = P.sb("posf", [128, NE, 32], F32)
    s_d = P.sem("rd")
    s_cc = P.sem("rcc")
    v = P.dma("gpsimd", AFF_IN.ap(), aff[:].rearrange("p t e -> p (t e)"), s_d)
    P.wait("gpsimd", s_d, v)
    P.op("gpsimd", lambda e: e.collective_compute("AllGather", ALU.bypass, replica_groups=[[0, 1], [2, 3], [4, 5], [6, 7]], ins=[AFF_IN.ap().opt()], outs=[AFF_OUT.ap().opt()]), s_cc, 1)
    P.wait("gpsimd", s_cc, 1)
    P.dma("gpsimd", affall[:, 0:32, :], AFF_OUT.ap()[0:128, :].rearrange("p (t e) -> p t e", e=NE), s_d)
    v = P.dma("gpsimd", affall[:, 32:64, :], AFF_OUT.ap()[128:256, :].rearrange("p (t e) -> p t e", e=NE), s_d)
    P.wait("vector", s_d, v)
    P.op("vector", lambda e: e.tensor_copy(out=affT[:], in_=affall[:].rearrange("p t e -> p e t")))
    P.op("vector", lambda e: e.tensor_copy(out=affTo[:], in_=aff[:].rearrange("p t e -> p e t")))
    P.opf("vector", lambda e: e.memset(lo[:], 0.0))
    P.op("vector", lambda e: e.tensor_scalar(out=Ub[:], in0=iotg[:], scalar1=iopg[:, 0:1], scalar2=None, op0=ALU.is_gt))
    for itn in range(NITER):
        step = 2.0 ** -(itn + 1)
        P.opf("vector", lambda e, step=step: e.tensor_scalar(out=mid[:], in0=lo[:], scalar1=step, scalar2=None, op0=ALU.add))
        for ex_ in range(NE):
            fn = lambda e, ex_=ex_: e.tensor_scalar(out=junk[:], in0=affT[:, ex_, :], scalar1=mid[:, ex_:ex_ + 1], scalar2=0.0, op0=ALU.is_gt, op1=ALU.add, accum_out=cnt[:, ex_:ex_ + 1])
            if ex_ == NE - 1:
                tc_ = tk.do("vector", fn)
            else:
                P.op("vector", fn)
        bank = psum[itn % 2]
        tp = tk.do("tensor", lambda e, bank=bank: e.matmul(bank[:, 0:NE], lhsT=ones_f[:], rhs=cnt[:], start=True, stop=True), [tc_])
        P.wait("vector", tk.s["tensor"], tp[1])
        P.opf("vector", lambda e, bank=bank: e.tensor_scalar(out=ge[:], in0=bank[:, 0:NE], scalar1=float(CPAD) - 0.5, scalar2=None, op0=ALU.is_ge))
        P.opf("vector", lambda e, step=step: e.scalar_tensor_tensor(out=lo[:], in0=ge[:], scalar=step, in1=lo[:], op0=ALU.mult, op1=ALU.add))
    for ex_ in range(NE):
        P.op("vector", lambda e, ex_=ex_: e.tensor_scalar(out=maskT[:, ex_, :], in0=affTo[:, ex_, :], scalar1=lo[:, ex_:ex_ + 1], scalar2=None, op0=ALU.is_gt))
    P.op("vector", lambda e: e.tensor_tensor(out=gmask[:], in0=maskT[:], in1=affTo[:], op=ALU.mult))
    P.op("vector", lambda e: e.tensor_copy(out=maskb[:], in_=maskT[:]))
    P.opf("vector", lambda e: e.memset(Sx[:], 0.0))
    for t in range(1, 32):
        P.opf("vector", lambda e, t=t: e.tensor_tensor(out=Sx[:, :, t], in0=Sx[:, :, t - 1], in1=maskT[:, :, t - 1], op=ALU.add))
    tsb = tk.do("vector", lambda e: e.tensor_copy(out=Sb[:], in_=Sx[:]))
    P.wait("tensor", tk.s["vector"], tsb[1])
    P.op("tensor", lambda e: e.matmul(psum[2][:, :], lhsT=ones_b[:], rhs=Sb[:].rearrange("p e t -> p (e t)"), start=True, stop=False))
    tpp = tk.do("tensor", lambda e: e.matmul(psum[2][:, :], lhsT=Ub[:], rhs=maskb[:].rearrange("p e t -> p (e t)"), start=False, stop=True))
    P.wait("vector", tk.s["tensor"], tpp[1])
    P.op("vector", lambda e: e.scalar_tensor_tensor(out=posf[:].rearrange("p e t -> p (e t)"), in0=maskT[:].rearrange("p e t -> p (e t)"), scalar=-1048576.0, in1=psum[2][:, :], op0=ALU.mult, op1=ALU.add))
    P.op("vector", lambda e: e.tensor_scalar(out=posf[:], in0=posf[:], scalar1=1048576.0, scalar2=None, op0=ALU.add))
    tdb = tk.do("vector", lambda e: e.tensor_copy(out=slotidx[:], in_=posf[:]))
    if "DBGT" in dbg:
        P.wait("sync", tk.s["vector"], tdb[1])
        P.dma("sync", DBGT[:, 0:512], aff[:].rearrange("p t e -> p (t e)"), s_d)
        P.dma("sync", DBGT[:, 512:1024], posf[:].rearrange("p e t -> p (e t)"), s_d)
        P.dma("sync", DBGT[:, 1024:1536], gmask[:].rearrange("p e t -> p (e t)"), s_d)
        vdb = P.dma("sync", DBGT[:, 1536:1552], lo[:], s_d)
        P.wait("sync", s_d, vdb)
    P.end()
    if stage <= 4:
        top.close()
        return nc

    P.begin()
    tk = Tk("m")
    w1b = [P.sb("w1b%d" % i, [128, 8, D], BF16) for i in range(2)]
    w3b = [P.sb("w3b%d" % i, [128, 8, D], BF16) for i in range(2)]
    w2b = [P.sb("w2b%d" % i, [128, 8, D], BF16) for i in range(2)]
    h2t = [P.sb("h2t%d" % i, [128, D], BF16) for i in range(4)]
    xtok = [P.sb("xtok%d" % i, [128, 4, D], BF16) for i in range(2)]
    xeT = [P.sb("xeT%d" % i, [128, 8, 512], BF16) for i in range(2)]
    hid = P.sb("hid", [128, 8, 512], BF16)
    sgt = [P.sb("sgt%d" % i, [128, 512], F32) for i in range(2)]
    yst = [P.sb("yst%d" % i, [128, D], F32) for i in range(2)]
    s_wl = [P.sem("mw0"), P.sem("mw1")]
    s_hl = [P.sem("mh%d" % i) for i in range(4)]
    s_sc = [P.sem("msc%d" % i) for i in range(4)]
    s_scall = P.sem("mscall")
    s_xl = [P.sem("mx0"), P.sem("mx1")]
    s_yo = [P.sem("my0"), P.sem("my1")]
    ring = PsRing(P, psum, "m")
    psb = [psall[:, i, :].bitcast(BF16) for i in range(8)]
    wl_val = {}
    sc_done = {}
    exp_done = {}
    hcount = [0]
    h_hist = []
    sc_hist = []

    def load_w(e_):
        b = e_ % 2
        if e_ >= 2:
            a_, v_ = exp_done[e_ - 2]
            P.wait("gpsimd", ring.free["scalar"], a_)
            P.wait("gpsimd", ring.free["vector"], v_)
        P.dma("gpsimd", w1b[b][:], moe_w1[e_].rearrange("(k p) n -> p k n", p=128), s_wl[b])
        P.dma("gpsimd", w3b[b][:], moe_w3[e_].rearrange("(k p) n -> p k n", p=128), s_wl[b])
        wl_val[e_] = P.dma("gpsimd", w2b[b][:], moe_w2[e_].rearrange("(k p) n -> p k n", p=128), s_wl[b])

    def dispatch(e_):
        base = hcount[0]

        def load(t):
            i = base + t
            if i >= 4:
                P.wait("gpsimd", s_sc[i % 4], sc_hist[i - 4])
            return P.dma("gpsimd", h2t[i % 4][:], H2D[t * 128:(t + 1) * 128, :], s_hl[i % 4])

        lv = {}
        lv[0] = load(0)
        lv[1] = load(1)
        for t in range(32):
            i = base + t
            hb = h2t[i % 4]
            P.wait("gpsimd", s_hl[i % 4], lv[t])
            P.op("gpsimd", lambda e, e_=e_, t=t, hb=hb: e.indirect_dma_start(out=XE[e_], out_offset=bass.IndirectOffsetOnAxis(ap=slotidx[:, e_, t:t + 1], axis=0), in_=hb[:, :], in_offset=None, bounds_check=breg["r"], oob_is_err=False), s_sc[i % 4], 16)
            sc_hist.append(s_sc[i % 4].n)
            if t + 2 < 32:
                lv[t + 2] = load(t + 2)
        hcount[0] += 32
        sc_done[e_] = [s_sc[k].n for k in range(4)]

    alt2 = [0]

    def nxt():
        alt2[0] += 1
        return "scalar" if alt2[0] % 2 else "vector"

    xcount = [0]
    x_hist = []
    ycount = [0]
    y_hist = []
    breg = {}

    def _mk_reg(e):
        breg["r"] = e.alloc_register("bchk")
        e.reg_mov(breg["r"], CPAD - 1)

    P.op("gpsimd", _mk_reg)
    NST_ = CPAD // 512
    vxs = {}

    def issue_xload(T):
        ee, st_ = T // NST_, T % NST_
        xb_ = xtok[T % 2]
        if st_ == 0:
            for k in range(4):
                P.wait("sync", s_sc[k], sc_done[ee][k])
        if T >= 2:
            a_, v_ = x_hist[T - 2]
            P.wait("sync", ring.free["scalar"], a_)
            P.wait("sync", ring.free["vector"], v_)
        vxs[T] = P.dma("sync", xb_[:], XE[ee][st_ * 512:(st_ + 1) * 512, :].rearrange("(s p) d -> p s d", p=128), s_xl[T % 2])

    load_w(0)
    dispatch(0)
    dispatch(1)
    issue_xload(0)
    for e_ in range(NE):
        b = e_ % 2
        if e_ + 1 < NE:
            load_w(e_ + 1)
        if e_ + 2 < NE:
            dispatch(e_ + 2)
        P.wait("tensor", s_wl[b], wl_val[e_])
        for st in range(NST_):
            xi = xcount[0]
            xcount[0] += 1
            xb = xtok[xi % 2]
            P.wait("tensor", s_xl[xi % 2], vxs[xi])
            xT_ = xeT[xi % 2]
            for c in range(8):
                it, bank = ring.acquire()
                pb = psb[(it) % 8]
                for s4 in range(4):
                    fn = lambda e, c=c, s4=s4, pb=pb, xb=xb: e.transpose(out=pb[:, s4 * 128:(s4 + 1) * 128], in_=xb[:, s4, c * 128:(c + 1) * 128], identity=ident_b[:])
                    if s4 == 3:
                        ring.produced(fn)
                    else:
                        P.op("tensor", fn)
                eng = nxt()
                ring.consume_wait(eng, it)
                if eng == "scalar":
                    vt = ring.release(eng, [it], lambda e, c=c, pb=pb, xT_=xT_: e.activation(out=xT_[:, c, :], in_=pb[:, 0:512], func=AF.Copy))
                else:
                    vt = ring.release(eng, [it], lambda e, c=c, pb=pb, xT_=xT_: e.tensor_copy(out=xT_[:, c, :], in_=pb[:, 0:512]))
            x_hist.append((ring.free["scalar"].n, ring.free["vector"].n))
            if xi + 1 < NE * NST_:
                issue_xload(xi + 1)
            P.wait("tensor", ring.free["scalar"], ring.free["scalar"].n)
            P.wait("tensor", ring.free["vector"], ring.free["vector"].n)
            for f in range(8):
                itA, bA = ring.acquire()
                for c in range(8):
                    fn = lambda e, c=c, f=f, bA=bA, b=b, xT_=xT_: e.matmul(bA[:, :], lhsT=w1b[b][:, c, f * 128:(f + 1) * 128], rhs=xT_[:, c, :], start=(c == 0), stop=(c == 7))
                    if c == 7:
                        ring.produced(fn)
                    else:
                        P.op("tensor", fn)
                itB, bB = ring.acquire()
                for c in range(8):
                    fn = lambda e, c=c, f=f, bB=bB, b=b, xT_=xT_: e.matmul(bB[:, :], lhsT=w3b[b][:, c, f * 128:(f + 1) * 128], rhs=xT_[:, c, :], start=(c == 0), stop=(c == 7))
                    if c == 7:
                        ring.produced(fn)
                    else:
                        P.op("tensor", fn)
                sg = sgt[f % 2]
                ring.consume_wait("scalar", itA)
                if f >= 2:
                    P.wait("scalar", ring.free["vector"], sg_free[f % 2])
                else:
                    if f == 0:
                        sg_free = {}
                    if (e_, st) != (0, 0):
                        P.wait("scalar", ring.free["vector"], prev_sg_free[f % 2])
                va = ring.release("scalar", [itA], lambda e, bA=bA, sg=sg: e.activation(out=sg[:], in_=bA[:, :], func=AF.Silu))
                ring.consume_wait("vector", itB)
                P.wait("vector", ring.free["scalar"], va)
                sg_free[f % 2] = ring.release("vector", [itB], lambda e, bB=bB, sg=sg, f=f: e.tensor_tensor(out=hid[:, f, :], in0=sg[:], in1=bB[:, :], op=ALU.mult))
            prev_sg_free = dict(sg_free)
            P.wait("tensor", ring.free["vector"], ring.free["vector"].n)
            for s4 in range(4):
                yi = ycount[0]
                ycount[0] += 1
                yb = yst[yi % 2]
                for nh in range(2):
                    it, bank = ring.acquire()
                    for f in range(8):
                        fn = lambda e, f=f, s4=s4, nh=nh, bank=bank, b=b: e.matmul(bank[:, :], lhsT=hid[:, f, s4 * 128:(s4 + 1) * 128], rhs=w2b[b][:, f, nh * 512:(nh + 1) * 512], start=(f == 0), stop=(f == 7))
                        if f == 7:
                            ring.produced(fn)
                        else:
                            P.op("tensor", fn)
                    eng = "scalar" if nh == 0 else "vector"
                    ring.consume_wait(eng, it)
                    if nh == 0 and yi >= 2:
                        P.wait("scalar", s_yo[yi % 2], y_hist[yi - 2])
                    if nh == 1 and yi >= 2:
                        P.wait("vector", s_yo[yi % 2], y_hist[yi - 2])
                    if eng == "scalar":
                        vy0 = ring.release(eng, [it], lambda e, bank=bank, yb=yb: e.activation(out=yb[:, 0:512], in_=bank[:, :], func=AF.Copy))
                    else:
                        vy1 = ring.release(eng, [it], lambda e, bank=bank, yb=yb: e.tensor_copy(out=yb[:, 512:1024], in_=bank[:, :]))
                P.wait("sync", ring.free["scalar"], vy0)
                P.wait("sync", ring.free["vector"], vy1)
                r0 = st * 512 + s4 * 128
                y_hist.append(P.dma("sync", YE[e_][r0:r0 + 128, :], yb[:], s_yo[yi % 2]))
        exp_done[e_] = (ring.free["scalar"].n, ring.free["vector"].n)
    for so in s_yo:
        P.wait("sync", so, so.n)
    P.end()
    if stage <= 5:
        top.close()
        return nc

    P.begin()
    tk = Tk("z")
    NG = 6
    gb = [P.sb("gb%d" % i, [128, D], F32) for i in range(NG)]
    acc = [P.sb("acc%d" % i, [128, D], F32) for i in range(2)]
    x1l = [P.sb("x1l%d" % i, [128, D], F32) for i in range(2)]
    lnr2 = P.sb("lnr2", [128, 2, D], F32)
    bst2 = P.sb("bst2", [128, 2, 6], F32)
    spc = P.sb("spc", [128, 512], F32)
    junk2 = P.sb("junk2", [128, D], F32)
    s12 = P.sb("s12", [128, 2], F32)
    msq = P.sb("msq", [128, 1], F32)
    mv2 = P.sb("mv2", [128, 2], F32)
    sd2 = P.sb("sd2", [128, 1], F32)
    rs2 = P.sb("rs2", [128, 1], F32)
    s_g = [P.sem("zg%d" % i) for i in range(NG)]
    s_x1 = [P.sem("zx0"), P.sem("zx1")]
    s_oo = [P.sem("zo0"), P.sem("zo1")]
    s_l = P.sem("zl")
    v_l = P.dma("sync", lnr2[:], lnrows[:, 2:4, :], s_l)
    breg2 = {}

    def _mk_reg2(e):
        breg2["r"] = e.alloc_register("bchk2")
        e.reg_mov(breg2["r"], CPAD - 1)

    P.op("gpsimd", _mk_reg2)
    for i in range(NG):
        P.op("gpsimd", lambda e, i=i: e.memset(gb[i][:], 0.0))
    P.wait("vector", s_l, v_l)
    gcount = 0
    g_hist = []
    gfree = []
    o_hist = []
    accfree = {}
    NGA = 32 * NE
    vgs = {}
    vx1s = {}

    def issue(i):
        t, e_ = (i // NE, i % NE) if i < NGA else (0, 0)
        g = gb[i % NG]
        if i >= NG:
            P.wait("gpsimd", tk.s["vector"], gfree[i - NG])
        vgs[i] = P.op("gpsimd", lambda e, e_=e_, t=t, g=g: e.indirect_dma_start(out=g[:, :], out_offset=None, in_=YE[e_], in_offset=bass.IndirectOffsetOnAxis(ap=slotidx[:, e_, t:t + 1], axis=0), bounds_check=breg2["r"], oob_is_err=False), s_g[i % NG], 16)

    def consume(i):
        t, e_ = i // NE, i % NE
        a = acc[t % 2]
        g = gb[i % NG]
        if e_ == 0:
            xl = x1l[t % 2]
            if t >= 2:
                P.wait("sync", tk.s["vector"], accfree[t - 2])
            vx1s[t] = P.dma("sync", xl[:], X1[t * 128:(t + 1) * 128, :], s_x1[t % 2])
        for k in (i, i + 1, i + 2):
            P.wait("vector", s_g[k % NG], vgs[k])
        if e_ == 0:
            if t >= 2:
                P.wait("vector", s_oo[t % 2], o_hist[t - 2])
            tg = tk.do("vector", lambda e, g=g, a=a, e_=e_, t=t: e.tensor_scalar(out=a[:], in0=g[:], scalar1=gmask[:, e_, t:t + 1], scalar2=None, op0=ALU.mult))
        else:
            tg = tk.do("vector", lambda e, g=g, a=a, e_=e_, t=t: e.scalar_tensor_tensor(out=a[:], in0=g[:], scalar=gmask[:, e_, t:t + 1], in1=a[:], op0=ALU.mult, op1=ALU.add))
        gfree.append(tg[1])

    def finish_tile(t):
        a = acc[t % 2]
        xl = x1l[t % 2]
        vx1 = vx1s[t]
        P.wait("vector", s_x1[t % 2], vx1)
        P.op("vector", lambda e, a=a: e.tensor_tensor(out=a[:], in0=a[:], in1=modrow[:, 3072:4096], op=ALU.mult))
        P.op("vector", lambda e, a=a, xl=xl: e.scalar_tensor_tensor(out=a[:], in0=xl[:], scalar=ALPHA, in1=a[:], op0=ALU.mult, op1=ALU.add))
        tv = tk.do("vector", lambda e, a=a: e.tensor_copy(out=spc[:], in_=a[:, 0:512]))
        P.wait("scalar", tk.s["vector"], tv[1])
        P.op("scalar", lambda e, a=a: e.activation(out=junk2[:], in_=a[:], func=AF.Copy, accum_out=s12[:, 0:1]))
        ts = tk.do("scalar", lambda e, a=a: e.activation(out=junk2[:], in_=a[:], func=AF.Square, accum_out=s12[:, 1:2]))
        P.wait("vector", tk.s["scalar"], ts[1])
        P.opf("vector", lambda e: e.tensor_scalar(out=mv2[:, 0:1], in0=s12[:, 0:1], scalar1=1.0 / D, scalar2=None, op0=ALU.mult))
        P.opf("vector", lambda e: e.tensor_tensor(out=msq[:], in0=mv2[:, 0:1], in1=mv2[:, 0:1], op=ALU.mult))
        t3 = tk.do("vector", lambda e: e.scalar_tensor_tensor(out=mv2[:, 1:2], in0=s12[:, 1:2], scalar=1.0 / D, in1=msq[:], op0=ALU.mult, op1=ALU.subtract))
        t4 = tk.do("scalar", lambda e: e.activation(out=sd2[:], in_=mv2[:, 1:2], func=AF.Sqrt, bias=eps_t[:, 0:1], scale=1.0), [t3])
        P.wait("vector", tk.s["scalar"], t4[1])
        P.opf("vector", lambda e: e.reciprocal(out=rs2[:], in_=sd2[:]))
        P.op("vector", lambda e, a=a: e.tensor_scalar(out=a[:], in0=a[:], scalar1=mv2[:, 0:1], scalar2=rs2[:, 0:1], op0=ALU.subtract, op1=ALU.mult))
        P.op("vector", lambda e, a=a: e.tensor_tensor(out=a[:], in0=a[:], in1=lnr2[:, 0, :], op=ALU.mult))
        t5 = tk.do("vector", lambda e, a=a: e.tensor_tensor(out=a[:], in0=a[:], in1=lnr2[:, 1, :], op=ALU.add))
        accfree[t] = t5[1]
        P.wait("sync", tk.s["vector"], t5[1])
        o_hist.append(P.dma("sync", out[t * 128:(t + 1) * 128, :], a[:], s_oo[t % 2]))
    for i in range(NGA + 2):
        issue(i)
        if i >= 2:
            consume(i - 2)
            if (i - 2) % NE == NE - 1:
                finish_tile((i - 2) // NE)
    for so in s_oo:
        P.wait("sync", so, so.n)
    P.end()
    top.close()
    return nc


def _rope_tables(tok_idx):
    row = (tok_idx // 64).astype(np.float32)
    col = (tok_idx % 64).astype(np.float32)

    def tab(h):
        half = h // 2
        inv = (10000.0 ** (-(np.arange(half, dtype=np.float32) * 2.0 / h))).astype(np.float32)
        ar = (row[None, :] * inv[:, None]).astype(np.float32)
        ac = (col[None, :] * inv[:, None]).astype(np.float32)
        c = np.concatenate([np.cos(ar), np.cos(ar), np.cos(ac), np.cos(ac)], 0)
        s = np.concatenate([-np.sin(ar), np.sin(ar), -np.sin(ac), np.sin(ac)], 0)
        return c.astype(np.float32), s.astype(np.float32)

    c64, s64 = tab(32)
    c32, s32 = tab(16)
    return (np.tile(c64, (2, 1)), np.tile(s64, (2, 1)), np.tile(c32, (4, 1)), np.tile(s32, (4, 1)))


def _perm(d):
    q = d // 4
    return np.concatenate([np.arange(q, 2 * q), np.arange(0, q), np.arange(3 * q, 4 * q), np.arange(2 * q, 3 * q)])


def _prep(inp):
    f = lambda a: np.ascontiguousarray(np.asarray(a, dtype=np.float32))
    x, c, ctx, c_ctx = f(inp["x"]), f(inp["c"]), f(inp["ctx"]), f(inp["c_ctx"])
    w_in = f(inp["w_in"])[0]
    p64 = _perm(64)
    p32 = _perm(32)
    w_all = np.zeros((D, NW), np.float32)
    w_all[:, O_CQ:O_CQ + 256] = w_in[:, 0:256]
    w_all[:, O_CKV:O_CKV + 128] = w_in[:, 256:384]
    w_all[:, O_KR:O_KR + 32] = w_in[:, 384:416]
    w_all[:, O_KRP:O_KRP + 32] = w_in[:, 384:416][:, p32]
    dq = w_in[:, 416:928]
    dk = w_in[:, 928:1440]
    pall = np.concatenate([g * 64 + p64 for g in range(8)])
    w_all[:, O_DQ:O_DQ + 512] = dq
    w_all[:, O_DQP:O_DQP + 512] = dq[:, pall]
    w_all[:, O_DK:O_DK + 512] = dk
    w_all[:, O_DKP:O_DKP + 512] = dk[:, pall]
    w_all[:, O_DV:O_DV + 512] = w_in[:, 1440:1952]
    w_all[:, O_G:O_G + 2048] = w_in[:, 1952:4000]
    wuq = f(inp["mla_w_uq"])[0].reshape(256, 8, 96)
    w_uq_all = np.zeros((256, 1024), np.float32)
    w_uq_all[:, 0:512] = wuq[:, :, 0:64].reshape(256, 512)
    w_uq_all[:, 512:768] = wuq[:, :, 64:96].reshape(256, 256)
    w_uq_all[:, 768:1024] = wuq[:, :, 64:96][:, :, p32].reshape(256, 256)
    wukv = f(inp["mla_w_ukv"])[0].reshape(128, 8, 128)
    w_ukv_all = np.concatenate([wukv[:, :, 0:64].reshape(128, 512), wukv[:, :, 64:128].reshape(128, 512)], 1)
    b_ada = f(inp["b_ada"])[0]
    common = {
        "w_ada": f(inp["w_ada"])[0],
        "b_ada_fm": np.ascontiguousarray(b_ada[0:2048].reshape(16, 128).T),
        "b_ada_row": b_ada.reshape(1, 6 * D),
        "w_all": w_all,
        "w_uq_all": w_uq_all,
        "w_ukv_all": np.ascontiguousarray(w_ukv_all),
        "g_q_fm": np.ascontiguousarray(f(inp["mla_g_q"])[0].reshape(2, 128).T),
        "g_kv_fm": f(inp["mla_g_kv"])[0].reshape(128, 1),
        "w_o_mla": f(inp["mla_w_o"])[0],
        "w_o_diff": f(inp["diff_w_o"])[0],
        "w_out": f(inp["w_out"])[0],
        "dlam": np.ascontiguousarray(np.broadcast_to(f(inp["diff_lambda"])[0].reshape(1, 256), (128, 256))),
        "g_sub_fm": f(inp["diff_g_subln"])[0].reshape(128, 1),
        "lnrows": np.ascontiguousarray(np.broadcast_to(np.stack([f(inp["ln1_g"])[0], f(inp["ln1_b"])[0], f(inp["ln2_g"])[0], f(inp["ln2_b"])[0]], 0)[None], (128, 4, D))),
        "w_router": f(inp["moe_w_router"])[0],
        "b_router": f(inp["moe_b_router"])[0].reshape(1, NE),
        "moe_w1": f(inp["moe_w1"])[0],
        "moe_w3": f(inp["moe_w3"])[0],
        "moe_w2": f(inp["moe_w2"])[0],
    }
    maps = []
    for core in range(8):
        b, hf = core // 2, core % 2
        own = np.arange(hf * NQ, (hf + 1) * NQ)
        oth = np.arange((1 - hf) * NQ, (2 - hf) * NQ)
        order = np.concatenate([own, oth])
        c64, s64, c32, s32 = _rope_tables(order)
        m = dict(common)
        m["xT"] = np.ascontiguousarray(x[b][order].T)
        m["xown"] = np.ascontiguousarray(x[b][own])
        m["ctxT"] = np.ascontiguousarray(ctx[b].T)
        cv = np.stack([c[b].reshape(8, 128).T, c_ctx.reshape(8, 128).T], -1)
        m["cvec"] = np.ascontiguousarray(cv)
        m["rope64_c"], m["rope64_s"], m["rope32_c"], m["rope32_s"] = c64, s64, c32, s32
        maps.append(m)
    return maps


_NC_CACHE = {}


def kernel(**inputs):
    maps = _prep(inputs)
    if "nc" not in _NC_CACHE:
        _NC_CACHE["nc"] = build()
    res = run_bass_kernel_spmd(_NC_CACHE["nc"], maps, core_ids=list(range(8)))
    outp = np.zeros((4, SEQ, D), np.float32)
    for core in range(8):
        b, hf = core // 2, core % 2
        outp[b, hf * NQ:(hf + 1) * NQ] = res.results[core]["out"]
    return outp
```

```python
import contextlib
import numpy as np
import concourse.bass as bass
import concourse.mybir as mybir
from concourse.bass_utils import run_bass_kernel_spmd

F32 = mybir.dt.float32
BF16 = mybir.dt.bfloat16
U32 = mybir.dt.uint32
I32 = mybir.dt.int32
AF = mybir.ActivationFunctionType
ALU = mybir.AluOpType
AX = mybir.AxisListType

D = 1024
SEQ = 8192
NQ = 4096
CTX = 256
NK = CTX + SEQ
NKT = NK // 128
EPS = 1e-6
MLA_SCALE = 96.0 ** -0.5
DIFF_SCALE = 64.0 ** -0.5
ALPHA = 2.0 ** 0.25
NE = 16
CPAD = 1024
NITER = 28

O_CQ, O_CKV, O_KR, O_KRP, O_DQ, O_DQP, O_DK, O_DKP, O_DV, O_G = 0, 256, 384, 416, 448, 960, 1472, 1984, 2496, 3008
NW = 5056
ENGS = ["sync", "scalar", "vector", "gpsimd", "tensor"]


class Sem:
    def __init__(self, h):
        self.h = h
        self.n = 0


class Prog:
    def __init__(self, nc):
        self.nc = nc
        self.q = {e: [] for e in ENGS}
        self.es = None

    def begin(self):
        self.es = contextlib.ExitStack()
        self.q = {e: [] for e in ENGS}
        self.fs = {}
        self.nphase = getattr(self, "nphase", 0) + 1

    def sem(self, name):
        return Sem(self.es.enter_context(self.nc.semaphore(name)))

    def sb(self, name, shape, dt):
        return self.es.enter_context(self.nc.sbuf_tensor(name, shape, dt))

    def op(self, eng, fn, sem=None, inc=1):
        if sem is None:
            self.q[eng].append(fn)
            return None
        h = sem.h
        self.q[eng].append(lambda e, fn=fn, h=h, inc=inc: fn(e).then_inc(h, inc))
        sem.n += inc
        return sem.n

    def opf(self, eng, fn):
        if self.fs.get(eng) is None:
            self.fs[eng] = self.sem("fence" + eng[:3] + str(self.nphase))
        v = self.op(eng, fn, self.fs[eng], 1)
        self.wait(eng, self.fs[eng], v)
        return v

    def dma(self, eng, out, in_, sem):
        return self.op(eng, lambda e, out=out, in_=in_: e.dma_start(out=out, in_=in_), sem, 16)

    def wait(self, eng, sem, val):
        if val is None or val <= 0:
            return
        h = sem.h
        self.q[eng].append(lambda e, h=h, val=val: e.wait_ge(h, val))

    def end(self):
        with self.nc.Block() as blk:
            for en in ENGS:
                fns = self.q[en]
                if not fns:
                    continue

                def body(e, fns=fns):
                    for f in fns:
                        f(e)

                getattr(blk, en)(body)
        self.es.close()
        self.es = None


class PsRing:
    def __init__(self, P, banks, tag):
        self.P = P
        self.banks = banks
        self.full = P.sem("full" + tag)
        self.free = {"scalar": P.sem("fra" + tag), "vector": P.sem("frv" + tag)}
        self.hist = []
        self.it = 0

    def acquire(self):
        it = self.it
        self.it += 1
        n = len(self.banks)
        if it >= n:
            eng, val = self.hist[it - n]
            self.P.wait("tensor", self.free[eng], val)
        self.hist.append(None)
        return it, self.banks[it % n]

    def produced(self, fn):
        return self.P.op("tensor", fn, self.full, 1)

    def consume_wait(self, eng, it):
        self.P.wait(eng, self.full, it + 1)

    def release(self, eng, its, fn):
        v = self.P.op(eng, fn, self.free[eng], 1)
        for it in its:
            self.hist[it] = (eng, v)
        return v


def build(debug=None, stage=99):
    nc = bass.Bass("TRN2", target_bir_lowering=False)
    dbg = set(debug or [])

    def din(name, shape, dt=F32):
        return nc.dram_tensor(name, list(shape), dt, kind="ExternalInput").ap()

    def scratch(name, shape, dt):
        if name in dbg:
            return nc.dram_tensor(name, list(shape), dt, kind="ExternalOutput").ap()
        return nc.dram_tensor(name, list(shape), dt).ap()

    xT = din("xT", [D, SEQ])
    xown = din("xown", [NQ, D])
    ctxT = din("ctxT", [D, CTX])
    cvec = din("cvec", [128, 8, 2])
    w_ada = din("w_ada", [D, 6 * D])
    b_ada_fm = din("b_ada_fm", [128, 16])
    b_ada_row = din("b_ada_row", [1, 6 * D])
    w_all = din("w_all", [D, NW])
    w_uq_all = din("w_uq_all", [256, 1024])
    w_ukv_all = din("w_ukv_all", [128, 1024])
    g_q_fm = din("g_q_fm", [128, 2])
    g_kv_fm = din("g_kv_fm", [128, 1])
    w_o_mla = din("w_o_mla", [512, D])
    w_o_diff = din("w_o_diff", [512, D])
    w_out = din("w_out", [D, D])
    dlam = din("dlam", [128, 256])
    g_sub_fm = din("g_sub_fm", [128, 1])
    lnrows = din("lnrows", [128, 4, D])
    w_router = din("w_router", [D, NE])
    b_router = din("b_router", [1, NE])
    moe_w1 = din("moe_w1", [NE, D, D])
    moe_w3 = din("moe_w3", [NE, D, D])
    moe_w2 = din("moe_w2", [NE, D, D])
    rope64_c = din("rope64_c", [128, SEQ])
    rope64_s = din("rope64_s", [128, SEQ])
    rope32_c = din("rope32_c", [128, SEQ])
    rope32_s = din("rope32_s", [128, SEQ])
    out = nc.dram_tensor("out", [NQ, D], F32, kind="ExternalOutput").ap()

    KTm = scratch("KTm", [8, 64, NK], BF16)
    KR = scratch("KR", [32, NK], BF16)
    Vm = scratch("Vm", [NK, 8, 65], BF16)
    QTm = scratch("QTm", [8, 96, NQ], BF16)
    KTd = scratch("KTd", [4, 128, NK], BF16)
    Vd = scratch("Vd", [NK, 512], BF16)
    QTd = scratch("QTd", [4, 128, NQ], BF16)
    GT = scratch("GT", [16, 128, NQ], BF16)
    OTm = scratch("OTm", [8, 64, NQ], BF16)
    OTd = scratch("OTd", [4, 128, NQ], F32)
    X1 = scratch("X1", [NQ, D], F32)
    AFF_IN = nc.dram_tensor("AFF_IN", [128, 32 * NE], F32)
    AFF_OUT = nc.dram_tensor("AFF_OUT", [256, 32 * NE], F32)
    XE = [scratch("XE%d" % i, [CPAD, D], BF16) for i in range(NE)]
    YE = [scratch("YE%d" % i, [CPAD, D], F32) for i in range(NE)]
    DBGT = scratch("DBGT", [128, 4096], F32)

    P = Prog(nc)
    top = contextlib.ExitStack()

    def gsb(name, shape, dt):
        return top.enter_context(nc.sbuf_tensor(name, shape, dt))

    modrow = gsb("modrow", [128, 4096], F32)
    modfm = gsb("modfm", [128, 16, 2], F32)
    ones_f = gsb("ones_f", [128, 128], F32)
    ones_b = gsb("ones_b", [128, 128], BF16)
    ident_f = gsb("ident_f", [128, 128], F32)
    ident_b = gsb("ident_b", [128, 128], BF16)
    neglam = gsb("neglam", [128, 1], F32)
    gsub = gsb("gsub", [128, 1], F32)
    eps_t = gsb("eps_t", [128, 1], F32)
    psall = top.enter_context(nc.psum_tensor("psall", [128, 8, 512], F32))
    psum = [psall[:, i, :] for i in range(8)]

    P.begin()
    s_ld = P.sem("p0ld")
    s_a = P.sem("p0a")
    s_v = P.sem("p0v")
    s_g = P.sem("p0g")
    s_pe = P.sem("p0pe")
    s_wfb = [P.sem("p0wf0"), P.sem("p0wf1")]
    cv = P.sb("cv", [128, 8, 2], F32)
    sv = P.sb("sv", [128, 8, 2], F32)
    srep = P.sb("srep", [128, 8, 128], F32)
    zer = P.sb("zer", [128, 128], F32)
    wch = [P.sb("wch%d" % i, [128, 8, 512], F32) for i in range(2)]
    brow = P.sb("brow", [1, 6 * D], F32)
    bfm = P.sb("bfm", [128, 16], F32)
    dl = P.sb("dl", [128, 256], F32)
    dlp = P.sb("dlp", [128, 128], F32)
    lsum = P.sb("lsum", [128, 2], F32)
    iot = P.sb("iot", [128, 128], F32)
    iop = P.sb("iop", [128, 1], F32)

    P.dma("sync", cv[:], cvec, s_ld)
    P.dma("sync", brow[:], b_ada_row, s_ld)
    P.dma("sync", bfm[:], b_ada_fm, s_ld)
    P.dma("sync", dl[:], dlam, s_ld)
    v_ld0 = P.dma("sync", gsub[:], g_sub_fm, s_ld)
    P.op("gpsimd", lambda e: e.memset(ones_f[:], 1.0))
    P.op("gpsimd", lambda e: e.memset(ones_b[:], 1.0))
    P.op("gpsimd", lambda e: e.memset(zer[:], 0.0))
    P.op("gpsimd", lambda e: e.memset(eps_t[:], EPS))
    P.op("gpsimd", lambda e: e.iota(iot[:], [[1, 128]], base=0, channel_multiplier=0, allow_small_or_imprecise_dtypes=True))
    P.opf("gpsimd", lambda e: e.iota(iop[:], [[0, 1]], base=0, channel_multiplier=1, allow_small_or_imprecise_dtypes=True))
    P.opf("gpsimd", lambda e: e.tensor_scalar(out=ident_f[:], in0=iot[:], scalar1=iop[:, 0:1], scalar2=None, op0=ALU.is_equal))
    v_g0 = P.op("gpsimd", lambda e: e.tensor_copy(out=ident_b[:], in_=ident_f[:]), s_g, 1)
    P.wait("scalar", s_ld, v_ld0)
    P.wait("scalar", s_g, v_g0)
    P.opf("scalar", lambda e: e.activation(out=sv[:], in_=cv[:], func=AF.Silu))
    for k in range(8):
        va = P.op("scalar", lambda e, k=k: e.activation(out=srep[:, k, :], in_=zer[:], func=AF.Identity, bias=sv[:, k, 0:1], scale=1.0), s_a, 1)
    v_srep = va
    P.wait("vector", s_ld, v_ld0)
    P.op("vector", lambda e: e.tensor_tensor(out=dlp[:, 0:64], in0=dl[:, 0:64], in1=dl[:, 64:128], op=ALU.mult))
    P.opf("vector", lambda e: e.tensor_tensor(out=dlp[:, 64:128], in0=dl[:, 128:192], in1=dl[:, 192:256], op=ALU.mult))
    P.op("vector", lambda e: e.tensor_reduce(out=lsum[:, 0:1], in_=dlp[:, 0:64], axis=AX.X, op=ALU.add))
    v_ls = P.op("vector", lambda e: e.tensor_reduce(out=lsum[:, 1:2], in_=dlp[:, 64:128], axis=AX.X, op=ALU.add), s_v, 1)
    P.wait("scalar", s_v, v_ls)
    v_le = P.op("scalar", lambda e: e.activation(out=lsum[:], in_=lsum[:], func=AF.Exp), s_a, 1)
    P.wait("vector", s_a, v_le)
    P.opf("vector", lambda e: e.tensor_scalar(out=neglam[:], in0=lsum[:, 1:2], scalar1=lsum[:, 0:1], scalar2=-0.2, op0=ALU.subtract, op1=ALU.add))

    wsrc = w_ada.rearrange("(k p) n -> p k n", p=128)
    pe_done = []
    fm_done = []
    row_done = []
    for j in range(12):
        buf = wch[j % 2]
        if j >= 2:
            P.wait("gpsimd", s_v, pe_done[j - 2])
        v_w = P.dma("gpsimd", buf[:], wsrc[:, :, j * 512:(j + 1) * 512], s_wfb[j % 2])
        P.wait("tensor", s_wfb[j % 2], v_w)
        if j == 0:
            P.wait("tensor", s_a, v_srep)
        if j < 4:
            for q in range(4):
                jj = j * 4 + q
                if jj >= 2:
                    P.wait("tensor", s_v, fm_done[jj - 2])
                for k in range(8):
                    fn = lambda e, k=k, q=q, jj=jj, buf=buf: e.matmul(psum[jj % 2][:, 0:2], lhsT=buf[:, k, q * 128:(q + 1) * 128], rhs=sv[:, k, :], start=(k == 0), stop=(k == 7))
                    if k == 7:
                        vp = P.op("tensor", fn, s_pe, 1)
                    else:
                        P.op("tensor", fn)
                P.wait("vector", s_pe, vp)
                if jj == 0:
                    P.wait("vector", s_ld, v_ld0)
                fm_done.append(P.op("vector", lambda e, jj=jj: e.tensor_scalar(out=modfm[:, jj, :], in0=psum[jj % 2][:, 0:2], scalar1=bfm[:, jj:jj + 1], scalar2=(1.0 if jj >= 8 else 0.0), op0=ALU.add, op1=ALU.add), s_v, 1))
            pe_done.append(fm_done[-1])
        else:
            jb = j - 4
            bank = psum[2 + (jb % 2)]
            if jb >= 2:
                P.wait("tensor", s_v, row_done[jb - 2])
            for k in range(8):
                P.op("tensor", lambda e, k=k, buf=buf, bank=bank: e.matmul(bank[:], lhsT=srep[:, k, :], rhs=buf[:, k, :], start=(k == 0), stop=False))
            vp = P.op("tensor", lambda e, j=j, bank=bank: e.matmul(bank[:], lhsT=ones_f[0:1, :], rhs=brow[0:1, j * 512:(j + 1) * 512], start=False, stop=True), s_pe, 1)
            P.wait("vector", s_pe, vp)
            addc = 1.0 if jb in (4, 5) else 0.0
            row_done.append(P.op("vector", lambda e, jb=jb, bank=bank, addc=addc: e.tensor_scalar(out=modrow[:, jb * 512:(jb + 1) * 512], in0=bank[:], scalar1=addc, scalar2=None, op0=ALU.add), s_v, 1))
            pe_done.append(row_done[-1])
    if "DBG0" in dbg:
        dbt = P.sb("dbt", [128, 64], F32)
        P.op("vector", lambda e: e.memset(dbt[:], 0.0))
        P.op("vector", lambda e: e.tensor_copy(out=dbt[:, 0:1], in_=neglam[:]))
        P.op("vector", lambda e: e.tensor_copy(out=dbt[:, 1:3], in_=lsum[:]))
        P.op("vector", lambda e: e.tensor_copy(out=dbt[:, 3:35], in_=modfm[:].rearrange("p a b -> p (a b)")))
        vdd = P.op("vector", lambda e: e.tensor_copy(out=dbt[:, 35:51], in_=dl[:, 0:16]), s_v, 1)
        P.wait("sync", s_v, vdd)
        P.dma("sync", DBGT[:, 64:4096], modrow[:, 64:4096], s_ld)
        P.dma("sync", DBGT[:, 0:64], dbt[:], s_ld)
        P.wait("sync", s_ld, s_ld.n)
    P.end()
    if stage <= 0:
        top.close()
        return nc

    P.begin()
    wall = P.sb("wall", [128, 8, NW], BF16)
    wuq = P.sb("wuq", [128, 2, 1024], BF16)
    wukv = P.sb("wukv", [128, 1024], BF16)
    gq = P.sb("gq", [128, 2], F32)
    gkv = P.sb("gkv", [128, 1], F32)
    xs = [P.sb("xs%d" % i, [128, 8, 512], F32) for i in range(2)]
    hT = [P.sb("hT%d" % i, [128, 8, 512], BF16) for i in range(2)]
    rt = [[P.sb("rt%d_%d" % (i, t), [128, 512], F32) for t in range(4)] for i in range(2)]
    ckv_sb = P.sb("ckv_sb", [128, 512], F32)
    ckv_sq = P.sb("ckv_sq", [128, 512], F32)
    ckvn = P.sb("ckvn", [128, 512], BF16)
    cq_sb = P.sb("cq_sb", [128, 2, 512], F32)
    cq_sq = P.sb("cq_sq", [128, 2, 512], F32)
    cqn = P.sb("cqn", [128, 2, 512], BF16)
    rtmps = [P.sb("rtmp%d" % i, [128, 512], F32) for i in range(4)]
    rt1 = P.sb("rt1", [128, 512], F32)
    rt2 = P.sb("rt2", [128, 512], F32)
    NST = 6
    stg = {"scalar": [P.sb("stga%d" % i, [128, 512], BF16) for i in range(NST)],
           "vector": [P.sb("stgv%d" % i, [128, 512], BF16) for i in range(NST)]}
    vst = [P.sb("vst%d" % i, [128, 8, 65], BF16) for i in range(2)]

    s_w = P.sem("p1w")
    s_xb = [P.sem("p1x0"), P.sem("p1x1")]
    s_h = P.sem("p1h")
    s_hfree = P.sem("p1hf")
    s_xfree = P.sem("p1xf")
    s_rtfree = P.sem("p1rf")
    s_cn = P.sem("p1cn")
    s_cs = P.sem("p1cs")
    s_st = {"scalar": P.sem("p1sta"), "vector": P.sem("p1stv")}
    s_outs = {"scalar": [P.sem("p1oa%d" % i) for i in range(NST)], "vector": [P.sem("p1ov%d" % i) for i in range(NST)]}
    s_vst = P.sem("p1vst")
    s_vouts = [P.sem("p1vo0"), P.sem("p1vo1")]
    ring = PsRing(P, psum, "p1")
    stg_n = {"scalar": 0, "vector": 0}
    stg_hist = {"scalar": [], "vector": []}

    wsrc = w_all.rearrange("(k p) n -> p k n", p=128)
    for c in range(8):
        P.dma("gpsimd", wall[:, :, c * 632:(c + 1) * 632], wsrc[:, :, c * 632:(c + 1) * 632], s_w)
    P.dma("gpsimd", wuq[:], w_uq_all.rearrange("(k p) n -> p k n", p=128), s_w)
    P.dma("gpsimd", wukv[:], w_ukv_all, s_w)
    P.dma("gpsimd", gq[:], g_q_fm, s_w)
    v_w = P.dma("gpsimd", gkv[:], g_kv_fm, s_w)
    P.op("gpsimd", lambda e: e.memset(vst[0][:, :, 64:65], 1.0))
    v_vm = P.op("gpsimd", lambda e: e.memset(vst[1][:, :, 64:65], 1.0), s_cs, 1)
    P.wait("tensor", s_w, v_w)
    P.wait("vector", s_w, v_w)
    P.wait("scalar", s_w, v_w)
    P.wait("scalar", s_cs, v_vm)
    P.wait("vector", s_cs, v_vm)

    def stage_out(eng, its, compute_fn, dst_list):
        i = stg_n[eng]
        stg_n[eng] += 1
        slot = stg[eng][i % NST]
        so = s_outs[eng][i % NST]
        if i >= NST:
            P.wait(eng, so, stg_hist[eng][i - NST])
        ring.release(eng, its, lambda e, slot=slot: compute_fn(e, slot))
        val = ring.free[eng].n
        P.wait("sync", ring.free[eng], val)
        last = None
        for dram_ap, sl in dst_list:
            last = P.dma("sync", dram_ap, sl(slot), so)
        stg_hist[eng].append(last)

    vst_n = [0]
    vst_hist = []
    alt = [0]

    def next_eng():
        alt[0] += 1
        return "scalar" if alt[0] % 2 else "vector"

    hfree_hist = []
    xfree_hist = []
    rtfree_hist = []
    for tt in range(17):
        N = 256 if tt == 0 else 512
        own = 1 <= tt <= 8
        isctx = tt == 0
        kcol = 0 if isctx else 256 + (tt - 1) * 512
        qcol = (tt - 1) * 512
        b = tt % 2
        mi = 1 if isctx else 0
        if tt >= 2:
            P.wait("gpsimd", s_h, xfree_hist[tt - 2])
        src = (ctxT if isctx else xT[:, qcol:qcol + 512]).rearrange("(k p) n -> p k n", p=128)
        s_x = s_xb[b]
        v_x = P.dma("gpsimd", xs[b][:, :, 0:N], src, s_x)
        if not isctx:
            if tt >= 3:
                P.wait("gpsimd", ring.free["vector"], rtfree_hist[tt - 3])
            for ti, tab in enumerate([rope64_c, rope64_s, rope32_c, rope32_s]):
                v_x = P.dma("gpsimd", rt[b][ti][:], tab[:, qcol:qcol + 512], s_x)
        P.wait("vector", s_x, v_x)
        if tt >= 2:
            P.wait("vector", ring.free["scalar"], hfree_hist[tt - 2])
        for k in range(8):
            fn = lambda e, k=k, b=b, N=N, mi=mi: e.tensor_scalar(out=hT[b][:, k, 0:N], in0=xs[b][:, k, 0:N], scalar1=modfm[:, 8 + k, mi:mi + 1], scalar2=modfm[:, k, mi:mi + 1], op0=ALU.mult, op1=ALU.add)
            if k == 7:
                v_h = P.op("vector", fn, s_h, 1)
            else:
                P.op("vector", fn)
        xfree_hist.append(v_h)
        P.wait("tensor", s_h, v_h)
        H = hT[b]

        def mm_full(col0, M, N=N, H=H):
            it, bank = ring.acquire()
            for k in range(8):
                fn = lambda e, k=k, bank=bank: e.matmul(bank[0:M, 0:N], lhsT=wall[:, k, col0:col0 + M], rhs=H[:, k, 0:N], start=(k == 0), stop=(k == 7))
                if k == 7:
                    ring.produced(fn)
                else:
                    P.op("tensor", fn)
            return it, bank

        it_ckv, bk_ckv = mm_full(O_CKV, 128)
        ring.consume_wait("scalar", it_ckv)
        P.op("scalar", lambda e, bk=bk_ckv, N=N: e.activation(out=ckv_sb[:, 0:N], in_=bk[:, 0:N], func=AF.Copy))
        v_sq = ring.release("scalar", [it_ckv], lambda e, bk=bk_ckv, N=N: e.activation(out=ckv_sq[:, 0:N], in_=bk[:, 0:N], func=AF.Square))
        if own:
            its_cq = []
            for c in range(2):
                it, bk = mm_full(O_CQ + c * 128, 128)
                ring.consume_wait("scalar", it)
                P.op("scalar", lambda e, bk=bk, c=c: e.activation(out=cq_sb[:, c, :], in_=bk[:], func=AF.Copy))
                v_sq = ring.release("scalar", [it], lambda e, bk=bk, c=c: e.activation(out=cq_sq[:, c, :], in_=bk[:], func=AF.Square))

        def rope_item(colx, colp, M, tabc, tabs, scale, dsts):
            it1, b1 = mm_full(colx, M)
            if isctx:
                eng = next_eng()
                ring.consume_wait(eng, it1)
                if eng == "scalar":
                    stage_out(eng, [it1], lambda e, slot, b1=b1, N=N: e.activation(out=slot[0:M, 0:N], in_=b1[0:M, 0:N], func=AF.Copy, scale=scale), dsts)
                else:
                    stage_out(eng, [it1], lambda e, slot, b1=b1, N=N: e.tensor_scalar(out=slot[0:M, 0:N], in0=b1[0:M, 0:N], scalar1=scale, scalar2=None, op0=ALU.mult), dsts)
                return
            it2, b2 = mm_full(colp, M)
            ring.consume_wait("vector", it2)
            P.op("vector", lambda e, b1=b1: e.tensor_tensor(out=rt1[0:M, :], in0=b1[0:M, :], in1=tabc[0:M, :], op=ALU.mult))
            P.op("vector", lambda e, b2=b2: e.tensor_tensor(out=rt2[0:M, :], in0=b2[0:M, :], in1=tabs[0:M, :], op=ALU.mult))
            stage_out("vector", [it1, it2], lambda e, slot: e.tensor_tensor(out=slot[0:M, :], in0=rt1[0:M, :], in1=rt2[0:M, :], op=ALU.add), dsts)

        R = rt[b]
        for hd in range(4):
            rope_item(O_DK + hd * 128, O_DKP + hd * 128, 128, R[0], R[1], 1.0,
                      [(KTd[hd, :, kcol:kcol + N], lambda s, N=N: s[:, 0:N])])
        rope_item(O_KR, O_KRP, 32, R[2], R[3], 1.0, [(KR[:, kcol:kcol + N], lambda s, N=N: s[0:32, 0:N])])
        for s4 in range(N // 128):
            it, bank = ring.acquire()
            for k in range(8):
                fn = lambda e, k=k, bank=bank, s4=s4, H=H: e.matmul(bank[:], lhsT=H[:, k, s4 * 128:(s4 + 1) * 128], rhs=wall[:, k, O_DV:O_DV + 512], start=(k == 0), stop=(k == 7))
                if k == 7:
                    ring.produced(fn)
                else:
                    P.op("tensor", fn)
            eng = next_eng()
            ring.consume_wait(eng, it)
            r0 = kcol + s4 * 128
            if eng == "scalar":
                stage_out(eng, [it], lambda e, slot, bank=bank: e.activation(out=slot[:], in_=bank[:], func=AF.Copy), [(Vd[r0:r0 + 128, :], lambda s: s[:])])
            else:
                stage_out(eng, [it], lambda e, slot, bank=bank: e.tensor_copy(out=slot[:], in_=bank[:]), [(Vd[r0:r0 + 128, :], lambda s: s[:])])

        def rms_finish(sq_aps, nin, src_aps, g_ap_fn, dst_aps, rtmp, rtmp2, N=N):
            it, bank = ring.acquire()
            P.wait("tensor", ring.free["scalar"], v_sq)
            for c in range(len(sq_aps)):
                fn = lambda e, c=c, bank=bank: e.matmul(bank[:, 0:N], lhsT=ones_f[:], rhs=sq_aps[c], start=(c == 0), stop=(c == len(sq_aps) - 1))
                if c == len(sq_aps) - 1:
                    ring.produced(fn)
                else:
                    P.op("tensor", fn)
            ring.consume_wait("scalar", it)
            v = ring.release("scalar", [it], lambda e, bank=bank: e.activation(out=rtmp[:, 0:N], in_=bank[:, 0:N], func=AF.Sqrt, bias=eps_t[:, 0:1], scale=1.0 / nin))
            P.wait("vector", ring.free["scalar"], v)
            P.op("vector", lambda e: e.reciprocal(out=rtmp2[:, 0:N], in_=rtmp[:, 0:N]))
            for c in range(len(src_aps)):
                fn = lambda e, c=c: e.scalar_tensor_tensor(out=dst_aps[c], in0=src_aps[c], scalar=g_ap_fn(c), in1=rtmp2[:, 0:N], op0=ALU.mult, op1=ALU.mult)
                if c == len(src_aps) - 1:
                    vv = P.op("vector", fn, s_cn, 1)
                else:
                    P.op("vector", fn)
            return vv

        v_ckvn = rms_finish([ckv_sq[:, 0:N]], 128.0, [ckv_sb[:, 0:N]], lambda c: gkv[:, 0:1], [ckvn[:, 0:N]], rtmps[0], rtmps[1])
        if own:
            v_cqn = rms_finish([cq_sq[:, 0, :], cq_sq[:, 1, :]], 256.0, [cq_sb[:, 0, :], cq_sb[:, 1, :]], lambda c: gq[:, c:c + 1], [cqn[:, 0, :], cqn[:, 1, :]], rtmps[2], rtmps[3])
            for hd in range(4):
                rope_item_q = None
                it1, b1 = mm_full(O_DQ + hd * 128, 128)
                it2, b2 = mm_full(O_DQP + hd * 128, 128)
                ring.consume_wait("vector", it2)
                P.op("vector", lambda e, b1=b1, R=R: e.tensor_tensor(out=rt1[:], in0=b1[:], in1=R[0][:], op=ALU.mult))
                P.op("vector", lambda e, b2=b2, R=R: e.tensor_tensor(out=rt2[:], in0=b2[:], in1=R[1][:], op=ALU.mult))
                P.op("vector", lambda e: e.tensor_tensor(out=rt1[:], in0=rt1[:], in1=rt2[:], op=ALU.add))
                stage_out("vector", [it1, it2], lambda e, slot: e.tensor_scalar(out=slot[:], in0=rt1[:], scalar1=DIFF_SCALE, scalar2=None, op0=ALU.mult),
                          [(QTd[hd, :, qcol:qcol + 512], lambda s: s[:])])
            for gc in range(16):
                it, bank = mm_full(O_G + gc * 128, 128)
                ring.consume_wait("scalar", it)
                stage_out("scalar", [it], lambda e, slot, bank=bank: e.activation(out=slot[:], in_=bank[:], func=AF.Sigmoid),
                          [(GT[gc, :, qcol:qcol + 512], lambda s: s[:])])
        P.wait("tensor", s_cn, v_ckvn)
        for j in range(4):
            it, bank = ring.acquire()
            ring.produced(lambda e, j=j, bank=bank, N=N: e.matmul(bank[:, 0:N], lhsT=wukv[:, j * 128:(j + 1) * 128], rhs=ckvn[:, 0:N], start=True, stop=True))
            eng = next_eng()
            ring.consume_wait(eng, it)
            dsts = [(KTm[2 * j, :, kcol:kcol + N], lambda s, N=N: s[0:64, 0:N]), (KTm[2 * j + 1, :, kcol:kcol + N], lambda s, N=N: s[64:128, 0:N])]
            if eng == "scalar":
                stage_out(eng, [it], lambda e, slot, bank=bank, N=N: e.activation(out=slot[:, 0:N], in_=bank[:, 0:N], func=AF.Copy), dsts)
            else:
                stage_out(eng, [it], lambda e, slot, bank=bank, N=N: e.tensor_copy(out=slot[:, 0:N], in_=bank[:, 0:N]), dsts)
        for s4 in range(N // 128):
            it, bank = ring.acquire()
            ring.produced(lambda e, s4=s4, bank=bank: e.matmul(bank[:], lhsT=ckvn[:, s4 * 128:(s4 + 1) * 128], rhs=wukv[:, 512:1024], start=True, stop=True))
            i = vst_n[0]
            vst_n[0] += 1
            vs = vst[i % 2]
            ring.consume_wait("vector", it)
            s_vout = s_vouts[i % 2]
            if i >= 2:
                P.wait("vector", s_vout, vst_hist[i - 2])
            v = ring.release("vector", [it], lambda e, vs=vs, bank=bank: e.tensor_copy(out=vs[:, :, 0:64], in_=bank[:].rearrange("p (h d) -> p h d", h=8)))
            P.wait("sync", ring.free["vector"], v)
            r0 = kcol + s4 * 128
            vst_hist.append(P.dma("sync", Vm[r0:r0 + 128, :, :], vs[:], s_vout))
        if own:
            P.wait("tensor", s_cn, v_cqn)
            for j in range(4):
                it, bank = ring.acquire()
                P.op("tensor", lambda e, j=j, bank=bank: e.matmul(bank[:], lhsT=wuq[:, 0, j * 128:(j + 1) * 128], rhs=cqn[:, 0, :], start=True, stop=False))
                ring.produced(lambda e, j=j, bank=bank: e.matmul(bank[:], lhsT=wuq[:, 1, j * 128:(j + 1) * 128], rhs=cqn[:, 1, :], start=False, stop=True))
                ring.consume_wait("scalar", it)
                dsts = [(QTm[2 * j, 0:64, qcol:qcol + 512], lambda s: s[0:64, :]), (QTm[2 * j + 1, 0:64, qcol:qcol + 512], lambda s: s[64:128, :])]
                stage_out("scalar", [it], lambda e, slot, bank=bank: e.activation(out=slot[:], in_=bank[:], func=AF.Copy, scale=MLA_SCALE), dsts)
            for j in range(2):
                it1, b1 = ring.acquire()
                P.op("tensor", lambda e, j=j, b1=b1: e.matmul(b1[:], lhsT=wuq[:, 0, 512 + j * 128:512 + (j + 1) * 128], rhs=cqn[:, 0, :], start=True, stop=False))
                ring.produced(lambda e, j=j, b1=b1: e.matmul(b1[:], lhsT=wuq[:, 1, 512 + j * 128:512 + (j + 1) * 128], rhs=cqn[:, 1, :], start=False, stop=True))
                it2, b2 = ring.acquire()
                P.op("tensor", lambda e, j=j, b2=b2: e.matmul(b2[:], lhsT=wuq[:, 0, 768 + j * 128:768 + (j + 1) * 128], rhs=cqn[:, 0, :], start=True, stop=False))
                ring.produced(lambda e, j=j, b2=b2: e.matmul(b2[:], lhsT=wuq[:, 1, 768 + j * 128:768 + (j + 1) * 128], rhs=cqn[:, 1, :], start=False, stop=True))
                ring.consume_wait("vector", it2)
                P.op("vector", lambda e, b1=b1, R=R: e.tensor_tensor(out=rt1[:], in0=b1[:], in1=R[2][:], op=ALU.mult))
                P.op("vector", lambda e, b2=b2, R=R: e.tensor_tensor(out=rt2[:], in0=b2[:], in1=R[3][:], op=ALU.mult))
                P.op("vector", lambda e: e.tensor_tensor(out=rt1[:], in0=rt1[:], in1=rt2[:], op=ALU.add))
                dsts = [(QTm[4 * j + hh, 64:96, qcol:qcol + 512], lambda s, hh=hh: s[hh * 32:(hh + 1) * 32, :]) for hh in range(4)]
                stage_out("vector", [it1, it2], lambda e, slot: e.tensor_scalar(out=slot[:], in0=rt1[:], scalar1=MLA_SCALE, scalar2=None, op0=ALU.mult), dsts)
        hfree_hist.append(ring.free["scalar"].n)
        if not isctx:
            rtfree_hist.append(ring.free["vector"].n)
    for eng in ("scalar", "vector"):
        for so in s_outs[eng]:
            P.wait("sync", so, so.n)
    for so in s_vouts:
        P.wait("sync", so, so.n)
    P.end()
    if stage <= 1:
        top.close()
        return nc


    P.begin()
    KTb = [P.sb("KTb%d" % i, [128, NK], BF16) for i in range(2)]
    Vb = [P.sb("Vb%d" % i, [128, NKT, 128], BF16) for i in range(2)]
    QTb = [P.sb("QTb%d" % i, [128, NQ], BF16) for i in range(2)]
    Pb = P.sb("Pb", [128, 4, 512], BF16)
    osb = [P.sb("osb%d" % i, [128, 512], F32) for i in range(2)]
    rden = [P.sb("rden%d" % i, [128, 512], F32) for i in range(2)]
    ostm = [P.sb("ostm%d" % i, [64, 512], BF16) for i in range(2)]
    ostd = [P.sb("ostd%d" % i, [128, 512], F32) for i in range(2)]
    dr0 = P.sb("dr0", [128, 512], F32)
    dr1 = P.sb("dr1", [128, 512], F32)
    dt1 = P.sb("dt1", [128, 512], F32)
    s_uld = [P.sem("p2ld0"), P.sem("p2ld1")]
    s_pes = P.sem("p2pes")
    s_act = P.sem("p2act")
    s_pv = P.sem("p2pv")
    s_fv = P.sem("p2fv")
    s_bc = P.sem("p2bc")
    s_dacc = P.sem("p2dacc")
    s_ones = P.sem("p2ones")
    accD = [P.sb("accD%d" % i, [128, 2, 512], F32) for i in range(2)]
    dstate = {"m": 0, "q": 0, "ones": []}
    s_ods = [P.sem("p2od0"), P.sem("p2od1")]
    od_hist = []
    nstep = [0]
    unit_end_pv = []

    def load_unit(u):
        b = u % 2
        if u >= 2:
            P.wait("sync", s_pv, unit_end_pv[u - 2])
        sem = s_uld[b]
        if u < 8:
            P.dma("sync", KTb[b][0:64, :], KTm[u], sem)
            P.dma("sync", KTb[b][64:96, :], KR, sem)
            P.dma("sync", QTb[b][0:96, :], QTm[u], sem)
            for g in range(6):
                P.dma("sync", Vb[b][:, g * 11:(g + 1) * 11, 0:65], Vm[g * 1408:(g + 1) * 1408, u, :].rearrange("(i p) d -> p i d", p=128), sem)
        else:
            hd = u - 8
            P.dma("sync", KTb[b][:, :], KTd[hd], sem)
            P.dma("sync", QTb[b][:, :], QTd[hd], sem)
            for g in range(6):
                P.dma("sync", Vb[b][:, g * 11:(g + 1) * 11, :], Vd[g * 1408:(g + 1) * 1408, hd * 128:(hd + 1) * 128].rearrange("(i p) d -> p i d", p=128), sem)
        return sem.n

    fin_state = {"n": 0, "bc_free": 0, "o_free": [0, 0], "od": []}
    uld_val = {0: load_unit(0)}
    for u in range(12):
        b = u % 2
        mla = u < 8
        if u + 1 < 12:
            uld_val[u + 1] = load_unit(u + 1)
        P.wait("tensor", s_uld[b], uld_val[u])
        R = 4 if mla else 2
        L = 3 if mla else 1
        dbase = dstate["m"]
        steps = [(j, i) for j in range(8) for i in range(NKT)]
        base = nstep[0]
        KT, V, QT = KTb[b], Vb[b], QTb[b]
        pend_fin = []

        def emit_S(s, base=base, mla=mla, R=R, KT=KT, QT=QT):
            j, i = steps[s]
            n = base + s
            if s >= R:
                P.wait("tensor", s_act, n - R + 1)
            elif base > 0:
                P.wait("tensor", s_act, base)
            if mla:
                P.op("tensor", lambda e, s=s, i=i, j=j: e.matmul(psum[s % 4][:, :], lhsT=KT[0:96, i * 128:(i + 1) * 128], rhs=QT[0:96, j * 512:(j + 1) * 512], start=True, stop=True), s_pes, 1)
            else:
                r = s % 2
                P.op("tensor", lambda e, r=r, i=i, j=j: e.matmul(psum[2 * r][:, :], lhsT=KT[0:64, i * 128:(i + 1) * 128], rhs=QT[0:64, j * 512:(j + 1) * 512], start=True, stop=True))
                P.op("tensor", lambda e, r=r, i=i, j=j: e.matmul(psum[2 * r + 1][:, :], lhsT=KT[64:128, i * 128:(i + 1) * 128], rhs=QT[64:128, j * 512:(j + 1) * 512], start=True, stop=True), s_pes, 1)

        def emit_exp(s, base=base, mla=mla, R=R):
            n = base + s
            P.wait("scalar", s_pes, n + 1)
            if s >= R:
                P.wait("scalar", s_pv, n - R + 1)
            elif base > 0:
                P.wait("scalar", s_pv, base)
            if mla:
                P.op("scalar", lambda e, s=s: e.activation(out=Pb[:, s % 4, :], in_=psum[s % 4][:, :], func=AF.Exp), s_act, 1)
            else:
                r = s % 2
                j, i = steps[s]
                need = (dbase + s - 1) if s >= 2 else dbase
                P.wait("scalar", s_dacc, need)
                P.op("scalar", lambda e, r=r: e.activation(out=Pb[:, 2 * r:2 * r + 2, :], in_=psall[:, 2 * r:2 * r + 2, :], func=AF.Exp), s_act, 1)
                qd = dstate["q"] + j
                a_ = accD[qd % 2]
                P.wait("vector", s_act, n + 1)
                if i == 0:
                    if qd >= 2:
                        P.wait("vector", s_ones, dstate["ones"][qd - 2])
                    P.op("vector", lambda e, a_=a_, r=r: e.tensor_copy(out=a_[:], in_=Pb[:, 2 * r:2 * r + 2, :]), s_dacc, 1)
                else:
                    P.op("vector", lambda e, a_=a_, r=r: e.tensor_tensor(out=a_[:], in0=a_[:], in1=Pb[:, 2 * r:2 * r + 2, :], op=ALU.add), s_dacc, 1)

        def emit_PV(s, base=base, mla=mla, V=V, u=u):
            j, i = steps[s]
            n = base + s
            P.wait("tensor", s_act, n + 1)
            if mla:
                ob = psum[4 + j % 2]
                if i == 0 and fin_state["o_free"][j % 2]:
                    P.wait("tensor", s_fv, fin_state["o_free"][j % 2])
                P.op("tensor", lambda e, s=s, i=i, ob=ob: e.matmul(ob[0:65, :], lhsT=V[:, i, 0:65], rhs=Pb[:, s % 4, :], start=(i == 0), stop=(i == NKT - 1)), s_pv, 1)
            else:
                r = s % 2
                if i == 0:
                    P.wait("tensor", s_fv, max(fin_state["o_free"][0], fin_state["o_free"][1], fin_state["bc_free"]))
                st, sp = (i == 0), (i == NKT - 1)
                P.op("tensor", lambda e, r=r, i=i, st=st, sp=sp: e.matmul(psum[4][:, :], lhsT=V[:, i, :], rhs=Pb[:, 2 * r, :], start=st, stop=sp))
                P.op("tensor", lambda e, r=r, i=i, st=st, sp=sp: e.matmul(psum[5][:, :], lhsT=V[:, i, :], rhs=Pb[:, 2 * r + 1, :], start=st, stop=sp), s_pv, 1)
            if i == NKT - 1:
                fin_dve(u, j, n + 1, dbase + s + 1)

        def fin_dve(u, j, pvval, accval=0):
            k = fin_state["n"]
            fin_state["n"] += 1
            P.wait("vector", s_pv, pvval)
            if u < 8:
                ob = psum[4 + j % 2]
                o, rd, stg = osb[k % 2], rden[k % 2], ostm[k % 2]
                v = P.op("vector", lambda e, ob=ob, o=o: e.tensor_copy(out=o[0:65, :], in_=ob[0:65, :]), s_fv, 1)
                fin_state["o_free"][j % 2] = v
                v2 = P.op("vector", lambda e, o=o, rd=rd: e.reciprocal(out=rd[64:65, :], in_=o[64:65, :]), s_fv, 1)
                pend_fin.append((u, j, k, v2))
            else:
                hd = u - 8
                stg = ostd[k % 2]
                qd = dstate["q"] + j
                P.wait("tensor", s_dacc, accval)
                P.wait("tensor", s_fv, max(fin_state["o_free"][0], fin_state["o_free"][1], fin_state["bc_free"]))
                P.op("tensor", lambda e, qd=qd: e.matmul(psum[6][:, :], lhsT=ones_f[:, :], rhs=accD[qd % 2][:, 0, :], start=True, stop=True))
                vo = P.op("tensor", lambda e, qd=qd: e.matmul(psum[7][:, :], lhsT=ones_f[:, :], rhs=accD[qd % 2][:, 1, :], start=True, stop=True), s_ones, 1)
                dstate["ones"].append(vo)
                P.wait("vector", s_ones, vo)
                s_od = s_ods[k % 2]
                if len(fin_state["od"]) >= 2:
                    P.wait("vector", s_od, fin_state["od"][-2])
                P.op("vector", lambda e: e.reciprocal(out=dr0[:], in_=psum[6][:, :]))
                P.op("vector", lambda e: e.reciprocal(out=dr1[:], in_=psum[7][:, :]))
                P.op("vector", lambda e: e.tensor_tensor(out=dr0[:], in0=psum[4][:, :], in1=dr0[:], op=ALU.mult))
                v = P.op("vector", lambda e: e.tensor_tensor(out=dt1[:], in0=psum[5][:, :], in1=dr1[:], op=ALU.mult), s_fv, 1)
                fin_state["o_free"][0] = v
                v3 = P.op("vector", lambda e, stg=stg: e.scalar_tensor_tensor(out=stg[:], in0=dt1[:], scalar=neglam[:, 0:1], in1=dr0[:], op0=ALU.mult, op1=ALU.add), s_fv, 1)
                P.wait("sync", s_fv, v3)
                fin_state["od"].append(P.dma("sync", OTd[hd, :, j * 512:(j + 1) * 512], stg[:], s_od))

        def fin_pe():
            while pend_fin:
                u_, j, k, v2 = pend_fin.pop(0)
                o, rd, stg = osb[k % 2], rden[k % 2], ostm[k % 2]
                P.wait("tensor", s_fv, max(v2, fin_state["bc_free"]))
                vb = P.op("tensor", lambda e, rd=rd: e.matmul(psum[6][0:64, :], lhsT=ones_f[64:65, 0:64], rhs=rd[64:65, :], start=True, stop=True), s_bc, 1)
                P.wait("vector", s_bc, vb)
                s_od = s_ods[k % 2]
                if len(fin_state["od"]) >= 2:
                    P.wait("vector", s_od, fin_state["od"][-2])
                v3 = P.op("vector", lambda e, o=o, stg=stg: e.tensor_tensor(out=stg[:, :], in0=o[0:64, :], in1=psum[6][0:64, :], op=ALU.mult), s_fv, 1)
                fin_state["bc_free"] = v3
                P.wait("sync", s_fv, v3)
                fin_state["od"].append(P.dma("sync", OTm[u_, :, j * 512:(j + 1) * 512], stg[:, :], s_od))

        ns = len(steps)
        for s in range(min(L, ns)):
            emit_S(s)
            emit_exp(s)
        for s in range(ns):
            if s + L < ns:
                emit_S(s + L)
                emit_exp(s + L)
            emit_PV(s)
            if mla and pend_fin and (steps[s][1] == 4):
                fin_pe()
        if mla:
            fin_pe()
        nstep[0] += ns
        if not mla:
            dstate["m"] += ns
            dstate["q"] += 8
        unit_end_pv.append(s_pv.n)
    for so in s_ods:
        P.wait("sync", so, so.n)
    P.end()
    if stage <= 2:
        top.close()
        return nc

    H2D = scratch("H2D", [NQ, D], BF16)
    aff = gsb("aff", [128, 32, NE], F32)
    iotg = gsb("iotg", [128, 128], F32)
    iopg = gsb("iopg", [128, 1], F32)

    class Tk:
        def __init__(self, tag):
            self.s = {en: P.sem(tag + en[:3]) for en in ENGS}

        def do(self, eng, fn, deps=(), dma=False):
            for d_ in deps:
                if d_ is not None and d_[0] != eng:
                    P.wait(eng, self.s[d_[0]], d_[1])
            v = P.op(eng, fn, self.s[eng], 1)
            return (eng, v)

    def dmado(eng, out_, in_, sem, deps, tk):
        for d_ in deps:
            if d_ is not None and d_[0] != eng:
                P.wait(eng, tk.s[d_[0]], d_[1])
        return P.dma(eng, out_, in_, sem)

    P.begin()
    tk = Tk("q")
    Wom = P.sb("Wom", [64, 8, D], BF16)
    Wod = P.sb("Wod", [128, 4, D], BF16)
    Wout = P.sb("Wout", [128, 8, D], BF16)
    wr = P.sb("wr", [128, 8, NE], F32)
    brt = P.sb("brt", [1, NE], F32)
    lnr = P.sb("lnr", [128, 4, D], F32)
    gsub08 = P.sb("gsub08", [128, 1], F32)
    om = P.sb("om", [64, 8, 512], BF16)
    od32 = P.sb("od32", [128, 4, 512], F32)
    odb = P.sb("odb", [128, 4, 512], BF16)
    gtb = P.sb("gtb", [128, 16, 512], BF16)
    yT = P.sb("yT", [128, 8, 512], BF16)
    xt = [P.sb("xt%d" % i, [128, D], F32) for i in range(2)]
    tA = P.sb("tA", [128, 512], F32)
    tB = P.sb("tB", [128, 512], F32)
    tS = P.sb("tS", [128, 512], F32)
    vvs = [P.sb("vv%d" % i, [128, D], F32) for i in range(2)]
    x1t = [P.sb("x1t%d" % i, [128, D], F32) for i in range(2)]
    h2fs = [P.sb("h2f%d" % i, [128, D], F32) for i in range(2)]
    h2b = [P.sb("h2b%d" % i, [128, D], BF16) for i in range(2)]
    h2Ts = [P.sb("h2T%d" % i, [128, 8, 128], F32) for i in range(2)]
    bsts = [P.sb("bst%d" % i, [128, 2, 6], F32) for i in range(2)]
    mvs = [P.sb("mv%d" % i, [128, 2], F32) for i in range(2)]
    sd1s = [P.sb("sd1%d" % i, [128, 1], F32) for i in range(2)]
    rs1s = [P.sb("rs1%d" % i, [128, 1], F32) for i in range(2)]
    lgs = [P.sb("lg%d" % i, [128, NE], F32) for i in range(2)]
    exs = [P.sb("ex%d" % i, [128, NE], F32) for i in range(2)]
    mxs = [P.sb("mx%d" % i, [128, 1], F32) for i in range(2)]
    ssums = [P.sb("ssum%d" % i, [128, 1], F32) for i in range(2)]
    s_w = P.sem("qw")
    s_lt = P.sem("qlt")
    s_lx = [P.sem("qlx0"), P.sem("qlx1")]
    s_o1 = [P.sem("qo10"), P.sem("qo11")]
    s_o2 = [P.sem("qo20"), P.sem("qo21")]
    P.dma("gpsimd", Wom[:], w_o_mla.rearrange("(h d) n -> d h n", d=64), s_w)
    P.dma("gpsimd", Wod[:], w_o_diff.rearrange("(h d) n -> d h n", d=128), s_w)
    P.dma("gpsimd", Wout[:], w_out.rearrange("(k p) n -> p k n", p=128), s_w)
    P.dma("gpsimd", wr[:], w_router.rearrange("(k p) n -> p k n", p=128), s_w)
    P.dma("gpsimd", brt[:], b_router, s_w)
    v_w = P.dma("gpsimd", lnr[:], lnrows, s_w)
    P.op("gpsimd", lambda e: e.iota(iotg[:], [[1, 128]], base=0, channel_multiplier=0, allow_small_or_imprecise_dtypes=True))
    P.op("gpsimd", lambda e: e.iota(iopg[:], [[0, 1]], base=0, channel_multiplier=1, allow_small_or_imprecise_dtypes=True))
    for en in ("tensor", "vector", "scalar"):
        P.wait(en, s_w, v_w)
    P.opf("vector", lambda e: e.tensor_scalar(out=gsub08[:], in0=gsub[:], scalar1=0.8, scalar2=None, op0=ALU.mult))
    ring = PsRing(P, psum, "q")
    last_tile_done = None
    xfree = {}
    x1_hist = {}
    h2_hist = {}
    h2f_free = {}
    lasts = {}
    for j in range(8):
        c0 = j * 512
        deps = [last_tile_done]
        for d_ in deps:
            if d_ is not None:
                P.wait("sync", tk.s[d_[0]], d_[1])
        P.dma("sync", om[:], OTm[:, :, c0:c0 + 512].rearrange("h d t -> d h t"), s_lt)
        P.dma("sync", od32[:], OTd[:, :, c0:c0 + 512].rearrange("h p t -> p h t"), s_lt)
        v_lt = P.dma("sync", gtb[:], GT[:, :, c0:c0 + 512].rearrange("c p t -> p c t"), s_lt)
        for en in ("tensor", "vector", "scalar"):
            P.wait(en, s_lt, v_lt)
        for hd in range(4):
            t1 = tk.do("scalar", lambda e, hd=hd: e.activation(out=tS[:], in_=od32[:, hd, :], func=AF.Square))
            it, bank = ring.acquire()
            P.wait("tensor", tk.s["scalar"], t1[1])
            ring.produced(lambda e, bank=bank: e.matmul(bank[:, :], lhsT=ones_f[:], rhs=tS[:], start=True, stop=True))
            ring.consume_wait("scalar", it)
            v = ring.release("scalar", [it], lambda e, bank=bank: e.activation(out=tA[:], in_=bank[:, :], func=AF.Sqrt, bias=eps_t[:, 0:1], scale=1.0 / 128.0))
            P.wait("vector", ring.free["scalar"], v)
            P.op("vector", lambda e: e.reciprocal(out=tB[:], in_=tA[:]))
            t2 = tk.do("vector", lambda e, hd=hd: e.scalar_tensor_tensor(out=odb[:, hd, :], in0=od32[:, hd, :], scalar=gsub08[:, 0:1], in1=tB[:], op0=ALU.mult, op1=ALU.mult))
            P.wait("scalar", tk.s["vector"], t2[1])
        P.wait("tensor", tk.s["vector"], t2[1])
        for c in range(8):
            itA, bA = ring.acquire()
            for h in range(8):
                fn = lambda e, h=h, c=c, bA=bA: e.matmul(bA[:, :], lhsT=Wom[0:64, h, c * 128:(c + 1) * 128], rhs=om[0:64, h, :], start=(h == 0), stop=(h == 7))
                if h == 7:
                    ring.produced(fn)
                else:
                    P.op("tensor", fn)
            itB, bB = ring.acquire()
            for hd in range(4):
                fn = lambda e, hd=hd, c=c, bB=bB: e.matmul(bB[:, :], lhsT=Wod[:, hd, c * 128:(c + 1) * 128], rhs=odb[:, hd, :], start=(hd == 0), stop=(hd == 3))
                if hd == 3:
                    ring.produced(fn)
                else:
                    P.op("tensor", fn)
            ring.consume_wait("vector", itB)
            P.op("vector", lambda e, c=c, bA=bA: e.tensor_tensor(out=tA[:], in0=bA[:, :], in1=gtb[:, c, :], op=ALU.mult))
            P.op("vector", lambda e, c=c, bB=bB: e.tensor_tensor(out=tB[:], in0=bB[:, :], in1=gtb[:, 8 + c, :], op=ALU.mult))
            vy = ring.release("vector", [itA, itB], lambda e, c=c: e.tensor_tensor(out=yT[:, c, :], in0=tA[:], in1=tB[:], op=ALU.add))
        P.wait("tensor", ring.free["vector"], vy)
        def subtile(t, s4, ltd):
            p_ = t % 2
            vv, h2f, h2T, bst, mv, sd1, rs1 = vvs[p_], h2fs[p_], h2Ts[p_], bsts[p_], mvs[p_], sd1s[p_], rs1s[p_]
            lg, ex, mx, ssum = lgs[p_], exs[p_], mxs[p_], ssums[p_]
            xb = xt[t % 2]
            r0 = t * 128
            if t >= 2:
                P.wait("gpsimd", tk.s["vector"], xfree[t - 2])
            v_x = P.dma("gpsimd", xb[:], xown[r0:r0 + 128, :], s_lx[t % 2])
            its = []
            bks = []
            for nh in range(2):
                it, bank = ring.acquire()
                for c in range(8):
                    fn = lambda e, c=c, nh=nh, s4=s4, bank=bank: e.matmul(bank[:, :], lhsT=yT[:, c, s4 * 128:(s4 + 1) * 128], rhs=Wout[:, c, nh * 512:(nh + 1) * 512], start=(c == 0), stop=(c == 7))
                    if c == 7:
                        ring.produced(fn)
                    else:
                        P.op("tensor", fn)
                its.append(it)
                bks.append(bank)
            yield
            ring.consume_wait("vector", its[1])
            P.op("vector", lambda e, b0=bks[0]: e.tensor_tensor(out=vv[:, 0:512], in0=b0[:, :], in1=modrow[:, 0:512], op=ALU.mult))
            ring.release("vector", its, lambda e, b1=bks[1]: e.tensor_tensor(out=vv[:, 512:1024], in0=b1[:, :], in1=modrow[:, 512:1024], op=ALU.mult))
            P.wait("vector", s_lx[t % 2], v_x)
            xfree[t] = tk.do("vector", lambda e, xb=xb: e.scalar_tensor_tensor(out=vv[:], in0=xb[:], scalar=ALPHA, in1=vv[:], op0=ALU.mult, op1=ALU.add))[1]
            P.op("vector", lambda e: e.bn_stats(out=bst[:, 0, :], in_=vv[:, 0:512]))
            P.opf("vector", lambda e: e.bn_stats(out=bst[:, 1, :], in_=vv[:, 512:1024]))
            P.opf("vector", lambda e: e.tensor_copy(out=tA[:], in_=vv[:, 0:512]))
            t3 = tk.do("vector", lambda e: e.bn_aggr(out=mv[:], in_=bst[:].rearrange("p a b -> p (a b)")))
            t4 = tk.do("scalar", lambda e: e.activation(out=sd1[:], in_=mv[:, 1:2], func=AF.Sqrt, bias=eps_t[:, 0:1], scale=1.0), [t3])
            yield
            P.wait("vector", tk.s["scalar"], t4[1])
            P.opf("vector", lambda e: e.reciprocal(out=rs1[:], in_=sd1[:]))
            x1b = x1t[t % 2]
            if t >= 2:
                P.wait("vector", s_o1[t % 2], x1_hist[t - 2])
            P.op("vector", lambda e, x1b=x1b: e.tensor_scalar(out=x1b[:], in0=vv[:], scalar1=mv[:, 0:1], scalar2=rs1[:, 0:1], op0=ALU.subtract, op1=ALU.mult))
            t5 = tk.do("vector", lambda e, x1b=x1b: e.tensor_tensor(out=x1b[:], in0=x1b[:], in1=lnr[:, 0, :], op=ALU.mult))
            t6 = tk.do("gpsimd", lambda e, x1b=x1b: e.tensor_tensor(out=x1b[:], in0=x1b[:], in1=lnr[:, 1, :], op=ALU.add), [t5, ltd])
            P.wait("sync", tk.s["gpsimd"], t6[1])
            x1_hist[t] = P.dma("sync", X1[r0:r0 + 128, :], x1b[:], s_o1[t % 2])
            if t >= 2:
                P.wait("gpsimd", tk.s["scalar"], h2f_free[t - 2])
            P.op("gpsimd", lambda e, x1b=x1b: e.tensor_tensor(out=h2f[:], in0=x1b[:], in1=modrow[:, 2048:3072], op=ALU.mult))
            t7 = tk.do("gpsimd", lambda e: e.tensor_tensor(out=h2f[:], in0=h2f[:], in1=modrow[:, 1024:2048], op=ALU.add))
            hb = h2b[t % 2]
            if t >= 2:
                P.wait("scalar", s_o2[t % 2], h2_hist[t - 2])
            t8 = tk.do("scalar", lambda e, hb=hb: e.activation(out=hb[:], in_=h2f[:], func=AF.Copy), [t7])
            P.wait("sync", tk.s["scalar"], t8[1])
            h2_hist[t] = P.dma("sync", H2D[r0:r0 + 128, :], hb[:], s_o2[t % 2])
            yield
            P.wait("tensor", tk.s["gpsimd"], t7[1])
            tits = []
            tbk = []
            for hh in range(2):
                it, bank = ring.acquire()
                for q in range(4):
                    c = hh * 4 + q
                    fn = lambda e, c=c, q=q, bank=bank: e.transpose(out=bank[:, q * 128:(q + 1) * 128], in_=h2f[:, c * 128:(c + 1) * 128], identity=ident_f[:])
                    if q == 3:
                        ring.produced(fn)
                    else:
                        P.op("tensor", fn)
                tits.append(it)
                tbk.append(bank)
            for hh in range(2):
                ring.consume_wait("scalar", tits[hh])
                vh = ring.release("scalar", [tits[hh]], lambda e, hh=hh, bank=tbk[hh]: e.activation(out=h2T[:, hh * 4:(hh + 1) * 4, :], in_=bank[:, :].rearrange("p (q n) -> p q n", q=4), func=AF.Copy))
            tk.do("scalar", lambda e: e.nop())
            h2f_free[t] = tk.s["scalar"].n
            yield
            P.wait("tensor", ring.free["scalar"], vh)
            it, bank = ring.acquire()
            for c in range(8):
                P.op("tensor", lambda e, c=c, bank=bank: e.matmul(bank[:, 0:NE], lhsT=h2T[:, c, :], rhs=wr[:, c, :], start=(c == 0), stop=False))
            ring.produced(lambda e, bank=bank: e.matmul(bank[:, 0:NE], lhsT=ones_f[0:1, :], rhs=brt[0:1, :], start=False, stop=True))
            ring.consume_wait("vector", it)
            P.opf("vector", lambda e, bank=bank: e.tensor_reduce(out=mx[:], in_=bank[:, 0:NE], axis=AX.X, op=ALU.max, negate=True))
            vl = ring.release("vector", [it], lambda e, bank=bank: e.tensor_copy(out=lg[:], in_=bank[:, 0:NE]))
            yield
            P.wait("scalar", ring.free["vector"], vl)
            t9 = tk.do("scalar", lambda e: e.activation(out=ex[:], in_=lg[:], func=AF.Exp, bias=mx[:, 0:1], scale=1.0, accum_out=ssum[:, 0:1]))
            P.wait("vector", tk.s["scalar"], t9[1])
            P.opf("vector", lambda e: e.reciprocal(out=ssum[:], in_=ssum[:]))
            last_ = tk.do("vector", lambda e, t=t: e.tensor_scalar(out=aff[:, t, :], in0=ex[:], scalar1=ssum[:, 0:1], scalar2=None, op0=ALU.mult))
            P.wait("scalar", tk.s["vector"], last_[1])
            lasts[t] = last_

        for pair in ((0, 1), (2, 3)):
            gens = [subtile(j * 4 + a_, a_, last_tile_done) for a_ in pair]
            alive = True
            while alive:
                alive = False
                for g_ in gens:
                    try:
                        next(g_)
                        alive = True
                    except StopIteration:
                        pass
        last = lasts[j * 4 + 3]
        last_tile_done = last
    for so in s_o1 + s_o2:
        P.wait("sync", so, so.n)
    P.end()
    if stage <= 3:
        top.close()
        return nc

    slotidx = gsb("slotidx", [128, NE, 32], I32)
    gmask = gsb("gmask", [128, NE, 32], F32)
    P.begin()
    tk = Tk("r")
    affall = P.sb("affall", [128, 64, NE], F32)
    affT = P.sb("affT", [128, NE, 64], F32)
    affTo = P.sb("affTo", [128, NE, 32], F32)
    junk = P.sb("junk", [128, 64], F32)
    cnt = P.sb("cnt", [128, NE], F32)
    lo = P.sb("lo", [128, NE], F32)
    mid = P.sb("mid", [128, NE], F32)
    ge = P.sb("ge", [128, NE], F32)
    maskT = P.sb("maskT", [128, NE, 32], F32)
    maskb = P.sb("maskb", [128, NE, 32], BF16)
    Sx = P.sb("Sx", [128, NE, 32], F32)
    Sb = P.sb("Sb", [128, NE, 32], BF16)
    Ub = P.sb("Ub", [128, 128], BF16)
    posf = P.sb("posf", [128, NE, 32], F32)
    s_d = P.sem("rd")
    s_cc = P.sem("rcc")
    v = P.dma("gpsimd", AFF_IN.ap(), aff[:].rearrange("p t e -> p (t e)"), s_d)
    P.wait("gpsimd", s_d, v)
    P.op("gpsimd", lambda e: e.collective_compute("AllGather", ALU.bypass, replica_groups=[[0, 1], [2, 3], [4, 5], [6, 7]], ins=[AFF_IN.ap().opt()], outs=[AFF_OUT.ap().opt()]), s_cc, 1)
    P.wait("gpsimd", s_cc, 1)
    P.dma("gpsimd", affall[:, 0:32, :], AFF_OUT.ap()[0:128, :].rearrange("p (t e) -> p t e", e=NE), s_d)
    v = P.dma("gpsimd", affall[:, 32:64, :], AFF_OUT.ap()[128:256, :].rearrange("p (t e) -> p t e", e=NE), s_d)
    P.wait("vector", s_d, v)
    P.op("vector", lambda e: e.tensor_copy(out=affT[:], in_=affall[:].rearrange("p t e -> p e t")))
    P.op("vector", lambda e: e.tensor_copy(out=affTo[:], in_=aff[:].rearrange("p t e -> p e t")))
    P.opf("vector", lambda e: e.memset(lo[:], 0.0))
    P.op("vector", lambda e: e.tensor_scalar(out=Ub[:], in0=iotg[:], scalar1=iopg[:, 0:1], scalar2=None, op0=ALU.is_gt))
    for itn in range(NITER):
        step = 2.0 ** -(itn + 1)
        P.opf("vector", lambda e, step=step: e.tensor_scalar(out=mid[:], in0=lo[:], scalar1=step, scalar2=None, op0=ALU.add))
        for ex_ in range(NE):
            fn = lambda e, ex_=ex_: e.tensor_scalar(out=junk[:], in0=affT[:, ex_, :], scalar1=mid[:, ex_:ex_ + 1], scalar2=0.0, op0=ALU.is_gt, op1=ALU.add, accum_out=cnt[:, ex_:ex_ + 1])
            if ex_ == NE - 1:
                tc_ = tk.do("vector", fn)
            else:
                P.op("vector", fn)
        bank = psum[itn % 2]
        tp = tk.do("tensor", lambda e, bank=bank: e.matmul(bank[:, 0:NE], lhsT=ones_f[:], rhs=cnt[:], start=True, stop=True), [tc_])
        P.wait("vector", tk.s["tensor"], tp[1])
        P.opf("vector", lambda e, bank=bank: e.tensor_scalar(out=ge[:], in0=bank[:, 0:NE], scalar1=float(CPAD) - 0.5, scalar2=None, op0=ALU.is_ge))
        P.opf("vector", lambda e, step=step: e.scalar_tensor_tensor(out=lo[:], in0=ge[:], scalar=step, in1=lo[:], op0=ALU.mult, op1=ALU.add))
    for ex_ in range(NE):
        P.op("vector", lambda e, ex_=ex_: e.tensor_scalar(out=maskT[:, ex_, :], in0=affTo[:, ex_, :], scalar1=lo[:, ex_:ex_ + 1], scalar2=None, op0=ALU.is_gt))
    P.op("vector", lambda e: e.tensor_tensor(out=gmask[:], in0=maskT[:], in1=affTo[:], op=ALU.mult))
    P.op("vector", lambda e: e.tensor_copy(out=maskb[:], in_=maskT[:]))
    P.opf("vector", lambda e: e.memset(Sx[:], 0.0))
    for t in range(1, 32):
        P.opf("vector", lambda e, t=t: e.tensor_tensor(out=Sx[:, :, t], in0=Sx[:, :, t - 1], in1=maskT[:, :, t - 1], op=ALU.add))
    tsb = tk.do("vector", lambda e: e.tensor_copy(out=Sb[:], in_=Sx[:]))
    P.wait("tensor", tk.s["vector"], tsb[1])
    P.op("tensor", lambda e: e.matmul(psum[2][:, :], lhsT=ones_b[:], rhs=Sb[:].rearrange("p e t -> p (e t)"), start=True, stop=False))
    tpp = tk.do("tensor", lambda e: e.matmul(psum[2][:, :], lhsT=Ub[:], rhs=maskb[:].rearrange("p e t -> p (e t)"), start=False, stop=True))
    P.wait("vector", tk.s["tensor"], tpp[1])
    P.op("vector", lambda e: e.scalar_tensor_tensor(out=posf[:].rearrange("p e t -> p (e t)"), in0=maskT[:].rearrange("p e t -> p (e t)"), scalar=-1048576.0, in1=psum[2][:, :], op0=ALU.mult, op1=ALU.add))
    P.op("vector", lambda e: e.tensor_scalar(out=posf[:], in0=posf[:], scalar1=1048576.0, scalar2=None, op0=ALU.add))
    tdb = tk.do("vector", lambda e: e.tensor_copy(out=slotidx[:], in_=posf[:]))
    if "DBGT" in dbg:
        P.wait("sync", tk.s["vector"], tdb[1])
        P.dma("sync", DBGT[:, 0:512], aff[:].rearrange("p t e -> p (t e)"), s_d)
        P.dma("sync", DBGT[:, 512:1024], posf[:].rearrange("p e t -> p (e t)"), s_d)
        P.dma("sync", DBGT[:, 1024:1536], gmask[:].rearrange("p e t -> p (e t)"), s_d)
        vdb = P.dma("sync", DBGT[:, 1536:1552], lo[:], s_d)
        P.wait("sync", s_d, vdb)
    P.end()
    if stage <= 4:
        top.close()
        return nc

    P.begin()
    tk = Tk("m")
    w1b = [P.sb("w1b%d" % i, [128, 8, D], BF16) for i in range(2)]
    w3b = [P.sb("w3b%d" % i, [128, 8, D], BF16) for i in range(2)]
    w2b = [P.sb("w2b%d" % i, [128, 8, D], BF16) for i in range(2)]
    h2t = [P.sb("h2t%d" % i, [128, D], BF16) for i in range(4)]
    xtok = [P.sb("xtok%d" % i, [128, 4, D], BF16) for i in range(2)]
    xeT = [P.sb("xeT%d" % i, [128, 8, 512], BF16) for i in range(2)]
    hid = P.sb("hid", [128, 8, 512], BF16)
    sgt = [P.sb("sgt%d" % i, [128, 512], F32) for i in range(2)]
    yst = [P.sb("yst%d" % i, [128, D], F32) for i in range(2)]
    s_wl = [P.sem("mw0"), P.sem("mw1")]
    s_hl = [P.sem("mh%d" % i) for i in range(4)]
    s_sc = [P.sem("msc%d" % i) for i in range(4)]
    s_scall = P.sem("mscall")
    s_xl = [P.sem("mx0"), P.sem("mx1")]
    s_yo = [P.sem("my0"), P.sem("my1")]
    ring = PsRing(P, psum, "m")
    psb = [psall[:, i, :].bitcast(BF16) for i in range(8)]
    wl_val = {}
    sc_done = {}
    exp_done = {}
    hcount = [0]
    h_hist = []
    sc_hist = []

    def load_w(e_):
        b = e_ % 2
        if e_ >= 2:
            a_, v_ = exp_done[e_ - 2]
            P.wait("gpsimd", ring.free["scalar"], a_)
            P.wait("gpsimd", ring.free["vector"], v_)
        P.dma("gpsimd", w1b[b][:], moe_w1[e_].rearrange("(k p) n -> p k n", p=128), s_wl[b])
        P.dma("gpsimd", w3b[b][:], moe_w3[e_].rearrange("(k p) n -> p k n", p=128), s_wl[b])
        wl_val[e_] = P.dma("gpsimd", w2b[b][:], moe_w2[e_].rearrange("(k p) n -> p k n", p=128), s_wl[b])

    def dispatch(e_):
        base = hcount[0]

        def load(t):
            i = base + t
            if i >= 4:
                P.wait("gpsimd", s_sc[i % 4], sc_hist[i - 4])
            return P.dma("gpsimd", h2t[i % 4][:], H2D[t * 128:(t + 1) * 128, :], s_hl[i % 4])

        lv = {}
        lv[0] = load(0)
        lv[1] = load(1)
        for t in range(32):
            i = base + t
            hb = h2t[i % 4]
            P.wait("gpsimd", s_hl[i % 4], lv[t])
            P.op("gpsimd", lambda e, e_=e_, t=t, hb=hb: e.indirect_dma_start(out=XE[e_], out_offset=bass.IndirectOffsetOnAxis(ap=slotidx[:, e_, t:t + 1], axis=0), in_=hb[:, :], in_offset=None, bounds_check=breg["r"], oob_is_err=False), s_sc[i % 4], 16)
            sc_hist.append(s_sc[i % 4].n)
            if t + 2 < 32:
                lv[t + 2] = load(t + 2)
        hcount[0] += 32
        sc_done[e_] = [s_sc[k].n for k in range(4)]

    alt2 = [0]

    def nxt():
        alt2[0] += 1
        return "scalar" if alt2[0] % 2 else "vector"

    xcount = [0]
    x_hist = []
    ycount = [0]
    y_hist = []
    breg = {}

    def _mk_reg(e):
        breg["r"] = e.alloc_register("bchk")
        e.reg_mov(breg["r"], CPAD - 1)

    P.op("gpsimd", _mk_reg)
    NST_ = CPAD // 512
    vxs = {}

    def issue_xload(T):
        ee, st_ = T // NST_, T % NST_
        xb_ = xtok[T % 2]
        if st_ == 0:
            for k in range(4):
                P.wait("sync", s_sc[k], sc_done[ee][k])
        if T >= 2:
            a_, v_ = x_hist[T - 2]
            P.wait("sync", ring.free["scalar"], a_)
            P.wait("sync", ring.free["vector"], v_)
        vxs[T] = P.dma("sync", xb_[:], XE[ee][st_ * 512:(st_ + 1) * 512, :].rearrange("(s p) d -> p s d", p=128), s_xl[T % 2])

    load_w(0)
    dispatch(0)
    dispatch(1)
    issue_xload(0)
    for e_ in range(NE):
        b = e_ % 2
        if e_ + 1 < NE:
            load_w(e_ + 1)
        if e_ + 2 < NE:
            dispatch(e_ + 2)
        P.wait("tensor", s_wl[b], wl_val[e_])
        for st in range(NST_):
            xi = xcount[0]
            xcount[0] += 1
            xb = xtok[xi % 2]
            P.wait("tensor", s_xl[xi % 2], vxs[xi])
            xT_ = xeT[xi % 2]
            for c in range(8):
                it, bank = ring.acquire()
                pb = psb[(it) % 8]
                for s4 in range(4):
                    fn = lambda e, c=c, s4=s4, pb=pb, xb=xb: e.transpose(out=pb[:, s4 * 128:(s4 + 1) * 128], in_=xb[:, s4, c * 128:(c + 1) * 128], identity=ident_b[:])
                    if s4 == 3:
                        ring.produced(fn)
                    else:
                        P.op("tensor", fn)
                eng = nxt()
                ring.consume_wait(eng, it)
                if eng == "scalar":
                    vt = ring.release(eng, [it], lambda e, c=c, pb=pb, xT_=xT_: e.activation(out=xT_[:, c, :], in_=pb[:, 0:512], func=AF.Copy))
                else:
                    vt = ring.release(eng, [it], lambda e, c=c, pb=pb, xT_=xT_: e.tensor_copy(out=xT_[:, c, :], in_=pb[:, 0:512]))
            x_hist.append((ring.free["scalar"].n, ring.free["vector"].n))
            if xi + 1 < NE * NST_:
                issue_xload(xi + 1)
            P.wait("tensor", ring.free["scalar"], ring.free["scalar"].n)
            P.wait("tensor", ring.free["vector"], ring.free["vector"].n)
            for f in range(8):
                itA, bA = ring.acquire()
                for c in range(8):
                    fn = lambda e, c=c, f=f, bA=bA, b=b, xT_=xT_: e.matmul(bA[:, :], lhsT=w1b[b][:, c, f * 128:(f + 1) * 128], rhs=xT_[:, c, :], start=(c == 0), stop=(c == 7))
                    if c == 7:
                        ring.produced(fn)
                    else:
                        P.op("tensor", fn)
                itB, bB = ring.acquire()
                for c in range(8):
                    fn = lambda e, c=c, f=f, bB=bB, b=b, xT_=xT_: e.matmul(bB[:, :], lhsT=w3b[b][:, c, f * 128:(f + 1) * 128], rhs=xT_[:, c, :], start=(c == 0), stop=(c == 7))
                    if c == 7:
                        ring.produced(fn)
                    else:
                        P.op("tensor", fn)
                sg = sgt[f % 2]
                ring.consume_wait("scalar", itA)
                if f >= 2:
                    P.wait("scalar", ring.free["vector"], sg_free[f % 2])
                else:
                    if f == 0:
                        sg_free = {}
                    if (e_, st) != (0, 0):
                        P.wait("scalar", ring.free["vector"], prev_sg_free[f % 2])
                va = ring.release("scalar", [itA], lambda e, bA=bA, sg=sg: e.activation(out=sg[:], in_=bA[:, :], func=AF.Silu))
                ring.consume_wait("vector", itB)
                P.wait("vector", ring.free["scalar"], va)
                sg_free[f % 2] = ring.release("vector", [itB], lambda e, bB=bB, sg=sg, f=f: e.tensor_tensor(out=hid[:, f, :], in0=sg[:], in1=bB[:, :], op=ALU.mult))
            prev_sg_free = dict(sg_free)
            P.wait("tensor", ring.free["vector"], ring.free["vector"].n)
            for s4 in range(4):
                yi = ycount[0]
                ycount[0] += 1
                yb = yst[yi % 2]
                for nh in range(2):
                    it, bank = ring.acquire()
                    for f in range(8):
                        fn = lambda e, f=f, s4=s4, nh=nh, bank=bank, b=b: e.matmul(bank[:, :], lhsT=hid[:, f, s4 * 128:(s4 + 1) * 128], rhs=w2b[b][:, f, nh * 512:(nh + 1) * 512], start=(f == 0), stop=(f == 7))
                        if f == 7:
                            ring.produced(fn)
                        else:
                            P.op("tensor", fn)
                    eng = "scalar" if nh == 0 else "vector"
                    ring.consume_wait(eng, it)
                    if nh == 0 and yi >= 2:
                        P.wait("scalar", s_yo[yi % 2], y_hist[yi - 2])
                    if nh == 1 and yi >= 2:
                        P.wait("vector", s_yo[yi % 2], y_hist[yi - 2])
                    if eng == "scalar":
                        vy0 = ring.release(eng, [it], lambda e, bank=bank, yb=yb: e.activation(out=yb[:, 0:512], in_=bank[:, :], func=AF.Copy))
                    else:
                        vy1 = ring.release(eng, [it], lambda e, bank=bank, yb=yb: e.tensor_copy(out=yb[:, 512:1024], in_=bank[:, :]))
                P.wait("sync", ring.free["scalar"], vy0)
                P.wait("sync", ring.free["vector"], vy1)
                r0 = st * 512 + s4 * 128
                y_hist.append(P.dma("sync", YE[e_][r0:r0 + 128, :], yb[:], s_yo[yi % 2]))
        exp_done[e_] = (ring.free["scalar"].n, ring.free["vector"].n)
    for so in s_yo:
        P.wait("sync", so, so.n)
    P.end()
    if stage <= 5:
        top.close()
        return nc

    P.begin()
    tk = Tk("z")
    NG = 8
    gb = [P.sb("gb%d" % i, [128, D], F32) for i in range(NG)]
    acc = [P.sb("acc%d" % i, [128, D], F32) for i in range(2)]
    x1l = [P.sb("x1l%d" % i, [128, D], F32) for i in range(2)]
    lnr2 = P.sb("lnr2", [128, 2, D], F32)
    bst2 = P.sb("bst2", [128, 2, 6], F32)
    spc = P.sb("spc", [128, 512], F32)
    junk2 = P.sb("junk2", [128, D], F32)
    s12 = P.sb("s12", [128, 2], F32)
    msq = P.sb("msq", [128, 1], F32)
    mv2 = P.sb("mv2", [128, 2], F32)
    sd2 = P.sb("sd2", [128, 1], F32)
    rs2 = P.sb("rs2", [128, 1], F32)
    s_g = [P.sem("zg%d" % i) for i in range(NG)]
    s_x1 = [P.sem("zx0"), P.sem("zx1")]
    s_oo = [P.sem("zo0"), P.sem("zo1")]
    s_l = P.sem("zl")
    v_l = P.dma("sync", lnr2[:], lnrows[:, 2:4, :], s_l)
    breg2 = {}

    def _mk_reg2(e):
        breg2["r"] = e.alloc_register("bchk2")
        e.reg_mov(breg2["r"], CPAD - 1)

    P.op("gpsimd", _mk_reg2)
    for i in range(NG):
        P.op("gpsimd", lambda e, i=i: e.memset(gb[i][:], 0.0))
    P.wait("vector", s_l, v_l)
    gcount = 0
    g_hist = []
    gfree = []
    o_hist = []
    accfree = {}
    NGA = 32 * NE
    vgs = {}
    vx1s = {}

    def issue(i):
        t, e_ = (i // NE, i % NE) if i < NGA else (0, 0)
        g = gb[i % NG]
        if i >= NG:
            P.wait("gpsimd", tk.s["vector"], gfree[i - NG])
        vgs[i] = P.op("gpsimd", lambda e, e_=e_, t=t, g=g: e.indirect_dma_start(out=g[:, :], out_offset=None, in_=YE[e_], in_offset=bass.IndirectOffsetOnAxis(ap=slotidx[:, e_, t:t + 1], axis=0), bounds_check=breg2["r"], oob_is_err=False), s_g[i % NG], 16)

    def consume(i):
        t, e_ = i // NE, i % NE
        a = acc[t % 2]
        g = gb[i % NG]
        if e_ == 0:
            xl = x1l[t % 2]
            if t >= 2:
                P.wait("sync", tk.s["vector"], accfree[t - 2])
            vx1s[t] = P.dma("sync", xl[:], X1[t * 128:(t + 1) * 128, :], s_x1[t % 2])
        for k in (i, i + 1, i + 2):
            P.wait("vector", s_g[k % NG], vgs[k])
        if e_ == 0:
            if t >= 2:
                P.wait("vector", s_oo[t % 2], o_hist[t - 2])
            tg = tk.do("vector", lambda e, g=g, a=a, e_=e_, t=t: e.tensor_scalar(out=a[:], in0=g[:], scalar1=gmask[:, e_, t:t + 1], scalar2=None, op0=ALU.mult))
        else:
            tg = tk.do("vector", lambda e, g=g, a=a, e_=e_, t=t: e.scalar_tensor_tensor(out=a[:], in0=g[:], scalar=gmask[:, e_, t:t + 1], in1=a[:], op0=ALU.mult, op1=ALU.add))
        gfree.append(tg[1])

    def finish_tile(t):
        a = acc[t % 2]
        xl = x1l[t % 2]
        vx1 = vx1s[t]
        P.wait("vector", s_x1[t % 2], vx1)
        P.op("vector", lambda e, a=a: e.tensor_tensor(out=a[:], in0=a[:], in1=modrow[:, 3072:4096], op=ALU.mult))
        P.op("vector", lambda e, a=a, xl=xl: e.scalar_tensor_tensor(out=a[:], in0=xl[:], scalar=ALPHA, in1=a[:], op0=ALU.mult, op1=ALU.add))
        tv = tk.do("vector", lambda e, a=a: e.tensor_copy(out=spc[:], in_=a[:, 0:512]))
        P.wait("scalar", tk.s["vector"], tv[1])
        P.op("scalar", lambda e, a=a: e.activation(out=junk2[:], in_=a[:], func=AF.Copy, accum_out=s12[:, 0:1]))
        ts = tk.do("scalar", lambda e, a=a: e.activation(out=junk2[:], in_=a[:], func=AF.Square, accum_out=s12[:, 1:2]))
        P.wait("vector", tk.s["scalar"], ts[1])
        P.opf("vector", lambda e: e.tensor_scalar(out=mv2[:, 0:1], in0=s12[:, 0:1], scalar1=1.0 / D, scalar2=None, op0=ALU.mult))
        P.opf("vector", lambda e: e.tensor_tensor(out=msq[:], in0=mv2[:, 0:1], in1=mv2[:, 0:1], op=ALU.mult))
        t3 = tk.do("vector", lambda e: e.scalar_tensor_tensor(out=mv2[:, 1:2], in0=s12[:, 1:2], scalar=1.0 / D, in1=msq[:], op0=ALU.mult, op1=ALU.subtract))
        t4 = tk.do("scalar", lambda e: e.activation(out=sd2[:], in_=mv2[:, 1:2], func=AF.Sqrt, bias=eps_t[:, 0:1], scale=1.0), [t3])
        P.wait("vector", tk.s["scalar"], t4[1])
        P.opf("vector", lambda e: e.reciprocal(out=rs2[:], in_=sd2[:]))
        P.op("vector", lambda e, a=a: e.tensor_scalar(out=a[:], in0=a[:], scalar1=mv2[:, 0:1], scalar2=rs2[:, 0:1], op0=ALU.subtract, op1=ALU.mult))
        P.op("vector", lambda e, a=a: e.tensor_tensor(out=a[:], in0=a[:], in1=lnr2[:, 0, :], op=ALU.mult))
        t5 = tk.do("vector", lambda e, a=a: e.tensor_tensor(out=a[:], in0=a[:], in1=lnr2[:, 1, :], op=ALU.add))
        accfree[t] = t5[1]
        P.wait("sync", tk.s["vector"], t5[1])
        o_hist.append(P.dma("sync", out[t * 128:(t + 1) * 128, :], a[:], s_oo[t % 2]))
    for i in range(NGA + 2):
        issue(i)
        if i >= 2:
            consume(i - 2)
            if (i - 2) % NE == NE - 1:
                finish_tile((i - 2) // NE)
    for so in s_oo:
        P.wait("sync", so, so.n)
    P.end()
    top.close()
    return nc


def _rope_tables(tok_idx):
    row = (tok_idx // 64).astype(np.float32)
    col = (tok_idx % 64).astype(np.float32)

    def tab(h):
        half = h // 2
        inv = (10000.0 ** (-(np.arange(half, dtype=np.float32) * 2.0 / h))).astype(np.float32)
        ar = (row[None, :] * inv[:, None]).astype(np.float32)
        ac = (col[None, :] * inv[:, None]).astype(np.float32)
        c = np.concatenate([np.cos(ar), np.cos(ar), np.cos(ac), np.cos(ac)], 0)
        s = np.concatenate([-np.sin(ar), np.sin(ar), -np.sin(ac), np.sin(ac)], 0)
        return c.astype(np.float32), s.astype(np.float32)

    c64, s64 = tab(32)
    c32, s32 = tab(16)
    return (np.tile(c64, (2, 1)), np.tile(s64, (2, 1)), np.tile(c32, (4, 1)), np.tile(s32, (4, 1)))


def _perm(d):
    q = d // 4
    return np.concatenate([np.arange(q, 2 * q), np.arange(0, q), np.arange(3 * q, 4 * q), np.arange(2 * q, 3 * q)])


def _prep(inp):
    f = lambda a: np.ascontiguousarray(np.asarray(a, dtype=np.float32))
    x, c, ctx, c_ctx = f(inp["x"]), f(inp["c"]), f(inp["ctx"]), f(inp["c_ctx"])
    w_in = f(inp["w_in"])[0]
    p64 = _perm(64)
    p32 = _perm(32)
    w_all = np.zeros((D, NW), np.float32)
    w_all[:, O_CQ:O_CQ + 256] = w_in[:, 0:256]
    w_all[:, O_CKV:O_CKV + 128] = w_in[:, 256:384]
    w_all[:, O_KR:O_KR + 32] = w_in[:, 384:416]
    w_all[:, O_KRP:O_KRP + 32] = w_in[:, 384:416][:, p32]
    dq = w_in[:, 416:928]
    dk = w_in[:, 928:1440]
    pall = np.concatenate([g * 64 + p64 for g in range(8)])
    w_all[:, O_DQ:O_DQ + 512] = dq
    w_all[:, O_DQP:O_DQP + 512] = dq[:, pall]
    w_all[:, O_DK:O_DK + 512] = dk
    w_all[:, O_DKP:O_DKP + 512] = dk[:, pall]
    w_all[:, O_DV:O_DV + 512] = w_in[:, 1440:1952]
    w_all[:, O_G:O_G + 2048] = w_in[:, 1952:4000]
    wuq = f(inp["mla_w_uq"])[0].reshape(256, 8, 96)
    w_uq_all = np.zeros((256, 1024), np.float32)
    w_uq_all[:, 0:512] = wuq[:, :, 0:64].reshape(256, 512)
    w_uq_all[:, 512:768] = wuq[:, :, 64:96].reshape(256, 256)
    w_uq_all[:, 768:1024] = wuq[:, :, 64:96][:, :, p32].reshape(256, 256)
    wukv = f(inp["mla_w_ukv"])[0].reshape(128, 8, 128)
    w_ukv_all = np.concatenate([wukv[:, :, 0:64].reshape(128, 512), wukv[:, :, 64:128].reshape(128, 512)], 1)
    b_ada = f(inp["b_ada"])[0]
    common = {
        "w_ada": f(inp["w_ada"])[0],
        "b_ada_fm": np.ascontiguousarray(b_ada[0:2048].reshape(16, 128).T),
        "b_ada_row": b_ada.reshape(1, 6 * D),
        "w_all": w_all,
        "w_uq_all": w_uq_all,
        "w_ukv_all": np.ascontiguousarray(w_ukv_all),
        "g_q_fm": np.ascontiguousarray(f(inp["mla_g_q"])[0].reshape(2, 128).T),
        "g_kv_fm": f(inp["mla_g_kv"])[0].reshape(128, 1),
        "w_o_mla": f(inp["mla_w_o"])[0],
        "w_o_diff": f(inp["diff_w_o"])[0],
        "w_out": f(inp["w_out"])[0],
        "dlam": np.ascontiguousarray(np.broadcast_to(f(inp["diff_lambda"])[0].reshape(1, 256), (128, 256))),
        "g_sub_fm": f(inp["diff_g_subln"])[0].reshape(128, 1),
        "lnrows": np.ascontiguousarray(np.broadcast_to(np.stack([f(inp["ln1_g"])[0], f(inp["ln1_b"])[0], f(inp["ln2_g"])[0], f(inp["ln2_b"])[0]], 0)[None], (128, 4, D))),
        "w_router": f(inp["moe_w_router"])[0],
        "b_router": f(inp["moe_b_router"])[0].reshape(1, NE),
        "moe_w1": f(inp["moe_w1"])[0],
        "moe_w3": f(inp["moe_w3"])[0],
        "moe_w2": f(inp["moe_w2"])[0],
    }
    maps = []
    for core in range(8):
        b, hf = core // 2, core % 2
        own = np.arange(hf * NQ, (hf + 1) * NQ)
        oth = np.arange((1 - hf) * NQ, (2 - hf) * NQ)
        order = np.concatenate([own, oth])
        c64, s64, c32, s32 = _rope_tables(order)
        m = dict(common)
        m["xT"] = np.ascontiguousarray(x[b][order].T)
        m["xown"] = np.ascontiguousarray(x[b][own])
        m["ctxT"] = np.ascontiguousarray(ctx[b].T)
        cv = np.stack([c[b].reshape(8, 128).T, c_ctx.reshape(8, 128).T], -1)
        m["cvec"] = np.ascontiguousarray(cv)
        m["rope64_c"], m["rope64_s"], m["rope32_c"], m["rope32_s"] = c64, s64, c32, s32
        maps.append(m)
    return maps


_NC_CACHE = {}


def kernel(**inputs):
    maps = _prep(inputs)
    if "nc" not in _NC_CACHE:
        _NC_CACHE["nc"] = build()
    res = run_bass_kernel_spmd(_NC_CACHE["nc"], maps, core_ids=list(range(8)))
    outp = np.zeros((4, SEQ, D), np.float32)
    for core in range(8):
        b, hf = core // 2, core % 2
        outp[b, hf * NQ:(hf + 1) * NQ] = res.results[core]["out"]
    return outp
```
